# Optimizing a Trainium2 kernel written in Bass

```python
import jax, jax.numpy as jnp
from jax import lax
import numpy as np

D_MODEL = 2048
BATCH = 2
SEQ = 16384
DEPTH = 2

GRID_W = 64
CTX_LEN = 256
HEAD_DIM = 128
A_HEADS = 8
A_KV_HEADS = 2
A_WINDOW = 128
A_BLOCK = 128
B_HEADS = 8
NA_ROWS = 8
NA_COLS = 16
ROPE_THETA = 10000.0
A_Q_COLS = A_HEADS * HEAD_DIM
A_KV_COLS = A_KV_HEADS * HEAD_DIM
B_COLS = B_HEADS * HEAD_DIM
QKV_COLS = A_Q_COLS + 2 * A_KV_COLS + 3 * B_COLS
QKV_SPLITS = (A_Q_COLS, A_Q_COLS + A_KV_COLS, A_Q_COLS + 2 * A_KV_COLS, A_Q_COLS + 2 * A_KV_COLS + B_COLS, A_Q_COLS + 2 * A_KV_COLS + 2 * B_COLS)
MIX_WIDTH = A_Q_COLS + B_COLS
RW_HEAD = 64
RW_HEADS = D_MODEL // RW_HEAD
RW_DECAY_LORA = 96
RW_AAA_LORA = 96
RW_GATE_LORA = 256
RW_LNX_EPS = 64e-5
N_GROUPS = 4
EXPERTS_PER_GROUP = 8
N_EXPERTS = N_GROUPS * EXPERTS_PER_GROUP
TOP_K = 2
EXPERT_FF = 512
MOE_BLOCK = 128
NORM_EPS = 1e-6
NEG_INF = -1e30

kernel_name = 'hybrid_diffusion_backbone'


def rmsnorm(x, g):
    xf = x.astype(jnp.float32)
    y = xf * lax.rsqrt(jnp.mean(xf * xf, axis=-1, keepdims=True) + NORM_EPS)
    return (y * g.astype(jnp.float32)).astype(x.dtype)


def ada_split(silu_cond, w, b):
    mod = (silu_cond @ w + b)[..., None, :]
    return jnp.split(mod, 6, axis=-1)


def axial_rope(x, row_pos, col_pos):
    half = x.shape[-1] // 2
    quarter = half // 2
    inv_freq = ROPE_THETA ** (-jnp.arange(quarter, dtype=jnp.float32) / quarter)

    def rotate(xp, pos):
        ang = pos.astype(jnp.float32)[:, None] * inv_freq[None, :]
        cos, sin = jnp.cos(ang)[None, :, None, :], jnp.sin(ang)[None, :, None, :]
        x1, x2 = xp[..., :quarter], xp[..., quarter:]
        return jnp.concatenate([x1 * cos - x2 * sin, x1 * sin + x2 * cos], axis=-1)

    xf = x.astype(jnp.float32)
    return jnp.concatenate([rotate(xf[..., :half], row_pos), rotate(xf[..., half:], col_pos)], axis=-1).astype(x.dtype)


def softmax_with_sink(logits, sink):
    m = jnp.maximum(jnp.max(logits, axis=-1, keepdims=True), sink)
    p = jnp.exp(logits - m)
    return p / (jnp.sum(p, axis=-1, keepdims=True) + jnp.exp(sink - m))


def split_heads(p):
    B, L, _ = p.shape
    parts = jnp.split(p, QKV_SPLITS, axis=-1)
    return tuple(t.reshape(B, L, -1, HEAD_DIM) for t in parts)


def window_attention(q, k, v, kc, vc, sink):
    B, L, _, dh = q.shape
    G = A_HEADS // A_KV_HEADS
    nb = L // A_BLOCK
    scale = dh ** -0.5
    qb = q.reshape(B, nb, A_BLOCK, A_KV_HEADS, G, dh)
    pad = ((0, 0), (A_BLOCK, A_BLOCK), (0, 0), (0, 0))
    kp = jnp.pad(k, pad).reshape(B, nb + 2, A_BLOCK, A_KV_HEADS, dh)
    vp = jnp.pad(v, pad).reshape(B, nb + 2, A_BLOCK, A_KV_HEADS, dh)
    kw = jnp.concatenate([kp[:, :-2], kp[:, 1:-1], kp[:, 2:]], axis=2)
    vw = jnp.concatenate([vp[:, :-2], vp[:, 1:-1], vp[:, 2:]], axis=2)
    blk = jnp.arange(nb)[:, None]
    qpos = blk * A_BLOCK + jnp.arange(A_BLOCK)[None, :]
    kpos = (blk - 1) * A_BLOCK + jnp.arange(3 * A_BLOCK)[None, :]
    valid = (jnp.abs(qpos[:, :, None] - kpos[:, None, :]) <= A_WINDOW) & (kpos[:, None, :] >= 0) & (kpos[:, None, :] < L)
    s_win = jnp.einsum('bnqkgd,bnskd->bnkgqs', qb, kw).astype(jnp.float32) * scale
    s_win = jnp.where(valid[None, :, None, None], s_win, NEG_INF)
    s_ctx = jnp.einsum('bnqkgd,bckd->bnkgqc', qb, kc).astype(jnp.float32) * scale
    sink_b = sink.astype(jnp.float32).reshape(A_KV_HEADS, G)[None, None, :, :, None, None]
    p = softmax_with_sink(jnp.concatenate([s_win, s_ctx], axis=-1), sink_b).astype(v.dtype)
    nw = 3 * A_BLOCK
    o = jnp.einsum('bnkgqs,bnskd->bnqkgd', p[..., :nw], vw) + jnp.einsum('bnkgqc,bckd->bnqkgd', p[..., nw:], vc)
    return o.reshape(B, L, A_HEADS * dh)


def neighbourhood_attention(q, k, v, kc, vc, rpb):
    B, L, H, dh = q.shape
    rows = L // GRID_W
    kr = min(NA_ROWS, rows)
    scale = dh ** -0.5
    r = jnp.arange(rows)
    col = jnp.arange(GRID_W)
    r0 = jnp.clip(r - kr // 2, 0, rows - kr)
    c0 = jnp.clip(col - NA_COLS // 2, 0, GRID_W - NA_COLS)
    band = r0[:, None] + jnp.arange(kr)[None, :]
    qg = q.reshape(B, rows, GRID_W, H, dh)
    kg = k.reshape(B, rows, GRID_W, H, dh)[:, band]
    vg = v.reshape(B, rows, GRID_W, H, dh)[:, band]
    col_ok = (col[None, :] >= c0[:, None]) & (col[None, :] < c0[:, None] + NA_COLS)
    dr = band - r[:, None] + (NA_ROWS - 1)
    dc = jnp.clip(col[None, :] - col[:, None] + (NA_COLS - 1), 0, 2 * NA_COLS - 2)
    bias = rpb.astype(jnp.float32)[:, dr[:, None, :, None], dc[None, :, None, :]]
    s_nb = jnp.einsum('brqhd,brjwhd->bhrqjw', qg, kg).astype(jnp.float32) * scale + bias
    s_nb = jnp.where(col_ok[:, None, :], s_nb, NEG_INF).reshape(B, H, rows, GRID_W, kr * GRID_W)
    s_ctx = jnp.einsum('brqhd,bchd->bhrqc', qg, kc).astype(jnp.float32) * scale
    p = jax.nn.softmax(jnp.concatenate([s_nb, s_ctx], axis=-1), axis=-1).astype(v.dtype)
    nk = kr * GRID_W
    p_nb = p[..., :nk].reshape(B, H, rows, GRID_W, kr, GRID_W)
    o = jnp.einsum('bhrqjw,brjwhd->brqhd', p_nb, vg) + jnp.einsum('bhrqc,bchd->brqhd', p[..., nk:], vc)
    return o.reshape(B, L, H * dh)


def context_attention(qc, kc, vc, sink):
    B, C, Hq, dh = qc.shape
    Hk = kc.shape[2]
    G = Hq // Hk
    qg = qc.reshape(B, C, Hk, G, dh)
    s = jnp.einsum('bqkgd,bckd->bkgqc', qg, kc).astype(jnp.float32) * dh ** -0.5
    if sink is None:
        p = jax.nn.softmax(s, axis=-1)
    else:
        p = softmax_with_sink(s, sink.astype(jnp.float32).reshape(Hk, G)[None, :, :, None, None])
    o = jnp.einsum('bkgqc,bckd->bqkgd', p.astype(vc.dtype), vc)
    return o.reshape(B, C, Hq * dh)


def attention_layer(h, hc, w_in, w_out, a_q_gain, a_k_gain, a_sink, b_q_gain, b_k_gain, b_rpb, row_pos, col_pos, need_ctx_out):
    qa, ka, va, qb, kb, vb = split_heads(h @ w_in)
    qa_c, ka_c, va_c, qb_c, kb_c, vb_c = split_heads(hc @ w_in)
    qa = axial_rope(rmsnorm(qa, a_q_gain), row_pos, col_pos)
    ka = axial_rope(rmsnorm(ka, a_k_gain), row_pos, col_pos)
    qb, kb = rmsnorm(qb, b_q_gain), rmsnorm(kb, b_k_gain)
    ka_c, kb_c = rmsnorm(ka_c, a_k_gain), rmsnorm(kb_c, b_k_gain)
    ya = window_attention(qa, ka, va, ka_c, va_c, a_sink)
    yb = neighbourhood_attention(qb, kb, vb, kb_c, vb_c, b_rpb)
    y = jnp.concatenate([ya, yb], axis=-1) @ w_out
    if not need_ctx_out:
        return y, None
    yac = context_attention(rmsnorm(qa_c, a_q_gain), ka_c, va_c, a_sink)
    ybc = context_attention(rmsnorm(qb_c, b_q_gain), kb_c, vb_c, None)
    return y, jnp.concatenate([yac, ybc], axis=-1) @ w_out


def centred_shift(x):
    xp = jnp.pad(x, ((0, 0), (1, 1), (0, 0)))
    return 0.5 * (xp[:, :-2] + xp[:, 2:]) - x


def l2_normalize(x):
    xf = x.astype(jnp.float32)
    return (xf * lax.rsqrt(jnp.sum(xf * xf, axis=-1, keepdims=True) + 1e-12)).astype(x.dtype)


def rwkv_prepare(x, mu, wr, wk, wv, w0, w1, w2, a0, a1, a2, g1, g2, k_k, k_a):
    B, L, D = x.shape
    heads = lambda t: t.reshape(B, L, RW_HEADS, RW_HEAD)
    xx = centred_shift(x)
    xr, xw, xk, xv, xa, xg = (x + xx * mu[m] for m in range(6))
    r = heads(xr @ wr)
    k = xk @ wk
    v = heads(xv @ wv)
    g = jax.nn.sigmoid(xg @ g1) @ g2
    kk = l2_normalize(heads(k * k_k))
    dirs = []
    for d in range(2):
        w_log = -jax.nn.softplus(-(w0[d] + jnp.tanh(xw @ w1[d]) @ w2[d]).astype(jnp.float32)) - 0.5
        decay = heads(jnp.exp(-jnp.exp(w_log)))
        a = jax.nn.sigmoid(a0[d] + (xa @ a1[d]) @ a2[d])
        kd = heads(k * (1 + (a - 1) * k_a))
        dirs.append((decay, kd, kk * heads(a)))
    return r, v, g, kk, dirs


def wkv_scan(state0, r, decay, k, v, a, b, reverse):
    xs = tuple(jnp.moveaxis(t.astype(jnp.float32), 1, 0) for t in (r, decay, k, v, a, b))

    def step(S, inp):
        rt, wt, kt, vt, at, bt = inp
        sa = jnp.einsum('bhvk,bhk->bhv', S, at)
        S = S * wt[:, :, None, :] + sa[..., None] * bt[:, :, None, :] + vt[..., None] * kt[:, :, None, :]
        return S, jnp.einsum('bhvk,bhk->bhv', S, rt)

    state, ys = lax.scan(step, state0, xs, reverse=reverse)
    return jnp.moveaxis(ys, 0, 1), state


def rwkv_output(y, r, v, g, k_sum, r_k, lnx_w, lnx_b, wo):
    B, L, H, N = y.shape
    mu = jnp.mean(y, axis=-1, keepdims=True)
    var = jnp.mean(jnp.square(y - mu), axis=-1, keepdims=True)
    yn = ((y - mu) * lax.rsqrt(var + RW_LNX_EPS)).reshape(B, L, H * N) * lnx_w + lnx_b
    bonus = jnp.sum((r * k_sum * r_k).astype(jnp.float32), axis=-1, keepdims=True) * v.astype(jnp.float32)
    return ((yn + bonus.reshape(B, L, H * N)).astype(g.dtype) * g) @ wo


def rwkv_layer(h, hc, mu, wr, wk, wv, wo, w0, w1, w2, a0, a1, a2, g1, g2, k_k, k_a, r_k, lnx_w, lnx_b, need_ctx_out):
    prep = lambda t: rwkv_prepare(t, mu, wr, wk, wv, w0, w1, w2, a0, a1, a2, g1, g2, k_k, k_a)
    r, v, g, kk, dirs = prep(h)
    rc, vc, gc, kkc, dirs_c = prep(hc)
    zero = jnp.zeros((h.shape[0], RW_HEADS, RW_HEAD, RW_HEAD), jnp.float32)
    y_parts, yc_parts = [], []
    for d, reverse in enumerate((False, True)):
        decay_c, k_c, b_c = dirs_c[d]
        yc_d, state_c = wkv_scan(zero, rc, decay_c, k_c, vc, -kkc, b_c, reverse)
        decay, kd, bd = dirs[d]
        y_d, _ = wkv_scan(state_c, r, decay, kd, v, -kk, bd, reverse)
        y_parts.append(y_d)
        yc_parts.append(yc_d)
    out = rwkv_output(y_parts[0] + y_parts[1], r, v, g, dirs[0][1] + dirs[1][1], r_k, lnx_w, lnx_b, wo)
    if not need_ctx_out:
        return out, None
    out_c = rwkv_output(yc_parts[0] + yc_parts[1], rc, vc, gc, dirs_c[0][1] + dirs_c[1][1], r_k, lnx_w, lnx_b, wo)
    return out, out_c


def hierarchical_moe(xt, w_grp, w_exp, w1, w3, w2):
    n, D = xt.shape
    xf = xt.astype(jnp.float32)
    p_grp = jax.nn.softmax(xf @ w_grp.astype(jnp.float32), axis=-1)
    g_sel = jnp.argmax(p_grp, axis=-1)
    p_sel = jnp.take_along_axis(p_grp, g_sel[:, None], axis=-1)
    logits = (xf @ w_exp.astype(jnp.float32)).reshape(n, N_GROUPS, EXPERTS_PER_GROUP)
    logits_g = jnp.take_along_axis(logits, g_sel[:, None, None], axis=1)[:, 0]
    top_val, top_idx = lax.top_k(logits_g, TOP_K)
    gate = p_sel * jax.nn.softmax(top_val, axis=-1)
    expert = g_sel[:, None] * EXPERTS_PER_GROUP + top_idx
    flat_e = expert.reshape(-1)
    flat_w = gate.reshape(-1)
    flat_tok = jnp.repeat(jnp.arange(n, dtype=jnp.int32), TOP_K)
    order = jnp.argsort(flat_e)
    se, stok, sw = flat_e[order], flat_tok[order], flat_w[order]
    counts = jnp.bincount(flat_e, length=N_EXPERTS)
    padded = (counts + MOE_BLOCK - 1) // MOE_BLOCK * MOE_BLOCK
    pad_end = jnp.cumsum(padded)
    start = jnp.cumsum(counts) - counts
    dest = pad_end[se] - padded[se] + jnp.arange(n * TOP_K) - start[se]
    n_blk = (n * TOP_K + N_EXPERTS * (MOE_BLOCK - 1) + MOE_BLOCK - 1) // MOE_BLOCK
    slots = n_blk * MOE_BLOCK
    slot_tok = jnp.full((slots,), n, jnp.int32).at[dest].set(stok)
    slot_w = jnp.zeros((slots,), jnp.float32).at[dest].set(sw)
    blk_expert = jnp.minimum(jnp.searchsorted(pad_end, jnp.arange(n_blk) * MOE_BLOCK, side='right'), N_EXPERTS - 1)
    x_rows = jnp.concatenate([xt, jnp.zeros((1, D), xt.dtype)], axis=0)[slot_tok].reshape(n_blk, MOE_BLOCK, D)

    def expert_block(args):
        xb, e = args
        return (jax.nn.silu(xb @ w1[e]) * (xb @ w3[e])) @ w2[e]

    y = lax.map(expert_block, (x_rows, blk_expert)).reshape(slots, D)
    out = jnp.zeros((n + 1, D), xt.dtype).at[slot_tok].add(y * slot_w[:, None].astype(xt.dtype))
    return out[:n]


def setup_inputs(seed: int = 0) -> dict:
    key = jax.random.key(seed)
    keys = iter(jax.random.split(key, 64))

    def normal(shape, scale):
        return jax.random.normal(next(keys), shape, jnp.float32) * scale

    def uniform(shape, lo, hi):
        return jax.random.uniform(next(keys), shape, jnp.float32, lo, hi)

    D = D_MODEL
    n_att = (DEPTH + 1) // 2
    n_rw = DEPTH // 2
    return {
        'x': normal((BATCH, SEQ, D), 1.0),
        'c': normal((BATCH, D), 1.0),
        'ctx': normal((BATCH, CTX_LEN, D), 1.0),
        'c_ctx': normal((D,), 1.0),
        'ada_w': normal((DEPTH, D, 6 * D), 0.5 * D ** -0.5),
        'ada_b': normal((DEPTH, 6 * D), 0.02),
        'norm_mix_g': 1.0 + normal((DEPTH, D), 0.02),
        'norm_ffn_g': 1.0 + normal((DEPTH, D), 0.02),
        'attn_w_in': normal((n_att, D, QKV_COLS), D ** -0.5),
        'attn_w_out': normal((n_att, MIX_WIDTH, D), MIX_WIDTH ** -0.5),
        'a_q_gain': 1.0 + normal((n_att, HEAD_DIM), 0.02),
        'a_k_gain': 1.0 + normal((n_att, HEAD_DIM), 0.02),
        'a_sink': normal((n_att, A_HEADS), 1.0),
        'b_q_gain': 1.0 + normal((n_att, HEAD_DIM), 0.02),
        'b_k_gain': 1.0 + normal((n_att, HEAD_DIM), 0.02),
        'b_rpb': normal((n_att, B_HEADS, 2 * NA_ROWS - 1, 2 * NA_COLS - 1), 0.5),
        'rw_mu': uniform((n_rw, 6, D), 0.0, 1.0),
        'rw_wr': normal((n_rw, D, D), D ** -0.5),
        'rw_wk': normal((n_rw, D, D), D ** -0.5),
        'rw_wv': normal((n_rw, D, D), D ** -0.5),
        'rw_wo': normal((n_rw, D, D), D ** -0.5),
        'rw_w0': uniform((n_rw, 2, D), -6.0, -1.0),
        'rw_w1': normal((n_rw, 2, D, RW_DECAY_LORA), D ** -0.5),
        'rw_w2': normal((n_rw, 2, RW_DECAY_LORA, D), 0.3 * RW_DECAY_LORA ** -0.5),
        'rw_a0': normal((n_rw, 2, D), 0.5),
        'rw_a1': normal((n_rw, 2, D, RW_AAA_LORA), D ** -0.5),
        'rw_a2': normal((n_rw, 2, RW_AAA_LORA, D), 0.5 * RW_AAA_LORA ** -0.5),
        'rw_g1': normal((n_rw, D, RW_GATE_LORA), D ** -0.5),
        'rw_g2': normal((n_rw, RW_GATE_LORA, D), RW_GATE_LORA ** -0.5),
        'rw_k_k': 1.0 + normal((n_rw, D), 0.1),
        'rw_k_a': 1.0 + normal((n_rw, D), 0.1),
        'rw_r_k': normal((n_rw, RW_HEADS, RW_HEAD), 0.1),
        'rw_lnx_w': 1.0 + normal((n_rw, D), 0.02),
        'rw_lnx_b': normal((n_rw, D), 0.02),
        'moe_w_grp': normal((DEPTH, D, N_GROUPS), D ** -0.5),
        'moe_w_exp': normal((DEPTH, D, N_EXPERTS), D ** -0.5),
        'moe_w1': normal((DEPTH, N_EXPERTS, D, EXPERT_FF), D ** -0.5),
        'moe_w3': normal((DEPTH, N_EXPERTS, D, EXPERT_FF), D ** -0.5),
        'moe_w2': normal((DEPTH, N_EXPERTS, EXPERT_FF, D), EXPERT_FF ** -0.5),
    }


def reference(x, c, ctx, c_ctx, ada_w, ada_b, norm_mix_g, norm_ffn_g, attn_w_in, attn_w_out, a_q_gain, a_k_gain, a_sink, b_q_gain, b_k_gain, b_rpb, rw_mu, rw_wr, rw_wk, rw_wv, rw_wo, rw_w0, rw_w1, rw_w2, rw_a0, rw_a1, rw_a2, rw_g1, rw_g2, rw_k_k, rw_k_a, rw_r_k, rw_lnx_w, rw_lnx_b, moe_w_grp, moe_w_exp, moe_w1, moe_w3, moe_w2):
    B, L, D = x.shape
    t = jnp.arange(L)
    row_pos, col_pos = t // GRID_W, t % GRID_W
    cond = jax.nn.silu(c)
    cond_ctx = jax.nn.silu(c_ctx)
    xc = ctx
    for i in range(DEPTH):
        last = i == DEPTH - 1
        j = i // 2
        sh1, sc1, gt1, sh2, sc2, gt2 = ada_split(cond, ada_w[i], ada_b[i])
        csh1, csc1, cgt1, csh2, csc2, cgt2 = ada_split(cond_ctx, ada_w[i], ada_b[i])
        h = rmsnorm(x, norm_mix_g[i]) * (1 + sc1) + sh1
        hc = rmsnorm(xc, norm_mix_g[i]) * (1 + csc1) + csh1
        if i % 2 == 0:
            y, yc = attention_layer(h, hc, attn_w_in[j], attn_w_out[j], a_q_gain[j], a_k_gain[j], a_sink[j], b_q_gain[j], b_k_gain[j], b_rpb[j], row_pos, col_pos, not last)
        else:
            y, yc = rwkv_layer(h, hc, rw_mu[j], rw_wr[j], rw_wk[j], rw_wv[j], rw_wo[j], rw_w0[j], rw_w1[j], rw_w2[j], rw_a0[j], rw_a1[j], rw_a2[j], rw_g1[j], rw_g2[j], rw_k_k[j], rw_k_a[j], rw_r_k[j], rw_lnx_w[j], rw_lnx_b[j], not last)
        x = x + gt1 * y
        h = rmsnorm(x, norm_ffn_g[i]) * (1 + sc2) + sh2
        experts = (moe_w_grp[i], moe_w_exp[i], moe_w1[i], moe_w3[i], moe_w2[i])
        if last:
            x = x + gt2 * hierarchical_moe(h.reshape(B * L, D), *experts).reshape(B, L, D)
        else:
            xc = xc + cgt1 * yc
            hc = rmsnorm(xc, norm_ffn_g[i]) * (1 + csc2) + csh2
            f = hierarchical_moe(jnp.concatenate([h.reshape(B * L, D), hc.reshape(-1, D)], axis=0), *experts)
            x = x + gt2 * f[:B * L].reshape(B, L, D)
            xc = xc + cgt2 * f[B * L:].reshape(hc.shape)
    return x
```

```python
import contextlib
import numpy as np
import concourse.bass as bass
import concourse.mybir as mybir
from concourse.bass_utils import run_bass_kernel_spmd

F32 = mybir.dt.float32
BF16 = mybir.dt.bfloat16
I32 = mybir.dt.int32
ALU = mybir.AluOpType
AF = mybir.ActivationFunctionType
AX = mybir.AxisListType

ENG = {'pe': 'tensor', 'act': 'scalar', 'dve': 'vector', 'pool': 'gpsimd', 'sp': 'sync'}


class _St:
    pass


class Buf:
    def __init__(self, name, t, st=None):
        self.t = t
        if st is None:
            st = _St()
            st.name = name
            st.w = {}
            st.r = {}
            st.dcnt = 0
            st.pre = {}
            st.is_psum = False
        self.__dict__['_s'] = st

    def alias(self, ap):
        return Buf(None, ap, st=self._s)

    def __getattr__(self, k):
        if k in ('name', 'w', 'r', 'dcnt', 'pre', 'is_psum'):
            return getattr(self.__dict__['_s'], k)
        raise AttributeError(k)

    def __setattr__(self, k, v):
        if k in ('name', 'w', 'r', 'dcnt', 'pre', 'is_psum'):
            setattr(self.__dict__['_s'], k, v)
        else:
            self.__dict__[k] = v

    def __getitem__(self, idx):
        return self.t[idx]


class Prog:
    def __init__(self):
        self.nc = bass.Bass("TRN2", target_bir_lowering=False)
        self.stack = contextlib.ExitStack()
        self.stream = {e: [] for e in ENG}
        self.cnt = {e: 0 for e in ENG}
        self.sems = {}
        self.seen = {e: {} for e in ENG}
        self.outs = []
        self.nbuf = 0
        self.dtot = {}

    def sem(self, key):
        if key not in self.sems:
            self.sems[key] = self.stack.enter_context(self.nc.semaphore("s%d_%s" % (len(self.sems), key[:20])))
        return self.sems[key]

    def sbuf(self, name, shape, dt=F32):
        self.nbuf += 1
        t = self.stack.enter_context(self.nc.sbuf_tensor("%s_%d" % (name, self.nbuf), list(shape), dt))
        return Buf("%s_%d" % (name, self.nbuf), t)

    def psum(self, name, shape, dt=F32):
        self.nbuf += 1
        t = self.stack.enter_context(self.nc.psum_tensor("%s_%d" % (name, self.nbuf), list(shape), dt))
        b = Buf("%s_%d" % (name, self.nbuf), t)
        b.is_psum = True
        return b

    def dram(self, name, shape, dt=F32, kind="Internal"):
        t = self.nc.dram_tensor(name, list(shape), dt, kind=kind).ap()
        b = Buf(name, t)
        if kind == "ExternalOutput":
            self.outs.append(b)
        return b

    def _waits(self, eng, r, w, dkey=None, selfsync=True):
        waits = {}
        for b in r:
            for k, v in b.w.items():
                waits[k] = max(waits.get(k, 0), v)
            if b.is_psum:
                for k, v in b.r.items():
                    if k != 'E_' + eng:
                        waits[k] = max(waits.get(k, 0), v)
        for b in w:
            for k, v in list(b.w.items()) + list(b.r.items()):
                if dkey is not None and k == dkey:
                    continue
                waits[k] = max(waits.get(k, 0), v)
        if eng == 'pe' or not selfsync:
            waits.pop('E_' + eng, None)
        wl = [(k, v) for k, v in waits.items() if v > self.seen[eng].get(k, 0)]
        for k, v in wl:
            self.seen[eng][k] = v
        return wl

    def op(self, eng, fn, r=(), w=(), selfsync=True):
        wl = self._waits(eng, r, w, selfsync=selfsync)
        key = 'E_' + eng
        self.sem(key)
        for k, _ in wl:
            self.sem(k)
        self.cnt[eng] += 1
        n = self.cnt[eng]
        self.stream[eng].append((wl, fn, (key, 1)))
        for b in r:
            b.r[key] = n
        for b in w:
            b.w = {key: n}
            b.r = {}
        return n

    def dma(self, q, out_ap, in_ap, r=(), w=(), **kw):
        assert len(w) == 1
        wb = w[0]
        dkey = 'D_' + wb.name
        self.sem(dkey)
        full = dict(wb.pre)
        for b in r:
            for k, v in b.w.items():
                full[k] = max(full.get(k, 0), v)
        for k, v in list(wb.w.items()) + list(wb.r.items()):
            if k != dkey:
                full[k] = max(full.get(k, 0), v)
        wb.pre = dict(full)
        wl = [(k, v) for k, v in full.items() if v > self.seen[q].get(k, 0)]
        for k, v in wl:
            self.seen[q][k] = v
            self.sem(k)
        wb.dcnt += 16
        val = wb.dcnt
        self.dtot[dkey] = val
        self.stream[q].append((wl, (lambda e: e.dma_start(out=out_ap, in_=in_ap, **kw)), (dkey, 16)))
        for b in r:
            b.r[dkey] = val
        keep = {k: v for k, v in wb.w.items() if k == dkey}
        wb.w = keep
        wb.w[dkey] = val
        wb.r = {}

    def barrier(self):
        snap = {}
        for e in ENG:
            if self.cnt[e] > 0:
                snap['E_' + e] = self.cnt[e]
        snap.update(self.dtot)
        for e in ENG:
            wl = [(k, v) for k, v in snap.items() if k != 'E_' + e and v > self.seen[e].get(k, 0)]
            for k, v in wl:
                self.seen[e][k] = v
            if wl:
                self.stream[e].append((wl, None, None))

    def view(self, name, ap):
        self.nbuf += 1
        return Buf("%s_%d" % (name, self.nbuf), ap)

    def finish(self):
        waits = {}
        for b in self.outs:
            for k, v in b.w.items():
                waits[k] = max(waits.get(k, 0), v)
        self.stream['sp'].append((list(waits.items()), None, None))
        with self.nc.Block() as block:
            for e, attr in ENG.items():
                lst = self.stream[e]
                if not lst:
                    continue

                def f(eng, lst=lst):
                    for wl, fn, inc in lst:
                        for k, v in wl:
                            eng.wait_ge(self.sems[k], v)
                        if fn is None:
                            continue
                        ins = fn(eng)
                        if inc is not None:
                            ins.then_inc(self.sems[inc[0]], inc[1])
                getattr(block, attr)(f)
        self.stack.close()
        return self.nc


D = 2048
NCORE = 8
import os
NL = int(os.environ.get('NL', '2'))
ADA_COLS = 6 * D // NCORE


def build_ada():
    P = Prog()
    nc = P.nc
    cT = P.dram("cT", [128, 16, 3], F32, kind="ExternalInput")
    w = P.dram("w", [2, 16, 128, ADA_COLS], F32, kind="ExternalInput")
    b = P.dram("b", [2, 1, ADA_COLS], F32, kind="ExternalInput")
    out = P.dram("out", [2, 3, ADA_COLS], F32, kind="ExternalOutput")
    c_sb = P.sbuf("c_sb", [128, 16, 3])
    s_sb = P.sbuf("s_sb", [128, 16, 3])
    ones = P.sbuf("ones", [1, 3])
    P.dma('sp', c_sb[:], cT[:], w=[c_sb])
    P.op('act', lambda e: e.activation(out=s_sb[:], in_=c_sb[:], func=AF.Silu), r=[c_sb], w=[s_sb])
    P.op('dve', lambda e: e.memset(ones[:], 1.0), w=[ones])
    w_sb = P.sbuf("w_sb", [128, 16, ADA_COLS])
    b_sb = P.sbuf("b_sb", [1, ADA_COLS])
    o_sb = P.sbuf("o_sb", [3, ADA_COLS])
    pss = [P.psum("ps", [3, 512]) for _ in range(3)]
    for i in range(NL):
        for kq in range(4):
            P.dma('sp' if kq % 2 == 0 else 'pool', w_sb[:, kq * 4:(kq + 1) * 4, :],
                  w[i, kq * 4:(kq + 1) * 4].rearrange("k p n -> p k n"), w=[w_sb])
        P.dma('sp', b_sb[:], b[i], w=[b_sb])
        for j in range(ADA_COLS // 512):
            ps = pss[j]
            for kc in range(16):
                P.op('pe', lambda e, kc=kc, j=j, ps=ps, w_sb=w_sb: e.matmul(
                    ps[:], lhsT=s_sb[:, kc, :], rhs=w_sb[:, kc, j * 512:(j + 1) * 512],
                    start=(kc == 0), stop=False), r=[s_sb, w_sb], w=[ps])
            P.op('pe', lambda e, j=j, ps=ps, b_sb=b_sb: e.matmul(
                ps[:], lhsT=ones[:], rhs=b_sb[:, j * 512:(j + 1) * 512], start=False, stop=True),
                r=[ones, b_sb], w=[ps])
            P.op('dve', lambda e, j=j, ps=ps, o_sb=o_sb: e.tensor_copy(out=o_sb[:, j * 512:(j + 1) * 512], in_=ps[:]),
                 r=[ps], w=[o_sb])
        P.dma('sp', out[i], o_sb[:], r=[o_sb], w=[out])
    return P.finish()


def run_ada(c, c_ctx, ada_w, ada_b):
    cvec = np.stack([c[0], c[1], c_ctx], axis=0)
    cT = np.ascontiguousarray(cvec.reshape(3, 16, 128).transpose(2, 1, 0))
    in_maps = []
    for k in range(NCORE):
        ws = ada_w[:, :, k * ADA_COLS:(k + 1) * ADA_COLS].reshape(2, 16, 128, ADA_COLS)
        bs = ada_b[:, k * ADA_COLS:(k + 1) * ADA_COLS].reshape(2, 1, ADA_COLS)
        in_maps.append({"cT": cT, "w": np.ascontiguousarray(ws), "b": np.ascontiguousarray(bs)})
    nc = build_ada()
    res = run_bass_kernel_spmd(nc, in_maps, core_ids=list(range(NCORE)))
    mod = np.concatenate([res.results[k]["out"] for k in range(NCORE)], axis=-1)
    return mod.reshape(2, 3, 6, D)


import os
TQ = os.environ.get('TQ', 'act')

NORM_EPS = 1e-6


def AP3(ap, dims):
    return bass.AP(ap.tensor, ap.offset, [list(ap.ap[0])] + [list(d) for d in dims])


def build_moe(NT, tile_set, D=2048, FF=512, E=32, NG=4, stage='all'):
    P = Prog()
    KC = D // 128
    FFC = FF // 128
    DS = D // 512
    EG = E // NG
    NR = NG + E
    ntile = NT // 128
    ngroup = NT // 512
    nset = max(tile_set) + 1
    x0 = P.dram("x0", [NT, D], F32, kind="ExternalInput")
    z = P.dram("z", [NT, D], F32, kind="ExternalInput")
    wo = P.dram("wo", [D, D], F32, kind="ExternalInput")
    mods = P.dram("mods", [nset, 4, 128, D], F32, kind="ExternalInput")
    gnorm = P.dram("gnorm", [128, D], F32, kind="ExternalInput")
    wr = P.dram("wr", [D, NR], F32, kind="ExternalInput")
    w1 = P.dram("w1", [E, D, FF], F32, kind="ExternalInput")
    w3 = P.dram("w3", [E, D, FF], F32, kind="ExternalInput")
    w2 = P.dram("w2", [E, FF, D], F32, kind="ExternalInput")
    ident_d = P.dram("ident", [128, 128], F32, kind="ExternalInput")
    out = P.dram("out", [NT, D], F32, kind="ExternalOutput")
    x1s = P.dram("x1s", [NT, D], F32)

    WSZ = max(KC * FF, FFC * D)
    wb = [[P.sbuf("w%d_%d" % (m, j), [128, WSZ], BF16) for j in range(2)] for m in range(3)]
    y_acc = P.sbuf("y_acc", [128, 4, D])
    hT_bf = P.sbuf("hT_bf", [128, KC, 512], BF16)
    tmpA = P.sbuf("tmpA", [128, D])
    xt = P.sbuf("xt", [128, D])
    htmp = P.sbuf("htmp", [128, D])
    m_a = P.sbuf("m_a", [128, D])
    m_b = P.sbuf("m_b", [128, D])
    PIECE = min(1024, KC * FF)
    stg = [P.sbuf("stg", [128, PIECE]) for _ in range(2)]
    stgc = [0]

    def load_cast(dst_buf, dst_off, src_ap):
        sb = stg[stgc[0] % 2]
        stgc[0] += 1
        P.dma('sp', sb[:] if len(src_ap.shape) == 2 else sb[:].rearrange("p (a b) -> p a b", a=src_ap.shape[1]),
              src_ap, w=[sb])
        P.op('pool', lambda e: e.tensor_copy(out=dst_buf[:, dst_off:dst_off + PIECE], in_=sb[:]), r=[sb], w=[dst_buf])
    s_sb = [P.sbuf("s_sb", [128, 512]) for _ in range(1)]
    actT = [P.sbuf("actT", [128, FFC, 512], BF16) for _ in range(2)]
    assert FFC * 512 == KC * 128
    zT_bf = actT[0].alias(actT[0][:].rearrange("p a (b c) -> p (a b) c", c=128))
    ident = P.sbuf("ident", [128, 128])
    wr_sb = P.sbuf("wr_sb", [128, KC, NR])
    G = P.sbuf("G", [128, 4, E])
    sm = P.sbuf("sm", [128, 16])
    lg = P.sbuf("lg", [128, NR])
    r1 = P.sbuf("r1", [128, E])
    r2 = P.sbuf("r2", [128, E])
    r3 = P.sbuf("r3", [128, E])
    r4 = P.sbuf("r4", [128, NG])
    TB = [P.psum("TB", [128, 512]) for _ in range(4)]
    YB = [P.psum("YB", [128, 512]) for _ in range(4)]

    epsb = P.sbuf("epsb", [128, 1])
    P.op('dve', lambda e: e.memset(epsb[:], NORM_EPS), w=[epsb])
    P.dma('sp', ident[:], ident_d[:], w=[ident])
    P.dma('sp', wr_sb[:], wr.t.rearrange("(kc p) n -> p kc n", p=128), w=[wr_sb])

    def transpose_to(src, dst_fn, dst_bufs, ncol=128):
        for q in range(KC // 4):
            bank = TB[q % 4]
            for j in range(4):
                kc = q * 4 + j
                P.op('pe', lambda e, bank=bank, j=j, kc=kc: e.transpose(
                    bank[:, j * 128:(j + 1) * 128], src[:, kc * 128:(kc + 1) * 128], ident[:]),
                    r=[src, ident], w=[bank])
            dst_fn(q, bank)

    per = WSZ // D
    wo_loc = []
    flat = [wb[m][j] for m in range(3) for j in range(2)]
    assert per * len(flat) >= KC
    for kc in range(KC):
        b = flat[kc // per]
        off = (kc % per) * D
        wo_loc.append((b, off))
        for pc in range(D // PIECE):
            load_cast(b, off + pc * PIECE, wo[kc * 128:(kc + 1) * 128, pc * PIECE:(pc + 1) * PIECE])
    cur_set = -1
    for t in range(ntile):
        if tile_set[t] != cur_set:
            cur_set = tile_set[t]
            P.dma(TQ, m_a[:], mods[cur_set, 0], w=[m_a])
        P.dma(TQ, tmpA[:], z[t * 128:(t + 1) * 128, :], w=[tmpA])
        P.dma(TQ, xt[:], x0[t * 128:(t + 1) * 128, :], w=[xt])

        def ev(q, bank):
            eng = 'act' if q % 2 == 0 else 'dve'
            if eng == 'act':
                P.op('act', lambda e, q=q, bank=bank: e.copy(
                    out=zT_bf[:, q * 4:(q + 1) * 4, :], in_=bank[:].rearrange("p (a b) -> p a b", a=4)),
                    r=[bank], w=[zT_bf])
            else:
                P.op('dve', lambda e, q=q, bank=bank: e.tensor_copy(
                    out=zT_bf[:, q * 4:(q + 1) * 4, :], in_=bank[:].rearrange("p (a b) -> p a b", a=4)),
                    r=[bank], w=[zT_bf])
        transpose_to(tmpA, ev, [zT_bf])
        for ds in range(DS):
            for kc in range(KC):
                b, off = wo_loc[kc]
                P.op('pe', lambda e, ds=ds, kc=kc, b=b, off=off: e.matmul(
                    YB[ds][:], lhsT=zT_bf[:, kc, :], rhs=b[:, off + ds * 512: off + (ds + 1) * 512],
                    start=(kc == 0), stop=(kc == KC - 1)), r=[zT_bf, b], w=[YB[ds]])
            P.op('dve', lambda e, ds=ds: e.tensor_tensor(
                out=htmp[:, ds * 512:(ds + 1) * 512], in0=YB[ds][:], in1=m_a[:, ds * 512:(ds + 1) * 512],
                op=ALU.mult), r=[YB[ds], m_a], w=[htmp])
        P.op('pool', lambda e: e.tensor_tensor(out=htmp[:], in0=htmp[:], in1=xt[:], op=ALU.add),
             r=[htmp, xt], w=[htmp])
        P.dma(TQ, x1s[t * 128:(t + 1) * 128, :], htmp[:], r=[htmp], w=[x1s])
        if stage == 'A':
            P.dma(TQ, out[t * 128:(t + 1) * 128, :], htmp[:], r=[htmp], w=[out])
    if stage == 'A':
        return P.finish()

    cur_set = -1
    wcount = [0, 0, 0]

    def load_w(m, e_idx):
        j = wcount[m] % 2
        wcount[m] += 1
        b = wb[m][j]
        tot = KC * FF
        for pc in range(tot // PIECE):
            if m < 2:
                nk = PIECE // FF
                src = (w1 if m == 0 else w3).t[e_idx, pc * nk * 128:(pc + 1) * nk * 128, :].rearrange(
                    "(kc p) f -> p kc f", p=128)
            else:
                per_row = D // PIECE
                fc_i, hf = pc // per_row, pc % per_row
                src = w2.t[e_idx, fc_i * 128:(fc_i + 1) * 128, hf * PIECE:(hf + 1) * PIECE]
            load_cast(b, pc * PIECE, src)
        return b

    for g in range(ngroup):
        gset = tile_set[g * 4]
        assert all(tile_set[g * 4 + i] == gset for i in range(4))
        if gset != cur_set:
            cur_set = gset
            P.dma(TQ, m_a[:], mods[cur_set, 1], w=[m_a])
            P.dma(TQ, m_b[:], mods[cur_set, 2], w=[m_b])
            P.dma(TQ, htmp[:], gnorm[:], w=[htmp])
            P.op('dve', lambda e: e.scalar_tensor_tensor(out=m_a[:], in0=m_a[:], scalar=1.0, in1=htmp[:],
                                                          op0=ALU.add, op1=ALU.mult), r=[m_a, htmp], w=[m_a])
        for sub in range(4):
            t = g * 4 + sub
            P.dma(TQ, xt[:], x1s[t * 128:(t + 1) * 128, :], r=[x1s], w=[xt])
            P.op('act', lambda e: e.activation(out=htmp[:], in_=xt[:], func=AF.Square, accum_out=sm[:, 0:1]),
                 r=[xt], w=[htmp, sm])
            P.op('act', lambda e: e.activation(out=sm[:, 1:2], in_=sm[:, 0:1], func=AF.Sqrt, bias=epsb[:, 0:1],
                                               scale=1.0 / D), r=[sm, epsb], w=[sm])
            P.op('dve', lambda e: e.reciprocal(out=sm[:, 2:3], in_=sm[:, 1:2]), r=[sm], w=[sm])
            P.op('dve', lambda e: e.scalar_tensor_tensor(out=htmp[:], in0=xt[:], scalar=sm[:, 2:3], in1=m_a[:],
                                                         op0=ALU.mult, op1=ALU.mult), r=[xt, sm, m_a], w=[htmp])
            P.op('pool', lambda e: e.tensor_tensor(out=htmp[:], in0=htmp[:], in1=m_b[:], op=ALU.add),
                 r=[htmp, m_b], w=[htmp])
            if stage == 'R1':
                P.dma(TQ, out[t * 128:(t + 1) * 128, :], htmp[:], r=[htmp], w=[out])
                continue

            def ev(q, bank, sub=sub):
                P.op('act', lambda e, q=q, bank=bank: e.copy(
                    out=tmpA[:, q * 512:(q + 1) * 512], in_=bank[:]), r=[bank], w=[tmpA])
                P.op('dve', lambda e, q=q, bank=bank, sub=sub: e.tensor_copy(
                    out=hT_bf[:, q * 4:(q + 1) * 4, sub * 128:(sub + 1) * 128],
                    in_=tmpA[:, q * 512:(q + 1) * 512].rearrange("p (a b) -> p a b", a=4)), r=[tmpA], w=[hT_bf])
            transpose_to(htmp, ev, [tmpA, hT_bf])
            for kc in range(KC):
                P.op('pe', lambda e, kc=kc: e.matmul(TB[0][:, 0:NR], lhsT=tmpA[:, kc * 128:(kc + 1) * 128],
                                                     rhs=wr_sb[:, kc, :], start=(kc == 0), stop=(kc == KC - 1)),
                     r=[tmpA, wr_sb], w=[TB[0]])
            P.op('dve', lambda e: e.tensor_copy(out=lg[:], in_=TB[0][:, 0:NR]), r=[TB[0]], w=[lg])
            if stage == 'R2':
                P.op('dve', lambda e: e.memset(htmp[:], 0.0), w=[htmp])
                P.op('dve', lambda e: e.tensor_copy(out=htmp[:, 0:NR], in_=lg[:]), r=[lg], w=[htmp])
                P.dma(TQ, out[t * 128:(t + 1) * 128, :], htmp[:], r=[htmp], w=[out])
                continue
            V = lambda fn, r, w: P.op('dve', fn, r=r, w=w)
            V(lambda e: e.tensor_reduce(out=sm[:, 3:4], in_=lg[:, 0:NG], axis=AX.X, op=ALU.max), [lg], [sm])
            V(lambda e: e.tensor_scalar(out=sm[:, 4:5], in0=sm[:, 3:4], scalar1=-1.0, scalar2=None, op0=ALU.mult),
              [sm], [sm])
            P.op('act', lambda e: e.activation(out=r4[:], in_=lg[:, 0:NG], func=AF.Exp, bias=sm[:, 4:5], scale=1.0,
                                               accum_out=sm[:, 5:6]), r=[lg, sm], w=[r4, sm])
            V(lambda e: e.reciprocal(out=sm[:, 6:7], in_=sm[:, 5:6]), [sm], [sm])
            V(lambda e: e.tensor_scalar(out=r4[:], in0=lg[:, 0:NG], scalar1=sm[:, 3:4], scalar2=None,
                                        op0=ALU.is_equal), [lg, sm], [r4])
            V(lambda e: e.tensor_scalar(out=r4[:], in0=r4[:], scalar1=1.0, scalar2=1e30, op0=ALU.subtract,
                                        op1=ALU.mult), [r4], [r4])
            V(lambda e: e.tensor_tensor(out=r1[:].rearrange("p (g k) -> p g k", g=NG),
                                        in0=lg[:, NG:NR].rearrange("p (g k) -> p g k", g=NG),
                                        in1=AP3(r4[:], [[1, NG], [0, EG]]), op=ALU.add), [lg, r4], [r1])
            V(lambda e: e.tensor_reduce(out=sm[:, 7:8], in_=r1[:], axis=AX.X, op=ALU.max), [r1], [sm])
            V(lambda e: e.tensor_scalar(out=r2[:], in0=r1[:], scalar1=sm[:, 7:8], scalar2=None, op0=ALU.is_equal),
              [r1, sm], [r2])
            V(lambda e: e.scalar_tensor_tensor(out=r1[:], in0=r2[:], scalar=-1e30, in1=r1[:], op0=ALU.mult,
                                               op1=ALU.add), [r1, r2], [r1])
            V(lambda e: e.tensor_reduce(out=sm[:, 8:9], in_=r1[:], axis=AX.X, op=ALU.max), [r1], [sm])
            V(lambda e: e.tensor_scalar(out=r3[:], in0=r1[:], scalar1=sm[:, 8:9], scalar2=None, op0=ALU.is_equal),
              [r1, sm], [r3])
            V(lambda e: e.tensor_tensor(out=sm[:, 9:10], in0=sm[:, 8:9], in1=sm[:, 7:8], op=ALU.subtract),
              [sm], [sm])
            P.op('act', lambda e: e.activation(out=sm[:, 10:11], in_=sm[:, 9:10], func=AF.Exp), r=[sm], w=[sm])
            V(lambda e: e.tensor_scalar(out=sm[:, 11:12], in0=sm[:, 10:11], scalar1=1.0, scalar2=None, op0=ALU.add),
              [sm], [sm])
            V(lambda e: e.reciprocal(out=sm[:, 11:12], in_=sm[:, 11:12]), [sm], [sm])
            V(lambda e: e.tensor_tensor(out=sm[:, 12:13], in0=sm[:, 10:11], in1=sm[:, 11:12], op=ALU.mult),
              [sm], [sm])
            V(lambda e: e.tensor_scalar(out=sm[:, 11:13], in0=sm[:, 11:13], scalar1=sm[:, 6:7], scalar2=None,
                                        op0=ALU.mult), [sm], [sm])
            V(lambda e: e.tensor_scalar(out=r2[:], in0=r2[:], scalar1=sm[:, 11:12], scalar2=None, op0=ALU.mult),
              [r2, sm], [r2])
            V(lambda e, sub=sub: e.scalar_tensor_tensor(out=G[:, sub, :], in0=r3[:], scalar=sm[:, 12:13], in1=r2[:],
                                                        op0=ALU.mult, op1=ALU.add), [r3, r2, sm], [G])
        if stage in ('R1', 'R2'):
            continue
        if stage == 'R':
            for sub in range(4):
                t = g * 4 + sub
                P.op('dve', lambda e, sub=sub: e.memset(htmp[:], 0.0), w=[htmp])
                P.op('dve', lambda e, sub=sub: e.tensor_copy(out=htmp[:, 0:E], in_=G[:, sub, :]), r=[G], w=[htmp])
                P.dma(TQ, out[t * 128:(t + 1) * 128, :], htmp[:], r=[htmp], w=[out])
            continue
        for ex in range(E):
            w1b = load_w(0, ex)
            w3b = load_w(1, ex)
            w2b = load_w(2, ex)
            aT = actT[ex % 2]
            for fc in range(FFC):
                hp1 = TB[(fc % 2) * 2]
                hp3 = TB[(fc % 2) * 2 + 1]
                for (hp, wbuf) in ((hp1, w1b), (hp3, w3b)):
                    for kc in range(KC):
                        P.op('pe', lambda e, hp=hp, wbuf=wbuf, kc=kc, fc=fc: e.matmul(
                            hp[:], lhsT=wbuf[:, kc * FF + fc * 128: kc * FF + (fc + 1) * 128], rhs=hT_bf[:, kc, :],
                            start=(kc == 0), stop=(kc == KC - 1)), r=[wbuf, hT_bf], w=[hp])
                sb = s_sb[0]
                P.op('act', lambda e, hp1=hp1, sb=sb: e.activation(out=sb[:], in_=hp1[:], func=AF.Silu),
                     r=[hp1], w=[sb])
                P.op('dve', lambda e, hp3=hp3, sb=sb, aT=aT, fc=fc: e.tensor_tensor(
                    out=aT[:, fc, :], in0=hp3[:], in1=sb[:], op=ALU.mult), r=[hp3, sb], w=[aT])
            for sub in range(4):
                for ds in range(DS):
                    for fc in range(FFC):
                        P.op('pe', lambda e, sub=sub, ds=ds, fc=fc, aT=aT, w2b=w2b: e.matmul(
                            YB[ds][:], lhsT=aT[:, fc, sub * 128:(sub + 1) * 128],
                            rhs=w2b[:, fc * D + ds * 512: fc * D + (ds + 1) * 512],
                            start=(fc == 0), stop=(fc == FFC - 1)), r=[aT, w2b], w=[YB[ds]])
                    if ex == 0:
                        P.op('dve', lambda e, sub=sub, ds=ds, ex=ex: e.tensor_scalar(
                            out=y_acc[:, sub, ds * 512:(ds + 1) * 512], in0=YB[ds][:], scalar1=G[:, sub, ex:ex + 1],
                            scalar2=None, op0=ALU.mult), r=[YB[ds], G], w=[y_acc])
                    else:
                        P.op('dve', lambda e, sub=sub, ds=ds, ex=ex: e.scalar_tensor_tensor(
                            out=y_acc[:, sub, ds * 512:(ds + 1) * 512], in0=YB[ds][:], scalar=G[:, sub, ex:ex + 1],
                            in1=y_acc[:, sub, ds * 512:(ds + 1) * 512], op0=ALU.mult, op1=ALU.add),
                            r=[YB[ds], G, y_acc], w=[y_acc])
        P.dma(TQ, tmpA[:], mods[cur_set, 3], w=[tmpA])
        m_c = tmpA
        for sub in range(4):
            t = g * 4 + sub
            P.dma(TQ, xt[:], x1s[t * 128:(t + 1) * 128, :], r=[x1s], w=[xt])
            P.op('pool', lambda e, sub=sub: e.tensor_tensor(out=htmp[:], in0=y_acc[:, sub, :], in1=m_c[:], op=ALU.mult),
                 r=[y_acc, m_c], w=[htmp])
            P.op('pool', lambda e: e.tensor_tensor(out=htmp[:], in0=htmp[:], in1=xt[:], op=ALU.add),
                 r=[htmp, xt], w=[htmp])
            P.dma(TQ, out[t * 128:(t + 1) * 128, :], htmp[:], r=[htmp], w=[out])
    return P.finish()


def bc128(v):
    return np.ascontiguousarray(np.broadcast_to(np.asarray(v, np.float32)[None, :], (128, v.shape[-1])))


import os

NORM_EPS = 1e-6
HD = 128
GRID_W = 64
NA_ROWS, NA_COLS = 8, 16
A_WINDOW = 128


def AP3(ap, dims):
    return bass.AP(ap.tensor, ap.offset, [list(ap.ap[0])] + [list(d) for d in dims])


def build_att(NQT=32, D=2048, stage='all'):
    P = Prog()
    KC = D // 128
    NKT = NQT + 4
    NT = NKT + 2
    NTOK = NT * 128
    NQKV = 4608
    scale = HD ** -0.5
    xh = P.dram("xh", [NT * 128, D], F32, kind="ExternalInput")
    mods = P.dram("mods", [2, 2, 128, D], F32, kind="ExternalInput")
    gmix = P.dram("gmix", [128, D], F32, kind="ExternalInput")
    w_in = P.dram("w_in", [D, NQKV], F32, kind="ExternalInput")
    gains = P.dram("gains", [128, 4], F32, kind="ExternalInput")
    ropeT = P.dram("ropeT", [2, 128, NTOK], F32, kind="ExternalInput")
    perm_d = P.dram("perm", [128, 128], F32, kind="ExternalInput")
    ident_d = P.dram("ident", [128, 128], F32, kind="ExternalInput")
    sinkbc = P.dram("sinkbc", [128, 8], F32, kind="ExternalInput")
    biasT = P.dram("biasT", [5, 128, 6, 8, 128], F32, kind="ExternalInput")
    maskA = P.dram("maskA", [3, 128, 3, 128], F32, kind="ExternalInput")
    zout = P.dram("zout", [(NQT + 2) * 128, D], F32, kind="ExternalOutput")
    QT_s = P.dram("QT_s", [16, 128, NTOK], BF16)
    KT_s = P.dram("KT_s", [10, 128, NTOK], BF16)
    V_s = P.dram("V_s", [NTOK, 1280], BF16)

    w_blk = [P.sbuf("w_blk", [128, KC * 512], BF16) for _ in range(2)]
    stg = [P.sbuf("stg", [128, 1024]) for _ in range(2)]
    hT_bf = P.sbuf("hT_bf", [128, KC * 512], BF16)
    xt = P.sbuf("xt", [128, D])
    htmp = P.sbuf("htmp", [128, D])
    m_a = P.sbuf("m_a", [128, D])
    m_b = P.sbuf("m_b", [128, D])
    ident = P.sbuf("ident", [128, 128])
    perm = P.sbuf("perm", [128, 128])
    ones_f = P.sbuf("ones_f", [128, 128])
    ones_b = P.sbuf("ones_b", [128, 128], BF16)
    zeros_b = P.sbuf("zeros_b", [128, 128], BF16)
    ones512 = P.sbuf("ones512", [128, 512], BF16)
    gsb = P.sbuf("gsb", [128, 4])
    epsb = P.sbuf("epsb", [128, 1])
    sm = P.sbuf("sm", [128, 8])
    qk = P.sbuf("qk", [128, 512])
    sq = P.sbuf("sq", [128, 512])
    rstd = P.sbuf("rstd", [128, 512])
    qn = P.sbuf("qn", [128, 512])
    t1 = P.sbuf("t1", [128, 512])
    ropeC = P.sbuf("ropeC", [128, 512])
    ropeS = P.sbuf("ropeS", [128, 512])
    ob = [P.sbuf("ob", [128, 512], BF16) for _ in range(2)]
    vob = [P.sbuf("vob", [128, 1280], BF16) for _ in range(2)]
    QTt = [P.sbuf("QTt", [128, 2048], BF16) for _ in range(2)]
    KTw = [w_blk[i].alias(w_blk[i][:, 0:10 * 768].rearrange("p (h t) -> p h t", h=10)) for i in range(2)]
    Vw0 = hT_bf.alias(hT_bf[:, 0:6 * 1280].rearrange("p (c f) -> p c f", c=6))
    Vw1 = P.sbuf("Vw1", [128, 6, 1280], BF16)
    Vw = [Vw0, Vw1]
    KTc = P.sbuf("KTc", [128, 10, 256], BF16)
    Vc = P.sbuf("Vc", [128, 2, 1280], BF16)
    bias_std = P.sbuf("bias_std", [128, 5, 8, 128])
    bias_sp = P.sbuf("bias_sp", [128, 6, 8, 128])
    mA = P.sbuf("mA", [128, 3, 128], BF16)
    mA_f = P.sbuf("mA_f", [128, 3, 128])
    mA_cur = P.sbuf("mA_cur", [128, 3, 128], BF16)
    PT = [P.sbuf("PT", [128, 512], BF16) for _ in range(3)]
    stmp = [P.sbuf("stmp", [128, 512]) for _ in range(2)]
    sinkA = P.sbuf("sinkA", [128, 2, 512])
    sk = P.sbuf("sk", [128, 8])
    den = P.sbuf("den", [128, 512])
    zT = xt.alias(xt[:])
    zo = htmp.alias(htmp[:])
    PB = [P.psum("PB", [128, 512]) for _ in range(8)]

    P.dma('sp', ident[:], ident_d[:], w=[ident])
    P.dma('sp', perm[:], perm_d[:], w=[perm])
    P.dma('sp', gsb[:], gains[:], w=[gsb])
    P.dma('sp', sk[:], sinkbc[:], w=[sk])
    P.op('dve', lambda e: e.memset(ones_f[:], 1.0), w=[ones_f])
    P.op('dve', lambda e: e.memset(ones_b[:], 1.0), w=[ones_b])
    P.op('dve', lambda e: e.memset(zeros_b[:], 0.0), w=[zeros_b])
    P.op('dve', lambda e: e.memset(ones512[:], 1.0), w=[ones512])
    P.op('dve', lambda e: e.memset(epsb[:], NORM_EPS), w=[epsb])
    P.op('act', lambda e: e.activation(out=sk[:], in_=sk[:], func=AF.Exp), r=[sk], w=[sk])
    for g in range(2):
        for hh in range(4):
            P.op('dve', lambda e, g=g, hh=hh: e.tensor_scalar(
                out=sinkA[:, g, hh * 128:(hh + 1) * 128], in0=ones_f[:], scalar1=sk[:, g * 4 + hh: g * 4 + hh + 1],
                scalar2=None, op0=ALU.mult), r=[ones_f, sk], w=[sinkA])

    stgc = [0]

    def load_cast(dst_buf, dst_off, src_ap, n):
        sb = stg[stgc[0] % 2]
        stgc[0] += 1
        if len(src_ap.shape) == 3:
            P.dma('sp', sb[:, 0:n].rearrange("p (a b) -> p a b", a=src_ap.shape[1]), src_ap, w=[sb])
        else:
            P.dma('sp', sb[:, 0:n], src_ap, w=[sb])
        P.op('pool', lambda e: e.tensor_copy(out=dst_buf[:, dst_off:dst_off + n], in_=sb[:, 0:n]), r=[sb], w=[dst_buf])

    groups = [(g * 4, 4) for g in range(NKT // 4)] + [(NKT, 2)]
    cur_set = -1
    wcnt = [0]
    obc = [0]
    for (tile0, ntl) in groups:
        gN = ntl * 128
        tok0 = tile0 * 128
        gset = 0 if tile0 < NKT else 1
        if gset != cur_set:
            cur_set = gset
            P.dma('act', m_a[:], mods[gset, 0], w=[m_a])
            P.dma('act', m_b[:], mods[gset, 1], w=[m_b])
            P.dma('act', htmp[:], gmix[:], w=[htmp])
            P.op('dve', lambda e: e.scalar_tensor_tensor(out=m_a[:], in0=m_a[:], scalar=1.0, in1=htmp[:],
                                                         op0=ALU.add, op1=ALU.mult), r=[m_a, htmp], w=[m_a])
        P.dma('act', ropeC[:, 0:gN], ropeT[0, :, tok0:tok0 + gN], w=[ropeC])
        P.dma('act', ropeS[:, 0:gN], ropeT[1, :, tok0:tok0 + gN], w=[ropeS])
        for sub in range(ntl):
            t = tile0 + sub
            P.dma('act', xt[:], xh[t * 128:(t + 1) * 128, :], w=[xt])
            P.op('act', lambda e: e.activation(out=htmp[:], in_=xt[:], func=AF.Square, accum_out=sm[:, 0:1]),
                 r=[xt], w=[htmp, sm])
            P.op('act', lambda e: e.activation(out=sm[:, 1:2], in_=sm[:, 0:1], func=AF.Sqrt, bias=epsb[:, 0:1],
                                               scale=1.0 / D), r=[sm, epsb], w=[sm])
            P.op('dve', lambda e: e.reciprocal(out=sm[:, 2:3], in_=sm[:, 1:2]), r=[sm], w=[sm])
            P.op('dve', lambda e: e.scalar_tensor_tensor(out=htmp[:], in0=xt[:], scalar=sm[:, 2:3], in1=m_a[:],
                                                         op0=ALU.mult, op1=ALU.mult), r=[xt, sm, m_a], w=[htmp])
            P.op('pool', lambda e: e.tensor_tensor(out=htmp[:], in0=htmp[:], in1=m_b[:], op=ALU.add),
                 r=[htmp, m_b], w=[htmp])
            for q in range(KC // 4):
                bank = PB[q % 4]
                for j in range(4):
                    kc = q * 4 + j
                    P.op('pe', lambda e, bank=bank, j=j, kc=kc: e.transpose(
                        bank[:, j * 128:(j + 1) * 128], htmp[:, kc * 128:(kc + 1) * 128], ident[:]),
                        r=[htmp, ident], w=[bank])
                eng = 'act' if q % 2 == 0 else 'dve'
                dst = hT_bf[:, q * 4 * 512:(q + 1) * 4 * 512].rearrange("p (a b) -> p a b", a=4)[:, :, sub * 128:(sub + 1) * 128]
                src = bank[:].rearrange("p (a b) -> p a b", a=4)
                if eng == 'act':
                    P.op('act', lambda e, dst=dst, src=src: e.copy(out=dst, in_=src), r=[bank], w=[hT_bf])
                else:
                    P.op('dve', lambda e, dst=dst, src=src: e.tensor_copy(out=dst, in_=src), r=[bank], w=[hT_bf])
        for blk in range(9):
            wb_ = w_blk[wcnt[0] % 2]
            wcnt[0] += 1
            for pc in range(KC // 2):
                load_cast(wb_, pc * 1024, w_in.t[pc * 256:(pc + 1) * 256, blk * 512:(blk + 1) * 512].rearrange(
                    "(kc p) f -> p kc f", p=128), 1024)
            for cc in range(4):
                col = blk * 512 + cc * 128
                if col < 1024:
                    kind, hidx, gi, rope = 'q', col // 128, 0, True
                elif col < 1280:
                    kind, hidx, gi, rope = 'k', (col - 1024) // 128, 1, True
                elif col < 1536:
                    kind = 'v'
                elif col < 2560:
                    kind, hidx, gi, rope = 'q', 8 + (col - 1536) // 128, 2, False
                elif col < 3584:
                    kind, hidx, gi, rope = 'k', 2 + (col - 2560) // 128, 3, False
                else:
                    kind = 'v'
                if kind == 'v':
                    continue
                pb = PB[4 + (cc % 2)]
                for kc in range(KC):
                    P.op('pe', lambda e, pb=pb, wb_=wb_, kc=kc, cc=cc, gN=gN: e.matmul(
                        pb[:, 0:gN], lhsT=wb_[:, kc * 512 + cc * 128: kc * 512 + (cc + 1) * 128],
                        rhs=hT_bf[:, kc * 512: kc * 512 + gN], start=(kc == 0), stop=(kc == KC - 1)),
                        r=[wb_, hT_bf], w=[pb])
                P.op('act', lambda e, pb=pb, gN=gN: e.copy(out=qk[:, 0:gN], in_=pb[:, 0:gN]), r=[pb], w=[qk])
                P.op('pool', lambda e, gN=gN: e.tensor_tensor(out=sq[:, 0:gN], in0=qk[:, 0:gN], in1=qk[:, 0:gN],
                                                              op=ALU.mult), r=[qk], w=[sq])
                P.op('pe', lambda e, gN=gN: e.matmul(PB[6][:, 0:gN], lhsT=ones_f[:], rhs=sq[:, 0:gN], start=True,
                                                     stop=True), r=[ones_f, sq], w=[PB[6]])
                P.op('act', lambda e, gN=gN: e.activation(out=rstd[:, 0:gN], in_=PB[6][:, 0:gN], func=AF.Sqrt,
                                                          bias=epsb[:, 0:1], scale=1.0 / HD), r=[PB[6], epsb], w=[rstd])
                P.op('dve', lambda e, gN=gN: e.reciprocal(out=rstd[:, 0:gN], in_=rstd[:, 0:gN]), r=[rstd], w=[rstd])
                P.op('dve', lambda e, gN=gN, gi=gi: e.scalar_tensor_tensor(
                    out=qn[:, 0:gN], in0=qk[:, 0:gN], scalar=gsb[:, gi:gi + 1], in1=rstd[:, 0:gN], op0=ALU.mult,
                    op1=ALU.mult), r=[qk, gsb, rstd], w=[qn])
                o_ = ob[obc[0] % 2]
                obc[0] += 1
                if rope:
                    P.op('pe', lambda e, gN=gN: e.matmul(PB[7][:, 0:gN], lhsT=perm[:], rhs=qn[:, 0:gN], start=True,
                                                         stop=True), r=[perm, qn], w=[PB[7]])
                    P.op('dve', lambda e, gN=gN: e.tensor_tensor(out=t1[:, 0:gN], in0=PB[7][:, 0:gN], in1=ropeS[:, 0:gN],
                                                                 op=ALU.mult), r=[PB[7], ropeS], w=[t1])
                    P.op('pool', lambda e, gN=gN: e.tensor_tensor(out=qn[:, 0:gN], in0=qn[:, 0:gN], in1=ropeC[:, 0:gN],
                                                                  op=ALU.mult), r=[qn, ropeC], w=[qn])
                    P.op('pool', lambda e, gN=gN, o_=o_: e.tensor_tensor(out=o_[:, 0:gN], in0=qn[:, 0:gN], in1=t1[:, 0:gN],
                                                                         op=ALU.add), r=[qn, t1], w=[o_])
                else:
                    P.op('pool', lambda e, gN=gN, o_=o_: e.tensor_copy(out=o_[:, 0:gN], in_=qn[:, 0:gN]), r=[qn], w=[o_])
                dst_s = QT_s if kind == 'q' else KT_s
                P.dma('act', dst_s[hidx, :, tok0:tok0 + gN], o_[:, 0:gN], r=[o_], w=[dst_s])
            vr = None
            if blk == 2:
                vr = (256, 512, 0)
            elif blk in (7, 8):
                vr = (0, 512, 256 + (blk - 7) * 512)
            if vr is not None:
                c0, c1, vcol = vr
                for sub in range(ntl):
                    pb = PB[4 + (sub % 2)]
                    for kc in range(KC):
                        P.op('pe', lambda e, pb=pb, wb_=wb_, kc=kc, sub=sub, c0=c0, c1=c1: e.matmul(
                            pb[:, 0:c1 - c0], lhsT=hT_bf[:, kc * 512 + sub * 128: kc * 512 + (sub + 1) * 128],
                            rhs=wb_[:, kc * 512 + c0: kc * 512 + c1], start=(kc == 0), stop=(kc == KC - 1)),
                            r=[wb_, hT_bf], w=[pb])
                    vb_ = vob[sub % 2]
                    P.op('act', lambda e, pb=pb, vb_=vb_, c0=c0, c1=c1: e.copy(out=vb_[:, 0:c1 - c0], in_=pb[:, 0:c1 - c0]),
                         r=[pb], w=[vb_])
                    t = tile0 + sub
                    P.dma('act', V_s[t * 128:(t + 1) * 128, vcol:vcol + (c1 - c0)], vb_[:, 0:c1 - c0], r=[vb_], w=[V_s])


    P.dma('sp', KTc[:], KT_s.t[:, :, NKT * 128:NTOK].rearrange("h d t -> d h t"), r=[KT_s], w=[KTc])
    P.dma('sp', Vc[:], V_s.t[NKT * 128:NTOK, :].rearrange("(c p) f -> p c f", p=128), r=[V_s], w=[Vc])
    P.dma('sp', bias_std[:], biasT[0, :, 0:5], w=[bias_std])
    P.dma('sp', mA_f[:], maskA[0], w=[mA_f])
    P.op('dve', lambda e: e.tensor_copy(out=mA[:], in_=mA_f[:]), r=[mA_f], w=[mA])
    ptc = [0]

    def next_pt():
        p_ = PT[ptc[0] % 3]
        ptc[0] += 1
        return p_

    for qi in range(NQT + 2):
        is_ctx = qi >= NQT
        j = qi + 2 if not is_ctx else NKT + (qi - NQT)
        bi = qi % 2
        P.dma('sp', QTt[bi][:].rearrange("p (h t) -> p h t", h=16), QT_s.t[:, :, j * 128:(j + 1) * 128].rearrange("h d t -> d h t"), r=[QT_s], w=[QTt[bi]])
        if not is_ctx:
            if qi == 0:
                b0, nch = j - 2, 6
            elif qi == NQT - 1:
                b0, nch = j - 3, 6
            else:
                b0, nch = j - 2, 5
            a0 = j - 1
            ktw, vw = KTw[bi], Vw[bi]
            P.dma('sp', ktw[:, 0:2, 0:384], KT_s.t[0:2, :, a0 * 128:(a0 + 3) * 128].rearrange("h d t -> d h t"),
                  r=[KT_s], w=[ktw])
            P.dma('sp', ktw[:, 2:10, 0:nch * 128], KT_s.t[2:10, :, b0 * 128:(b0 + nch) * 128].rearrange("h d t -> d h t"),
                  r=[KT_s], w=[ktw])
            P.dma('sp', vw[:, 0:3, 0:256], V_s.t[a0 * 128:(a0 + 3) * 128, 0:256].rearrange("(c p) f -> p c f", p=128),
                  r=[V_s], w=[vw])
            P.dma('sp', vw[:, 0:nch, 256:1280], V_s.t[b0 * 128:(b0 + nch) * 128, 256:1280].rearrange("(c p) f -> p c f", p=128),
                  r=[V_s], w=[vw])
            spi = {0: 1, 1: 2, NQT - 2: 3, NQT - 1: 4}.get(qi)
            if spi is not None:
                P.dma('sp', bias_sp[:], biasT[spi], w=[bias_sp])
                btab = bias_sp
            else:
                btab = bias_std
            if qi in (0, NQT - 1):
                P.dma('sp', mA_f[:], maskA[1 if qi == 0 else 2], w=[mA_f])
                P.op('dve', lambda e: e.tensor_copy(out=mA_cur[:], in_=mA_f[:]), r=[mA_f], w=[mA_cur])
                mtab = mA_cur
            else:
                mtab = mA
        qt = QTt[bi]
        for g in range(2):
            OB, DB = PB[4 + g], PB[6 + g]
            chunks = ([('w', c) for c in range(3)] if not is_ctx else []) + [('c', 0), ('c', 1)]
            for ci, (kind, c) in enumerate(chunks):
                SB = PB[ci % 2]
                if kind == 'w':
                    lhs = ktw[:, g, c * 128:(c + 1) * 128]
                    kbuf = ktw
                else:
                    lhs = KTc[:, g, c * 128:(c + 1) * 128]
                    kbuf = KTc
                P.op('pe', lambda e, SB=SB, lhs=lhs, qt=qt, g=g: e.matmul(
                    SB[:], lhsT=lhs, rhs=qt[:, g * 512:(g + 1) * 512], start=True, stop=True), r=[kbuf, qt], w=[SB])
                pt = next_pt()
                P.op('act', lambda e, SB=SB, pt=pt: e.activation(out=pt[:], in_=SB[:], func=AF.Exp, scale=scale),
                     r=[SB], w=[pt])
                if kind == 'w':
                    P.op('pool', lambda e, pt=pt, mtab=mtab, c=c: e.tensor_tensor(
                        out=pt[:].rearrange("p (h q) -> p h q", h=4), in0=pt[:].rearrange("p (h q) -> p h q", h=4),
                        in1=AP3(mtab[:, c, :], [[0, 4], [1, 128]]), op=ALU.mult), r=[pt, mtab], w=[pt])
                    vl = vw[:, c, g * 128:(g + 1) * 128]
                    vbuf = vw
                else:
                    vl = Vc[:, c, g * 128:(g + 1) * 128]
                    vbuf = Vc
                first, last = (ci == 0), (ci == len(chunks) - 1)
                P.op('pe', lambda e, OB=OB, vl=vl, pt=pt, first=first, last=last: e.matmul(
                    OB[:], lhsT=vl, rhs=pt[:], start=first, stop=last), r=[vbuf, pt], w=[OB])
                P.op('pe', lambda e, DB=DB, pt=pt, first=first, last=last: e.matmul(
                    DB[:], lhsT=ones_b[:], rhs=pt[:], start=first, stop=last), r=[ones_b, pt], w=[DB])
            P.op('dve', lambda e, DB=DB, g=g: e.tensor_tensor(out=den[:], in0=DB[:], in1=sinkA[:, g, :], op=ALU.add),
                 r=[DB, sinkA], w=[den])
            P.op('dve', lambda e: e.reciprocal(out=den[:], in_=den[:]), r=[den], w=[den])
            P.op('dve', lambda e, OB=OB, g=g: e.tensor_tensor(out=zT[:, g * 512:(g + 1) * 512], in0=OB[:], in1=den[:],
                                                              op=ALU.mult), r=[OB, den], w=[zT])
        for hf in range(2):
            OB, DB = PB[4 + hf], PB[6 + hf]
            chunks = ([('w', c) for c in range(nch)] if not is_ctx else []) + [('c', 0), ('c', 1)]
            P.op('pe', lambda e, OB=OB: e.matmul(OB[:], lhsT=zeros_b[:], rhs=ones512[:], start=True, stop=False),
                 r=[zeros_b, ones512], w=[OB])
            for ci, (kind, c) in enumerate(chunks):
                SB = PB[ci % 2]
                for hh in range(4):
                    h = hf * 4 + hh
                    if kind == 'w':
                        lhs, kbuf = ktw[:, 2 + h, c * 128:(c + 1) * 128], ktw
                    else:
                        lhs, kbuf = KTc[:, 2 + h, c * 128:(c + 1) * 128], KTc
                    P.op('pe', lambda e, SB=SB, lhs=lhs, qt=qt, h=h, hh=hh: e.matmul(
                        SB[:, hh * 128:(hh + 1) * 128], lhsT=lhs, rhs=qt[:, (8 + h) * 128:(9 + h) * 128], start=True, stop=True),
                        r=[kbuf, qt], w=[SB])
                pt = next_pt()
                if kind == 'w':
                    st_ = stmp[ci % 2]
                    P.op('dve', lambda e, SB=SB, st_=st_, btab=btab, c=c, hf=hf: e.scalar_tensor_tensor(
                        out=st_[:].rearrange("p (h q) -> p h q", h=4), in0=SB[:].rearrange("p (h q) -> p h q", h=4),
                        scalar=scale, in1=btab[:, c, hf * 4:(hf + 1) * 4, :], op0=ALU.mult, op1=ALU.add),
                        r=[SB, btab], w=[st_])
                    P.op('act', lambda e, st_=st_, pt=pt: e.activation(out=pt[:], in_=st_[:], func=AF.Exp),
                         r=[st_], w=[pt])
                else:
                    P.op('act', lambda e, SB=SB, pt=pt: e.activation(out=pt[:], in_=SB[:], func=AF.Exp, scale=scale),
                         r=[SB], w=[pt])
                first, last = (ci == 0), (ci == len(chunks) - 1)
                for hh in range(4):
                    h = hf * 4 + hh
                    if kind == 'w':
                        vl, vbuf = vw[:, c, 256 + h * 128: 256 + (h + 1) * 128], vw
                    else:
                        vl, vbuf = Vc[:, c, 256 + h * 128: 256 + (h + 1) * 128], Vc
                    P.op('pe', lambda e, OB=OB, vl=vl, pt=pt, hh=hh, first=first, last=last: e.matmul(
                        OB[:, hh * 128:(hh + 1) * 128], lhsT=vl, rhs=pt[:, hh * 128:(hh + 1) * 128], start=False,
                        stop=last), r=[vbuf, pt], w=[OB])
                P.op('pe', lambda e, DB=DB, pt=pt, first=first, last=last: e.matmul(
                    DB[:], lhsT=ones_b[:], rhs=pt[:], start=first, stop=last), r=[ones_b, pt], w=[DB])
            P.op('dve', lambda e, DB=DB: e.reciprocal(out=den[:], in_=DB[:]), r=[DB], w=[den])
            P.op('dve', lambda e, OB=OB, hf=hf: e.tensor_tensor(out=zT[:, 1024 + hf * 512: 1024 + (hf + 1) * 512],
                                                                in0=OB[:], in1=den[:], op=ALU.mult), r=[OB, den], w=[zT])
        for q4 in range(4):
            bank = PB[q4 % 4]
            for jj in range(4):
                hd = q4 * 4 + jj
                P.op('pe', lambda e, bank=bank, jj=jj, hd=hd: e.transpose(
                    bank[:, jj * 128:(jj + 1) * 128], zT[:, hd * 128:(hd + 1) * 128], ident[:]),
                    r=[zT, ident], w=[bank])
            if q4 % 2 == 0:
                P.op('act', lambda e, bank=bank, q4=q4: e.copy(out=zo[:, q4 * 512:(q4 + 1) * 512], in_=bank[:]),
                     r=[bank], w=[zo])
            else:
                P.op('dve', lambda e, bank=bank, q4=q4: e.tensor_copy(out=zo[:, q4 * 512:(q4 + 1) * 512], in_=bank[:]),
                     r=[bank], w=[zo])
        P.dma('act', zout[qi * 128:(qi + 1) * 128, :], zo[:], r=[zo], w=[zout])
    return P.finish()


def att_tables(rpb, L, q0_list_b, q0_first, q0_last):
    rows = L // GRID_W

    def btab(q0, k0s):
        q_tok = q0 + np.arange(128)
        rq, cq = q_tok // GRID_W, q_tok % GRID_W
        r0 = np.clip(rq - NA_ROWS // 2, 0, rows - NA_ROWS)
        c0 = np.clip(cq - NA_COLS // 2, 0, GRID_W - NA_COLS)
        out = np.full((128, 6, 8, 128), -1e30, np.float32)
        for c, k0 in enumerate(k0s):
            k_tok = k0 + np.arange(128)
            valid = (k_tok >= 0) & (k_tok < L)
            rk, ck = np.floor_divide(k_tok, GRID_W), np.mod(k_tok, GRID_W)
            ok = (valid[:, None] & (rk[:, None] >= r0[None]) & (rk[:, None] < r0[None] + NA_ROWS)
                  & (ck[:, None] >= c0[None]) & (ck[:, None] < c0[None] + NA_COLS))
            dr = np.clip(rk[:, None] - rq[None] + NA_ROWS - 1, 0, 2 * NA_ROWS - 2)
            dc = np.clip(ck[:, None] - cq[None] + NA_COLS - 1, 0, 2 * NA_COLS - 2)
            b = rpb[:, dr, dc]
            b = np.where(ok[None], b, np.float32(-1e30))
            out[:, c] = b.transpose(1, 0, 2)
        return out

    def mtab(q0):
        q_tok = q0 + np.arange(128)
        out = np.zeros((128, 3, 128), np.float32)
        for c in range(3):
            k_tok = q0 + (c - 1) * 128 + np.arange(128)
            ok = (np.abs(q_tok[None] - k_tok[:, None]) <= A_WINDOW) & (k_tok[:, None] >= 0) & (k_tok[:, None] < L)
            out[:, c] = ok.astype(np.float32)
        return out
    std_q0 = (rows // 2) * GRID_W
    tabs = [btab(std_q0, [std_q0 + (c - 2) * 128 for c in range(5)])]
    q0, q1, q30, q31 = q0_list_b
    tabs.append(btab(q0, [q0 + (c - 2) * 128 for c in range(6)]))
    tabs.append(btab(q1, [q1 + (c - 2) * 128 for c in range(5)]))
    tabs.append(btab(q30, [q30 + (c - 2) * 128 for c in range(5)]))
    tabs.append(btab(q31, [q31 + (c - 3) * 128 for c in range(6)]))
    biasT = np.stack(tabs)
    maskA = np.stack([mtab(std_q0), mtab(q0_first), mtab(q0_last)])
    return biasT, maskA


def rope_tables(tok_global, is_ctx):
    n = len(tok_global)
    quarter = 32
    inv_freq = (10000.0 ** (-np.arange(quarter, dtype=np.float32) / quarter)).astype(np.float32)
    row = (tok_global // GRID_W).astype(np.float32)
    col = (tok_global % GRID_W).astype(np.float32)
    C = np.ones((128, n), np.float32)
    S = np.zeros((128, n), np.float32)
    for d in range(128):
        half, idx = d // 64, d % 64
        pos = row if half == 0 else col
        ang = (pos * inv_freq[idx % quarter]).astype(np.float32)
        C[d] = np.cos(ang)
        S[d] = -np.sin(ang) if idx < quarter else np.sin(ang)
    C[:, is_ctx] = 1.0
    S[:, is_ctx] = 0.0
    perm = np.zeros((128, 128), np.float32)
    for d in range(128):
        idx = d % 64
        partner = d + 32 if idx < 32 else d - 32
        perm[partner, d] = 1.0
    return np.stack([C, S]), perm


def bc128(v):
    return np.ascontiguousarray(np.broadcast_to(np.asarray(v, np.float32)[None, :], (128, v.shape[-1])))


def att_inputs(NQT, L, tok_start, xb, ctxb, modlat, modctx, gmix, w_in, gains4, sink, rpb):
    D = xb.shape[1]
    NKT = NQT + 4
    tokg = tok_start - 256 + np.arange(NKT * 128)
    xh = np.zeros((NKT * 128 + 256, D), np.float32)
    ok = (tokg >= 0) & (tokg < L)
    xh[:NKT * 128][ok] = xb[tokg[ok]]
    xh[NKT * 128:] = ctxb
    allg = np.concatenate([tokg, np.zeros(256, np.int64)])
    is_ctx = np.concatenate([np.zeros(NKT * 128, bool), np.ones(256, bool)])
    ropeT, perm = rope_tables(allg, is_ctx)
    qs = [tok_start + i * 128 for i in (0, 1, NQT - 2, NQT - 1)]
    biasT, maskA = att_tables(rpb, L, qs, qs[0], qs[3])
    mods = np.stack([np.stack([bc128(modlat[0]), bc128(modlat[1])]), np.stack([bc128(modctx[0]), bc128(modctx[1])])])
    return {"xh": xh, "mods": mods, "gmix": bc128(gmix), "w_in": w_in,
            "gains": np.ascontiguousarray(np.stack(gains4, axis=1).astype(np.float32)),
            "ropeT": np.ascontiguousarray(ropeT), "perm": perm, "ident": np.eye(128, dtype=np.float32),
            "sinkbc": bc128(sink), "biasT": biasT, "maskA": maskA}


import os

NORM_EPS = 1e-6
RW_LNX_EPS = 64e-5
NCTX = 256
DEC_C = float(np.exp(-0.5))


def AP3(ap, dims):
    return bass.AP(ap.tensor, ap.offset, [list(ap.ap[0])] + [list(d) for d in dims])


def build_rwkv(NLAT=16384, D=2048, stage='all'):
    SS = int(os.environ.get('SCAN_STOP', '99'))
    P = Prog()
    KC = D // 128
    NC_C = NCTX // 128
    NC_L = NLAT // 128
    NCH = NC_C + NC_L
    NTOK = NCTX + NLAT
    xcT = P.dram("xcT", [KC, 128, NCTX + 2], F32, kind="ExternalInput")
    xlT = P.dram("xlT", [KC, 128, NLAT + 2], F32, kind="ExternalInput")
    modv = P.dram("modv", [128, 2, 2, KC], F32, kind="ExternalInput")
    gmixv = P.dram("gmixv", [128, KC], F32, kind="ExternalInput")
    muv = P.dram("muv", [128, 6, KC], F32, kind="ExternalInput")
    wr_d = P.dram("wr", [D, 512], F32, kind="ExternalInput")
    wk_d = P.dram("wk", [D, 512], F32, kind="ExternalInput")
    wv_d = P.dram("wv", [D, 512], F32, kind="ExternalInput")
    g1_d = P.dram("g1", [D, 256], F32, kind="ExternalInput")
    g2_d = P.dram("g2", [256, 512], F32, kind="ExternalInput")
    w1_d = P.dram("w1", [D, 192], F32, kind="ExternalInput")
    a1_d = P.dram("a1", [D, 192], F32, kind="ExternalInput")
    w2_d = P.dram("w2", [96, 2, 512], F32, kind="ExternalInput")
    a2_d = P.dram("a2", [96, 2, 512], F32, kind="ExternalInput")
    rows_d = P.dram("rows", [9, 128, 512], F32, kind="ExternalInput")
    masks_d = P.dram("masks", [4, 128, 128], F32, kind="ExternalInput")
    ident_d = P.dram("ident", [128, 128], F32, kind="ExternalInput")
    zout = P.dram("zout", [NLAT, 512], F32, kind="ExternalOutput")
    names = ["Rr", "Vv", "Gg", "KK", "LW0", "LW1", "KD0", "KD1", "BB0", "BB1"]
    S = {n: P.dram("s_" + n, [NTOK, 512], F32) for n in names}
    Yf = P.dram("s_Yf", [NLAT, 512], F32)

    ident = P.sbuf("ident", [128, 128])
    ones_f = P.sbuf("ones_f", [128, 128])
    epsb = P.sbuf("epsb", [128, 4])
    rows = P.sbuf("rows", [128, 9, 512])
    masks = P.sbuf("masks", [128, 4, 128])
    P.dma('sp', ident[:], ident_d[:], w=[ident])
    P.dma('sp', rows[:], rows_d.t.rearrange("n p f -> p n f"), w=[rows])
    P.dma('sp', masks[:], masks_d.t.rearrange("n p f -> p n f"), w=[masks])
    P.op('dve', lambda e: e.memset(ones_f[:], 1.0), w=[ones_f])
    P.op('dve', lambda e: e.memset(epsb[:, 0:1], NORM_EPS), w=[epsb])
    P.op('dve', lambda e: e.memset(epsb[:, 1:2], 1e-12), w=[epsb])
    P.op('dve', lambda e: e.memset(epsb[:, 2:3], RW_LNX_EPS), w=[epsb])
    PB = [P.psum("PB", [128, 512]) for _ in range(8)]
    pbc = [0]

    def nb():
        b = PB[pbc[0] % 8]
        pbc[0] += 1
        return b

    wr = P.sbuf("wr", [128, KC * 512], BF16)
    wk = P.sbuf("wk", [128, KC * 512], BF16)
    wv = P.sbuf("wv", [128, KC * 512], BF16)
    g1 = P.sbuf("g1", [128, KC * 256], BF16)
    w1 = P.sbuf("w1", [128, KC * 192], BF16)
    a1 = P.sbuf("a1", [128, KC * 192], BF16)
    g2 = P.sbuf("g2", [128, 2 * 512], BF16)
    w2 = P.sbuf("w2", [96, 2 * 512], BF16)
    a2 = P.sbuf("a2", [96, 2 * 512], BF16)
    stg = [P.sbuf("stg", [128, 1024]) for _ in range(2)]
    stgc = [0]

    def load_cast(dst_buf, np_, dst_off, src_ap, n):
        sb = stg[stgc[0] % 2]
        stgc[0] += 1
        if len(src_ap.shape) == 3:
            P.dma('sp', sb[0:np_, 0:n].rearrange("p (a b) -> p a b", a=src_ap.shape[1]), src_ap, w=[sb])
        else:
            P.dma('sp', sb[0:np_, 0:n], src_ap, w=[sb])
        P.op('pool', lambda e: e.tensor_copy(out=dst_buf[0:np_, dst_off:dst_off + n], in_=sb[0:np_, 0:n]), r=[sb], w=[dst_buf])

    for (dst, src, ncol) in ((wr, wr_d, 512), (wk, wk_d, 512), (wv, wv_d, 512), (g1, g1_d, 256), (w1, w1_d, 192),
                             (a1, a1_d, 192)):
        per = 1024 // ncol
        for pc in range((KC + per - 1) // per):
            k0 = pc * per
            nk = min(per, KC - k0)
            load_cast(dst, 128, k0 * ncol, src.t[k0 * 128:(k0 + nk) * 128, :].rearrange("(kc p) f -> p kc f", p=128),
                      nk * ncol)
    load_cast(g2, 128, 0, g2_d.t.rearrange("(c p) f -> p c f", p=128), 1024)
    load_cast(w2, 96, 0, w2_d[:], 1024)
    load_cast(a2, 96, 0, a2_d[:], 1024)
    mods = P.sbuf("mods", [128, 2, 2, KC])
    gmx = P.sbuf("gmx", [128, KC])
    mu = P.sbuf("mu", [128, 6, KC])
    P.dma('sp', mods[:], modv[:], w=[mods])
    P.dma('sp', gmx[:], gmixv[:], w=[gmx])
    P.dma('sp', mu[:], muv[:], w=[mu])
    G1 = P.sbuf("G1", [128, 2, KC])
    for st_ in range(2):
        P.op('dve', lambda e, st_=st_: e.scalar_tensor_tensor(out=G1[:, st_, :], in0=mods[:, st_, 0, :], scalar=1.0,
                                                              in1=gmx[:], op0=ALU.add, op1=ALU.mult),
             r=[mods, gmx], w=[G1])
    xe = P.sbuf("xe", [128, KC, 130])
    sq = P.sbuf("sq", [128, KC, 130])
    hh = P.sbuf("hh", [128, KC, 130])
    rs = P.sbuf("rs", [128, 130])
    xx = P.sbuf("xx", [128, KC, 128])
    tm = P.sbuf("tm", [128, KC, 128])
    xm = [P.sbuf("xm", [128, KC, 128], BF16) for _ in range(6)]
    ggT = [P.sbuf("ggT", [128, 128], BF16) for _ in range(2)]
    lrT = [P.sbuf("lrT", [96, 128], BF16) for _ in range(2)]
    E = {n: P.sbuf("e_" + n, [128, 512]) for n in ["r", "k", "v", "g", "kx", "kk", "t0", "t1", "as0", "as1", "lw0", "lw1",
                                                   "kd0", "kd1", "b0", "b1"]}
    sm = P.sbuf("sm", [128, 32])

    def mm_tm(dst_ps, xsrc, wsb, ncol, c0=0, cw=None):
        cw = ncol if cw is None else cw
        for kc in range(KC):
            P.op('pe', lambda e, kc=kc: e.matmul(dst_ps[:, 0:cw], lhsT=xsrc[:, kc, :],
                                                 rhs=wsb[:, kc * ncol + c0: kc * ncol + c0 + cw],
                                                 start=(kc == 0), stop=(kc == KC - 1)), r=[xsrc, wsb], w=[dst_ps])

    for c in range(NCH):
        is_ctx = c < NC_C
        st_ = 1 if is_ctx else 0
        src = xcT if is_ctx else xlT
        lc = c if is_ctx else c - NC_C
        nloc = NC_C if is_ctx else NC_L
        tok0 = c * 128
        P.dma('act', xe[:], src.t[:, :, lc * 128: lc * 128 + 130].rearrange("k p t -> p k t"), w=[xe])
        P.op('pool', lambda e: e.tensor_tensor(out=sq[:], in0=xe[:], in1=xe[:], op=ALU.mult), r=[xe], w=[sq])
        ssb = nb()
        for kc in range(KC):
            P.op('pe', lambda e, kc=kc, ssb=ssb: e.matmul(ssb[:, 0:130], lhsT=ones_f[:], rhs=sq[:, kc, :], start=(kc == 0),
                                                          stop=(kc == KC - 1)), r=[ones_f, sq], w=[ssb])
        P.op('act', lambda e, ssb=ssb: e.activation(out=rs[:], in_=ssb[:, 0:130], func=AF.Sqrt, bias=epsb[:, 0:1],
                                                    scale=1.0 / D), r=[ssb, epsb], w=[rs])
        P.op('dve', lambda e: e.reciprocal(out=rs[:], in_=rs[:]), r=[rs], w=[rs])
        P.op('dve', lambda e: e.tensor_tensor(out=hh[:], in0=xe[:], in1=AP3(rs[:], [[0, KC], [1, 130]]), op=ALU.mult),
             r=[xe, rs], w=[hh])
        P.op('pool', lambda e, st_=st_: e.tensor_tensor(out=hh[:], in0=hh[:], in1=AP3(G1[:, st_, :], [[1, KC], [0, 130]]),
                                                        op=ALU.mult), r=[hh, G1], w=[hh])
        P.op('pool', lambda e, st_=st_: e.tensor_tensor(out=hh[:], in0=hh[:],
                                                        in1=AP3(mods[:, st_, 1, :], [[1, KC], [0, 130]]), op=ALU.add),
             r=[hh, mods], w=[hh])
        if lc == 0:
            P.op('pool', lambda e: e.memset(hh[:, :, 0:1], 0.0), w=[hh])
        if lc == nloc - 1:
            P.op('pool', lambda e: e.memset(hh[:, :, 129:130], 0.0), w=[hh])
        P.op('dve', lambda e: e.tensor_tensor(out=xx[:], in0=hh[:, :, 0:128], in1=hh[:, :, 2:130], op=ALU.add),
             r=[hh], w=[xx])
        P.op('dve', lambda e: e.scalar_tensor_tensor(out=xx[:], in0=xx[:], scalar=0.5, in1=hh[:, :, 1:129], op0=ALU.mult,
                                                     op1=ALU.subtract), r=[xx, hh], w=[xx])
        for m in range(6):
            P.op('pool', lambda e, m=m: e.tensor_tensor(out=tm[:], in0=xx[:], in1=AP3(mu[:, m, :], [[1, KC], [0, 128]]),
                                                        op=ALU.mult), r=[xx, mu], w=[tm])
            P.op('dve', lambda e, m=m: e.tensor_tensor(out=xm[m][:], in0=tm[:], in1=hh[:, :, 1:129], op=ALU.add),
                 r=[tm, hh], w=[xm[m]])
        for (nm, m, wsb) in (("r", 0, wr), ("k", 2, wk), ("v", 3, wv)):
            pb = nb()
            mm_tm(pb, xm[m], wsb, 512)
            P.op('act', lambda e, pb=pb, nm=nm: e.copy(out=E[nm][:], in_=pb[:]), r=[pb], w=[E[nm]])
        for jc in range(2):
            pb = nb()
            for kc in range(KC):
                P.op('pe', lambda e, kc=kc, jc=jc, pb=pb: e.matmul(
                    pb[:, 0:128], lhsT=g1[:, kc * 256 + jc * 128: kc * 256 + (jc + 1) * 128], rhs=xm[5][:, kc, :],
                    start=(kc == 0), stop=(kc == KC - 1)), r=[g1, xm[5]], w=[pb])
            P.op('act', lambda e, pb=pb, jc=jc: e.activation(out=ggT[jc][:], in_=pb[:, 0:128], func=AF.Sigmoid),
                 r=[pb], w=[ggT[jc]])
        pb = nb()
        for jc in range(2):
            P.op('pe', lambda e, jc=jc, pb=pb: e.matmul(pb[:], lhsT=ggT[jc][:], rhs=g2[:, jc * 512:(jc + 1) * 512],
                                                        start=(jc == 0), stop=(jc == 1)), r=[ggT[jc], g2], w=[pb])
        P.op('act', lambda e, pb=pb: e.copy(out=E["g"][:], in_=pb[:]), r=[pb], w=[E["g"]])
        for d in range(2):
            for (kind, m, l1, l2, rowi, dst) in (("w", 1, w1, w2, d, "lw%d" % d), ("a", 4, a1, a2, 2 + d, "as%d" % d)):
                pb = nb()
                for kc in range(KC):
                    P.op('pe', lambda e, kc=kc, pb=pb, l1=l1, m=m, d=d: e.matmul(
                        pb[0:96, 0:128], lhsT=l1[:, kc * 192 + d * 96: kc * 192 + (d + 1) * 96], rhs=xm[m][:, kc, :],
                        start=(kc == 0), stop=(kc == KC - 1)), r=[l1, xm[m]], w=[pb])
                lt = lrT[0 if kind == "w" else 1]
                if kind == "w":
                    P.op('act', lambda e, pb=pb, lt=lt: e.activation(out=lt[:], in_=pb[0:96, 0:128], func=AF.Tanh),
                         r=[pb], w=[lt])
                else:
                    P.op('act', lambda e, pb=pb, lt=lt: e.copy(out=lt[:], in_=pb[0:96, 0:128]), r=[pb], w=[lt])
                pb2 = nb()
                P.op('pe', lambda e, pb2=pb2, lt=lt, l2=l2, d=d: e.matmul(
                    pb2[:], lhsT=lt[:], rhs=l2[:, d * 512:(d + 1) * 512], start=True, stop=True), r=[lt, l2], w=[pb2])
                P.op('dve', lambda e, pb2=pb2, rowi=rowi: e.tensor_tensor(out=E["t0"][:], in0=pb2[:], in1=rows[:, rowi, :],
                                                                          op=ALU.add), r=[pb2, rows], w=[E["t0"]])
                P.op('act', lambda e, dst=dst: e.activation(out=E[dst][:], in_=E["t0"][:], func=AF.Sigmoid),
                     r=[E["t0"]], w=[E[dst]])
                if kind == "w":
                    P.op('pool', lambda e, dst=dst: e.tensor_scalar(out=E[dst][:], in0=E[dst][:], scalar1=-DEC_C,
                                                                    scalar2=None, op0=ALU.mult), r=[E[dst]], w=[E[dst]])
        P.op('dve', lambda e: e.tensor_tensor(out=E["kx"][:], in0=E["k"][:], in1=rows[:, 4, :], op=ALU.mult),
             r=[E["k"], rows], w=[E["kx"]])
        P.op('pool', lambda e: e.tensor_tensor(out=E["t1"][:], in0=E["kx"][:], in1=E["kx"][:], op=ALU.mult),
             r=[E["kx"]], w=[E["t1"]])
        P.op('dve', lambda e: e.tensor_reduce(out=sm[:, 0:8], in_=E["t1"][:].rearrange("p (h n) -> p h n", h=8),
                                              axis=AX.X, op=ALU.add), r=[E["t1"]], w=[sm])
        P.op('act', lambda e: e.activation(out=sm[:, 8:16], in_=sm[:, 0:8], func=AF.Sqrt, bias=epsb[:, 1:2], scale=1.0),
             r=[sm, epsb], w=[sm])
        P.op('dve', lambda e: e.reciprocal(out=sm[:, 8:16], in_=sm[:, 8:16]), r=[sm], w=[sm])
        P.op('dve', lambda e: e.tensor_tensor(out=E["kk"][:].rearrange("p (h n) -> p h n", h=8),
                                              in0=E["kx"][:].rearrange("p (h n) -> p h n", h=8),
                                              in1=AP3(sm[:, 8:16], [[1, 8], [0, 64]]), op=ALU.mult),
             r=[E["kx"], sm], w=[E["kk"]])
        for d in range(2):
            a_ = E["as%d" % d]
            P.op('dve', lambda e, a_=a_: e.scalar_tensor_tensor(out=E["t1"][:], in0=a_[:], scalar=-1.0, in1=rows[:, 5, :],
                                                                op0=ALU.add, op1=ALU.mult), r=[a_, rows], w=[E["t1"]])
            P.op('dve', lambda e, d=d: e.scalar_tensor_tensor(out=E["kd%d" % d][:], in0=E["t1"][:], scalar=1.0,
                                                              in1=E["k"][:], op0=ALU.add, op1=ALU.mult),
                 r=[E["t1"], E["k"]], w=[E["kd%d" % d]])
            P.op('pool', lambda e, d=d, a_=a_: e.tensor_tensor(out=E["b%d" % d][:], in0=E["kk"][:], in1=a_[:], op=ALU.mult),
                 r=[E["kk"], a_], w=[E["b%d" % d]])
        for (sn, en) in (("Rr", "r"), ("Vv", "v"), ("Gg", "g"), ("KK", "kk"), ("LW0", "lw0"), ("LW1", "lw1"),
                         ("KD0", "kd0"), ("KD1", "kd1"), ("BB0", "b0"), ("BB1", "b1")):
            P.dma('act', S[sn][tok0:tok0 + 128, :], E[en][:], r=[E[en]], w=[S[sn]])

    if stage == 'prep':
        dbg = os.environ.get('DBG', 'LW0')
        for c in range(NC_L):
            P.dma('sp', E["t0"][:], S[dbg][(NC_C + c) * 128:(NC_C + c + 1) * 128, :], r=[S[dbg]], w=[E["t0"]])
            P.dma('sp', zout[c * 128:(c + 1) * 128, :], E["t0"][:], r=[E["t0"]], w=[zout])
        return P.finish()

    P.barrier()
    def carve(buf, n_views, shape, is_bf=True):
        nel = int(np.prod(shape[1:]))
        outl = []
        base = buf[:]
        if len(base.shape) == 3:
            base = base.rearrange("p a b -> p (a b)")
        for i in range(n_views):
            if is_bf:
                ap = base[:, i * nel * 2:(i + 1) * nel * 2].bitcast(F32)
            else:
                ap = base[:, i * nel:(i + 1) * nel]
            if len(shape) == 3:
                ap = ap.rearrange("p (a b) -> p a b", a=shape[1])
            outl.append(P.view("cv", ap))
        return outl
    gr_a = carve(wr, 4, [128, 8, 128])
    gr_b = carve(wk, 4, [128, 8, 128])
    gr_c = carve(wv, 4, [128, 8, 128])
    LabT, Lab, LakT, MrbT = gr_a
    MrkT, LT_b, Lm_b, XT_b = gr_b
    LT_a, Lm_a, XT_a, _sp = gr_c
    fm = carve(g1, 4, [128, 4, 128]) + carve(w1, 2, [128, 4, 128])
    KT, BT, RT0, RT1, AT0, AT1 = fm
    RTj = [RT0, RT1]
    ATj = [AT0, AT1]
    for b_ in (RT0, AT0):
        P.op('pool', lambda e, b_=b_: e.memset(b_[64:128, :, :], 0.0), w=[b_])
    for b_ in (RT1, AT1):
        P.op('pool', lambda e, b_=b_: e.memset(b_[0:64, :, :], 0.0), w=[b_])
    pool_ = []
    for b_ in (xe, sq, hh, xx, tm):
        pool_ += carve(b_, 4, [128, 512], is_bf=False)
    for b_ in xm:
        pool_ += carve(b_, 2, [128, 512], is_bf=True)
    pool_ = pool_[::-1]
    inb = [{n: pool_.pop() for n in ["r", "v", "kk", "lw", "kd", "b"]} for _ in range(2)]
    LPx, e1, e2, Rt, Kt, Bt, At, Wsb, Usb, ysb = [pool_.pop() for _ in range(10)]
    ST = P.sbuf("ST", [128, 4, 64])
    PTs = P.sbuf("PTs", [128, 4])
    identb = P.sbuf("identb", [128, 8, 128])
    for h in range(8):
        P.op('pool', lambda e, h=h: e.tensor_copy(out=identb[:, h, :], in_=ident[:]), r=[ident], w=[identb])
    ob = {n: pool_.pop() for n in ["yf", "g", "kd0", "t", "u"]}

    def scan_chunk(c, d, cb):
        reverse = (d == 1)
        mSU, mSL, mIU, mIL = 0, 1, 2, 3
        m_strict_st = mSL if reverse else mSU
        m_strict_ts = mSU if reverse else mSL
        m_incl_st = mIL if reverse else mIU
        I = inb[cb % 2]
        r0 = c * 128
        for (nm, sn) in (("r", "Rr"), ("v", "Vv"), ("kk", "KK"), ("lw", "LW%d" % d), ("kd", "KD%d" % d), ("b", "BB%d" % d)):
            P.dma('sp', I[nm][:], S[sn][r0:r0 + 128, :], r=[S[sn]], w=[I[nm]])
        lpb = nb()
        P.op('pe', lambda e, lpb=lpb: e.matmul(lpb[:], lhsT=masks[:, m_incl_st, :], rhs=I["lw"][:], start=True, stop=True),
             r=[masks, I["lw"]], w=[lpb])
        P.op('act', lambda e, lpb=lpb: e.activation(out=e1[:], in_=lpb[:], func=AF.Exp), r=[lpb], w=[e1])
        P.op('act', lambda e, lpb=lpb: e.activation(out=e2[:], in_=lpb[:], func=AF.Exp, scale=-1.0), r=[lpb], w=[e2])
        P.op('dve', lambda e, lpb=lpb: e.tensor_tensor(out=LPx[:], in0=lpb[:], in1=I["lw"][:], op=ALU.subtract),
             r=[lpb, I["lw"]], w=[LPx])
        P.op('act', lambda e: e.activation(out=LPx[:], in_=LPx[:], func=AF.Exp), r=[LPx], w=[LPx])
        P.op('dve', lambda e: e.tensor_tensor(out=Rt[:], in0=I["r"][:], in1=e1[:], op=ALU.mult), r=[I["r"], e1], w=[Rt])
        P.op('pool', lambda e: e.tensor_tensor(out=Kt[:], in0=I["kd"][:], in1=e2[:], op=ALU.mult), r=[I["kd"], e2], w=[Kt])
        P.op('pool', lambda e: e.tensor_tensor(out=Bt[:], in0=I["b"][:], in1=e2[:], op=ALU.mult), r=[I["b"], e2], w=[Bt])
        P.op('dve', lambda e: e.scalar_tensor_tensor(out=At[:], in0=I["kk"][:], scalar=-1.0, in1=LPx[:], op0=ALU.mult,
                                                     op1=ALU.mult), r=[I["kk"], LPx], w=[At])
        if SS <= 1:
            return None, I
        ptb = nb()
        for g in range(4):
            P.op('pe', lambda e, g=g, ptb=ptb: e.matmul(ptb[:, g:g + 1], lhsT=I["lw"][:, g * 128:(g + 1) * 128],
                                                        rhs=ones_f[:, 0:1], start=True, stop=True),
                 r=[I["lw"], ones_f], w=[ptb])
        P.op('act', lambda e, ptb=ptb: e.activation(out=PTs[:], in_=ptb[:, 0:4], func=AF.Exp), r=[ptb], w=[PTs])
        if SS <= 2:
            return None, I
        for (src, dsts) in ((Rt, RTj), (Kt, [KT]), (Bt, [BT]), (At, ATj)):
            tb = nb()
            for g in range(4):
                P.op('pe', lambda e, g=g, tb=tb, src=src: e.transpose(tb[:, g * 128:(g + 1) * 128],
                                                                      src[:, g * 128:(g + 1) * 128], ident[:]),
                     r=[src, ident], w=[tb])
            if len(dsts) == 1:
                dst = dsts[0]
                P.op('act', lambda e, tb=tb, dst=dst: e.copy(out=dst[:], in_=tb[:].rearrange("p (a b) -> p a b", a=4)),
                     r=[tb], w=[dst])
            else:
                for j in range(2):
                    dst = dsts[j]
                    P.op('dve', lambda e, tb=tb, dst=dst, j=j: e.tensor_copy(
                        out=dst[j * 64:(j + 1) * 64, :, :],
                        in_=tb[j * 64:(j + 1) * 64, :].rearrange("p (a b) -> p a b", a=4)), r=[tb], w=[dst])
        if SS <= 3:
            return None, I

        def fmv(buf, h):
            g, j = h // 2, h % 2
            if isinstance(buf, list):
                return buf[j][:, g, :]
            return buf[:, g, :]

        def bl(buf):
            return buf if isinstance(buf, list) else [buf]
        for (dst, lh, rh, mk) in ((LabT, BT, ATj, m_strict_st), (Lab, ATj, BT, m_strict_ts), (LakT, KT, ATj, m_strict_st),
                                  (MrbT, BT, RTj, m_incl_st), (MrkT, KT, RTj, m_incl_st)):
            for hq in range(2):
                gb = nb()
                for hi in range(4):
                    h = hq * 4 + hi
                    P.op('pe', lambda e, gb=gb, hi=hi, h=h, lh=lh, rh=rh: e.matmul(
                        gb[:, hi * 128:(hi + 1) * 128], lhsT=fmv(lh, h), rhs=fmv(rh, h), start=True, stop=True),
                        r=bl(lh) + bl(rh), w=[gb])
                P.op('dve', lambda e, gb=gb, hq=hq, dst=dst, mk=mk: e.tensor_tensor(
                    out=dst[:, hq * 4:(hq + 1) * 4, :], in0=gb[:].rearrange("p (a b) -> p a b", a=4),
                    in1=AP3(masks[:, mk, :], [[0, 4], [1, 128]]), op=ALU.mult), r=[gb, masks], w=[dst])
        if SS <= 4:
            return None, I
        P.op('pool', lambda e: e.tensor_tensor(out=XT_a[:], in0=LabT[:], in1=identb[:], op=ALU.add), r=[LabT, identb], w=[XT_a])
        LT, Lm, XT = LabT, Lab, XT_a
        pp = [(LT_a, Lm_a, XT_b), (LT_b, Lm_b, XT_a)]
        for it in range(6):
            LTn, Lmn, XTn = pp[it % 2]
            last = (it == 5)
            for hq in range(2):
                if not last:
                    b1 = nb()
                    for hi in range(4):
                        h = hq * 4 + hi
                        P.op('pe', lambda e, b1=b1, hi=hi, h=h, Lm=Lm, LT=LT: e.matmul(
                            b1[:, hi * 128:(hi + 1) * 128], lhsT=Lm[:, h, :], rhs=LT[:, h, :], start=True, stop=True),
                            r=[Lm, LT], w=[b1])
                    P.op('act', lambda e, b1=b1, LTn=LTn, hq=hq: e.copy(
                        out=LTn[:, hq * 4:(hq + 1) * 4, :], in_=b1[:].rearrange("p (a b) -> p a b", a=4)), r=[b1], w=[LTn])
                b2 = nb()
                for hi in range(4):
                    h = hq * 4 + hi
                    P.op('pe', lambda e, b2=b2, hi=hi, h=h, Lm=Lm, LT=LT: e.matmul(
                        b2[:, hi * 128:(hi + 1) * 128], lhsT=LT[:, h, :], rhs=Lm[:, h, :], start=True, stop=True),
                        r=[Lm, LT], w=[b2])
                P.op('act' if last else 'dve', (lambda e, b2=b2, Lmn=Lmn, hq=hq: e.copy(
                    out=Lmn[:, hq * 4:(hq + 1) * 4, :], in_=b2[:].rearrange("p (a b) -> p a b", a=4))) if last else
                    (lambda e, b2=b2, Lmn=Lmn, hq=hq: e.tensor_copy(
                        out=Lmn[:, hq * 4:(hq + 1) * 4, :], in_=b2[:].rearrange("p (a b) -> p a b", a=4))),
                    r=[b2], w=[Lmn])
                b3 = nb()
                for hi in range(4):
                    h = hq * 4 + hi
                    P.op('pe', lambda e, b3=b3, hi=hi, h=h, Lmn=Lmn, XT=XT: e.matmul(
                        b3[:, hi * 128:(hi + 1) * 128], lhsT=Lmn[:, h, :], rhs=XT[:, h, :], start=True, stop=True),
                        r=[Lmn, XT], w=[b3])
                P.op('dve', lambda e, b3=b3, XTn=XTn, XT=XT, hq=hq: e.tensor_tensor(
                    out=XTn[:, hq * 4:(hq + 1) * 4, :], in0=b3[:].rearrange("p (a b) -> p a b", a=4),
                    in1=XT[:, hq * 4:(hq + 1) * 4, :], op=ALU.add), r=[b3, XT], w=[XTn])
            LT, Lm, XT = LTn, Lmn, XTn
        if SS <= 5:
            return None, I
        wbk = nb()
        for h in range(8):
            g, j = h // 2, h % 2
            P.op('pe', lambda e, h=h, g=g, j=j, wbk=wbk: e.matmul(
                wbk[:, h * 64:(h + 1) * 64], lhsT=fmv(ATj, h), rhs=ST[:, g, :], start=True, stop=False),
                r=ATj + [ST], w=[wbk])
            P.op('pe', lambda e, h=h, wbk=wbk: e.matmul(
                wbk[:, h * 64:(h + 1) * 64], lhsT=LakT[:, h, :], rhs=I["v"][:, h * 64:(h + 1) * 64], start=False, stop=True),
                r=[LakT, I["v"]], w=[wbk])
        P.op('act', lambda e, wbk=wbk: e.copy(out=Wsb[:], in_=wbk[:]), r=[wbk], w=[Wsb])
        if SS <= 6:
            return None, I
        ubk = nb()
        for h in range(8):
            P.op('pe', lambda e, h=h, ubk=ubk, XT=XT: e.matmul(
                ubk[:, h * 64:(h + 1) * 64], lhsT=XT[:, h, :], rhs=Wsb[:, h * 64:(h + 1) * 64], start=True, stop=True),
                r=[XT, Wsb], w=[ubk])
        P.op('act', lambda e, ubk=ubk: e.copy(out=Usb[:], in_=ubk[:]), r=[ubk], w=[Usb])
        ybk = nb()
        for h in range(8):
            g, j = h // 2, h % 2
            P.op('pe', lambda e, h=h, g=g, j=j, ybk=ybk: e.matmul(
                ybk[:, h * 64:(h + 1) * 64], lhsT=fmv(RTj, h), rhs=ST[:, g, :], start=True, stop=False),
                r=RTj + [ST], w=[ybk])
            P.op('pe', lambda e, h=h, ybk=ybk: e.matmul(
                ybk[:, h * 64:(h + 1) * 64], lhsT=MrbT[:, h, :], rhs=Usb[:, h * 64:(h + 1) * 64], start=False, stop=False),
                r=[MrbT, Usb], w=[ybk])
            P.op('pe', lambda e, h=h, ybk=ybk: e.matmul(
                ybk[:, h * 64:(h + 1) * 64], lhsT=MrkT[:, h, :], rhs=I["v"][:, h * 64:(h + 1) * 64], start=False, stop=True),
                r=[MrkT, I["v"]], w=[ybk])
        if SS <= 7:
            return None, I
        sbk = nb()
        for h in range(8):
            g = h // 2
            P.op('pe', lambda e, h=h, g=g, sbk=sbk: e.matmul(
                sbk[:, h * 64:(h + 1) * 64], lhsT=Bt[:, g * 128:(g + 1) * 128], rhs=Usb[:, h * 64:(h + 1) * 64],
                start=True, stop=False), r=[Bt, Usb], w=[sbk])
            P.op('pe', lambda e, h=h, g=g, sbk=sbk: e.matmul(
                sbk[:, h * 64:(h + 1) * 64], lhsT=Kt[:, g * 128:(g + 1) * 128], rhs=I["v"][:, h * 64:(h + 1) * 64],
                start=False, stop=True), r=[Kt, I["v"]], w=[sbk])
        for j in range(2):
            ps_v = AP3(sbk[j * 64:(j + 1) * 64, j * 64:(j + 1) * 64], [[128, 4], [1, 64]])
            P.op('dve', lambda e, j=j, ps_v=ps_v: e.tensor_tensor(out=ST[j * 64:(j + 1) * 64, :, :], in0=ps_v,
                                                                 in1=ST[j * 64:(j + 1) * 64, :, :], op=ALU.add),
                 r=[sbk, ST], w=[ST])
        P.op('dve', lambda e: e.tensor_tensor(out=ST[:], in0=ST[:], in1=AP3(PTs[:], [[1, 4], [0, 64]]), op=ALU.mult),
             r=[ST, PTs], w=[ST])
        return ybk, I

    cbc = [0]
    for d in range(2):
        P.op('dve', lambda e: e.memset(ST[:], 0.0), w=[ST])
        ctx_order = list(range(NC_C)) if d == 0 else list(range(NC_C - 1, -1, -1))
        lat_order = list(range(NC_C, NCH)) if d == 0 else list(range(NCH - 1, NC_C - 1, -1))
        for c in ctx_order + lat_order:
            ybk, I = scan_chunk(c, d, cbc[0])
            cbc[0] += 1
            if c < NC_C or ybk is None:
                continue
            lrow = (c - NC_C) * 128
            if d == 0:
                P.op('act', lambda e, ybk=ybk: e.copy(out=ysb[:], in_=ybk[:]), r=[ybk], w=[ysb])
                P.dma('act', Yf[lrow:lrow + 128, :], ysb[:], r=[ysb], w=[Yf])
                continue
            r0 = c * 128
            P.dma('act', ob["yf"][:], Yf[lrow:lrow + 128, :], r=[Yf], w=[ob["yf"]])
            P.dma('act', ob["g"][:], S["Gg"][r0:r0 + 128, :], r=[S["Gg"]], w=[ob["g"]])
            P.dma('act', ob["kd0"][:], S["KD0"][r0:r0 + 128, :], r=[S["KD0"]], w=[ob["kd0"]])
            y3 = lambda b_: b_[:].rearrange("p (h n) -> p h n", h=8)
            P.op('dve', lambda e, ybk=ybk: e.tensor_tensor(out=ysb[:], in0=ybk[:], in1=ob["yf"][:], op=ALU.add),
                 r=[ybk, ob["yf"]], w=[ysb])
            P.op('dve', lambda e: e.tensor_reduce(out=sm[:, 0:8], in_=y3(ysb), axis=AX.X, op=ALU.add), r=[ysb], w=[sm])
            P.op('dve', lambda e: e.tensor_scalar(out=sm[:, 0:8], in0=sm[:, 0:8], scalar1=-1.0 / 64, scalar2=None,
                                                  op0=ALU.mult), r=[sm], w=[sm])
            P.op('dve', lambda e: e.tensor_tensor(out=y3(ysb), in0=y3(ysb), in1=AP3(sm[:, 0:8], [[1, 8], [0, 64]]),
                                                  op=ALU.add), r=[ysb, sm], w=[ysb])
            P.op('pool', lambda e: e.tensor_tensor(out=ob["t"][:], in0=ysb[:], in1=ysb[:], op=ALU.mult), r=[ysb], w=[ob["t"]])
            P.op('dve', lambda e: e.tensor_reduce(out=sm[:, 8:16], in_=y3(ob["t"]), axis=AX.X, op=ALU.add),
                 r=[ob["t"]], w=[sm])
            P.op('act', lambda e: e.activation(out=sm[:, 8:16], in_=sm[:, 8:16], func=AF.Sqrt, bias=epsb[:, 2:3],
                                               scale=1.0 / 64), r=[sm, epsb], w=[sm])
            P.op('dve', lambda e: e.reciprocal(out=sm[:, 8:16], in_=sm[:, 8:16]), r=[sm], w=[sm])
            P.op('dve', lambda e: e.tensor_tensor(out=y3(ysb), in0=y3(ysb), in1=AP3(sm[:, 8:16], [[1, 8], [0, 64]]),
                                                  op=ALU.mult), r=[ysb, sm], w=[ysb])
            P.op('pool', lambda e: e.tensor_tensor(out=ysb[:], in0=ysb[:], in1=rows[:, 7, :], op=ALU.mult),
                 r=[ysb, rows], w=[ysb])
            P.op('pool', lambda e: e.tensor_tensor(out=ysb[:], in0=ysb[:], in1=rows[:, 8, :], op=ALU.add),
                 r=[ysb, rows], w=[ysb])
            P.op('dve', lambda e, I=I: e.tensor_tensor(out=ob["t"][:], in0=ob["kd0"][:], in1=I["kd"][:], op=ALU.add),
                 r=[ob["kd0"], I["kd"]], w=[ob["t"]])
            P.op('dve', lambda e, I=I: e.tensor_tensor(out=ob["t"][:], in0=ob["t"][:], in1=I["r"][:], op=ALU.mult),
                 r=[ob["t"], I["r"]], w=[ob["t"]])
            P.op('pool', lambda e: e.tensor_tensor(out=ob["t"][:], in0=ob["t"][:], in1=rows[:, 6, :], op=ALU.mult),
                 r=[ob["t"], rows], w=[ob["t"]])
            P.op('dve', lambda e: e.tensor_reduce(out=sm[:, 16:24], in_=y3(ob["t"]), axis=AX.X, op=ALU.add),
                 r=[ob["t"]], w=[sm])
            P.op('dve', lambda e, I=I: e.tensor_tensor(out=y3(ob["u"]), in0=I["v"][:].rearrange("p (h n) -> p h n", h=8),
                                                       in1=AP3(sm[:, 16:24], [[1, 8], [0, 64]]), op=ALU.mult),
                 r=[I["v"], sm], w=[ob["u"]])
            P.op('pool', lambda e: e.tensor_tensor(out=ysb[:], in0=ysb[:], in1=ob["u"][:], op=ALU.add),
                 r=[ysb, ob["u"]], w=[ysb])
            P.op('pool', lambda e: e.tensor_tensor(out=ysb[:], in0=ysb[:], in1=ob["g"][:], op=ALU.mult),
                 r=[ysb, ob["g"]], w=[ysb])
            P.dma('act', zout[lrow:lrow + 128, :], ysb[:], r=[ysb], w=[zout])
    return P.finish()


def bc128(v):
    return np.ascontiguousarray(np.broadcast_to(np.asarray(v, np.float32).reshape(1, -1), (128, v.size)))


def rwkv_inputs(x1b, xcb, hg, modlat, modctx, gmix, W):
    D = x1b.shape[1]
    KC = D // 128
    F = slice(hg * 512, (hg + 1) * 512)

    def padT(x):
        xt = np.zeros((D, x.shape[0] + 2), np.float32)
        xt[:, 1:-1] = x.T
        return np.ascontiguousarray(xt.reshape(KC, 128, -1))

    def fm(v):
        return np.ascontiguousarray(np.asarray(v, np.float32).reshape(KC, 128).T)
    modv = np.stack([np.stack([fm(modlat[0]), fm(modlat[1])], axis=1), np.stack([fm(modctx[0]), fm(modctx[1])], axis=1)], axis=1)
    muv = np.stack([fm(W["mu"][m]) for m in range(6)], axis=1)
    rowsl = [W["w0"][0][F], W["w0"][1][F], W["a0"][0][F], W["a0"][1][F], W["k_k"][F], W["k_a"][F],
             W["r_k"].reshape(-1)[F], W["lnx_w"][F], W["lnx_b"][F]]
    idx = np.arange(128)
    SU = (idx[:, None] < idx[None, :]).astype(np.float32)
    masks = np.stack([SU, SU.T, SU + np.eye(128, dtype=np.float32), SU.T + np.eye(128, dtype=np.float32)])
    return {"xcT": padT(xcb), "xlT": padT(x1b), "modv": np.ascontiguousarray(modv), "gmixv": fm(gmix), "muv": np.ascontiguousarray(muv),
            "wr": np.ascontiguousarray(W["wr"][:, F]), "wk": np.ascontiguousarray(W["wk"][:, F]),
            "wv": np.ascontiguousarray(W["wv"][:, F]), "g1": np.ascontiguousarray(W["g1"]),
            "g2": np.ascontiguousarray(W["g2"][:, F]),
            "w1": np.ascontiguousarray(np.concatenate([W["w1"][0], W["w1"][1]], axis=1)),
            "a1": np.ascontiguousarray(np.concatenate([W["a1"][0], W["a1"][1]], axis=1)),
            "w2": np.ascontiguousarray(np.stack([W["w2"][0][:, F], W["w2"][1][:, F]], axis=1)),
            "a2": np.ascontiguousarray(np.stack([W["a2"][0][:, F], W["a2"][1][:, F]], axis=1)),
            "rows": np.stack([bc128(v) for v in rowsl]), "masks": masks, "ident": np.eye(128, dtype=np.float32)}


def _run(nc, in_maps):
    res = run_bass_kernel_spmd(nc, in_maps, core_ids=list(range(len(in_maps))))
    return res.results


def kernel(x, c, ctx, c_ctx, ada_w, ada_b, norm_mix_g, norm_ffn_g, attn_w_in, attn_w_out, a_q_gain, a_k_gain, a_sink,
           b_q_gain, b_k_gain, b_rpb, rw_mu, rw_wr, rw_wk, rw_wv, rw_wo, rw_w0, rw_w1, rw_w2, rw_a0, rw_a1, rw_a2, rw_g1,
           rw_g2, rw_k_k, rw_k_a, rw_r_k, rw_lnx_w, rw_lnx_b, moe_w_grp, moe_w_exp, moe_w1, moe_w3, moe_w2):
    f32 = lambda a: np.ascontiguousarray(np.asarray(a, dtype=np.float32))
    x, ctx = f32(x), f32(ctx)
    B, L, D = x.shape
    NC = 8
    QPB = NC // B
    TPC = L // QPB
    NQT = TPC // 128
    ident = np.eye(128, dtype=np.float32)
    mod = run_ada(f32(c), f32(c_ctx), f32(ada_w), f32(ada_b))

    def moe_launch(layer, x0_list, z_list, wo, modsets_list, tile_set):
        NT = x0_list[0].shape[0]
        nc = build_moe(NT, tile_set)
        wr_cat = np.ascontiguousarray(np.concatenate([f32(moe_w_grp[layer]), f32(moe_w_exp[layer])], axis=1))
        w1_, w3_, w2_ = f32(moe_w1[layer]), f32(moe_w3[layer]), f32(moe_w2[layer])
        gn = bc128(f32(norm_ffn_g[layer]))
        ims = []
        for k in range(NC):
            ms = np.stack([np.stack([bc128(v) for v in st]) for st in modsets_list[k]])
            ims.append({"x0": x0_list[k], "z": z_list[k], "wo": wo, "mods": ms, "gnorm": gn, "wr": wr_cat,
                        "w1": w1_, "w3": w3_, "w2": w2_, "ident": ident})
        return [r["out"] for r in _run(nc, ims)]

    nc = build_att(NQT=NQT)
    ims = []
    for k in range(NC):
        b, q = k // QPB, k % QPB
        ims.append(att_inputs(NQT, L, q * TPC, x[b], ctx[b], (mod[0, b, 1], mod[0, b, 0]), (mod[0, 2, 1], mod[0, 2, 0]),
                              f32(norm_mix_g[0]), f32(attn_w_in[0]),
                              [f32(a_q_gain[0]), f32(a_k_gain[0]), f32(b_q_gain[0]), f32(b_k_gain[0])],
                              f32(a_sink[0]), f32(b_rpb[0])))
    z0 = [r["zout"] for r in _run(nc, ims)]
    del ims
    pad = np.zeros((256, D), np.float32)
    x0_list, z_list, msets = [], [], []
    for k in range(NC):
        b, q = k // QPB, k % QPB
        x0_list.append(np.ascontiguousarray(np.concatenate([x[b, q * TPC:(q + 1) * TPC], ctx[b], pad], axis=0)))
        z_list.append(np.ascontiguousarray(np.concatenate([z0[k], pad], axis=0)))
        msets.append([[mod[0, b, 2], mod[0, b, 4], mod[0, b, 3], mod[0, b, 5]],
                      [mod[0, 2, 2], mod[0, 2, 4], mod[0, 2, 3], mod[0, 2, 5]]])
    o0 = moe_launch(0, x0_list, z_list, f32(attn_w_out[0]), msets, [0] * NQT + [1] * 4)
    del x0_list, z_list, z0
    x1 = np.stack([np.concatenate([o0[b * QPB + q][:TPC] for q in range(QPB)], axis=0) for b in range(B)])
    xc1 = np.stack([o0[b * QPB][TPC:TPC + 256] for b in range(B)])
    del o0
    if os.environ.get('KDUMP'):
        np.save(os.environ['KDUMP'] + '_x1.npy', x1); np.save(os.environ['KDUMP'] + '_xc1.npy', xc1)
    W = {"mu": f32(rw_mu[0]), "wr": f32(rw_wr[0]), "wk": f32(rw_wk[0]), "wv": f32(rw_wv[0]), "w0": f32(rw_w0[0]),
         "w1": f32(rw_w1[0]), "w2": f32(rw_w2[0]), "a0": f32(rw_a0[0]), "a1": f32(rw_a1[0]), "a2": f32(rw_a2[0]),
         "g1": f32(rw_g1[0]), "g2": f32(rw_g2[0]), "k_k": f32(rw_k_k[0]), "k_a": f32(rw_k_a[0]), "r_k": f32(rw_r_k[0]),
         "lnx_w": f32(rw_lnx_w[0]), "lnx_b": f32(rw_lnx_b[0])}
    nc = build_rwkv(NLAT=L)
    ims = []
    for k in range(NC):
        b, hg = k // QPB, k % QPB
        ims.append(rwkv_inputs(x1[b], xc1[b], hg, (mod[1, b, 1], mod[1, b, 0]), (mod[1, 2, 1], mod[1, 2, 0]),
                               f32(norm_mix_g[1]), W))
    zr = [r["zout"] for r in _run(nc, ims)]
    del ims
    z1 = np.stack([np.concatenate([zr[b * QPB + hg] for hg in range(QPB)], axis=1) for b in range(B)])
    del zr
    if os.environ.get('KDUMP'):
        np.save(os.environ['KDUMP'] + '_z1.npy', z1)
    x0_list, z_list, msets = [], [], []
    for k in range(NC):
        b, q = k // QPB, k % QPB
        x0_list.append(np.ascontiguousarray(x1[b, q * TPC:(q + 1) * TPC]))
        z_list.append(np.ascontiguousarray(z1[b, q * TPC:(q + 1) * TPC]))
        msets.append([[mod[1, b, 2], mod[1, b, 4], mod[1, b, 3], mod[1, b, 5]]])
    o1 = moe_launch(1, x0_list, z_list, f32(rw_wo[0]), msets, [0] * NQT)
    out = np.stack([np.concatenate([o1[b * QPB + q] for q in range(QPB)], axis=0) for b in range(B)])
    return np.ascontiguousarray(out.astype(np.float32))
```

```python
import contextlib
import numpy as np
import concourse.bass as bass
import concourse.mybir as mybir
from concourse.bass_utils import run_bass_kernel_spmd

F32 = mybir.dt.float32
BF16 = mybir.dt.bfloat16
I32 = mybir.dt.int32
ALU = mybir.AluOpType
AF = mybir.ActivationFunctionType
AX = mybir.AxisListType

ENG = {'pe': 'tensor', 'act': 'scalar', 'dve': 'vector', 'pool': 'gpsimd', 'sp': 'sync'}


class _St:
    pass


class Buf:
    def __init__(self, name, t, st=None):
        self.t = t
        if st is None:
            st = _St()
            st.name = name
            st.w = {}
            st.r = {}
            st.dcnt = 0
            st.pre = {}
            st.is_psum = False
        self.__dict__['_s'] = st

    def alias(self, ap):
        return Buf(None, ap, st=self._s)

    def __getattr__(self, k):
        if k in ('name', 'w', 'r', 'dcnt', 'pre', 'is_psum'):
            return getattr(self.__dict__['_s'], k)
        raise AttributeError(k)

    def __setattr__(self, k, v):
        if k in ('name', 'w', 'r', 'dcnt', 'pre', 'is_psum'):
            setattr(self.__dict__['_s'], k, v)
        else:
            self.__dict__[k] = v

    def __getitem__(self, idx):
        return self.t[idx]


class Prog:
    def __init__(self, arena=False, phased=False):
        self.nc = bass.Bass("TRN2", target_bir_lowering=False)
        self.stack = contextlib.ExitStack()
        self.arena_t = None
        self.dphys = {}
        self.dfree = []
        self.phased = phased
        self.pstack = contextlib.ExitStack()
        if phased:
            self.pbanks = []
            for i in range(8):
                b = Buf("PBK%d" % i, self.stack.enter_context(self.nc.psum_tensor("PBK%d" % i, [128, 512], F32)))
                b.is_psum = True
                self.pbanks.append(b)
            self.pcnt = 0
        if arena:
            self.AR = 53200
            self.arena_t = self.stack.enter_context(self.nc.sbuf_tensor("arena", [128, self.AR], F32))
            self.apos = 0
            self.pbanks = []
            for i in range(8):
                b = Buf("PBK%d" % i, self.stack.enter_context(self.nc.psum_tensor("PBK%d" % i, [128, 512], F32)))
                b.is_psum = True
                self.pbanks.append(b)
            self.pcnt = 0
        self.stream = {e: [] for e in ENG}
        self.cnt = {e: 0 for e in ENG}
        self.sems = {}
        self.seen = {e: {} for e in ENG}
        self.outs = []
        self.nbuf = 0
        self.dtot = {}

    def sem(self, key):
        if key not in self.sems:
            self.sems[key] = self.stack.enter_context(self.nc.semaphore("s%d_%s" % (len(self.sems), key[:20])))
        return self.sems[key]

    def sbuf(self, name, shape, dt=F32):
        self.nbuf += 1
        if self.arena_t is not None:
            nel = 1
            for d_ in shape[1:]:
                nel *= int(d_)
            nfl = nel if dt == F32 else nel // 2
            off = self.apos
            self.apos += (nfl + 15) // 16 * 16
            assert self.apos <= self.AR, ("arena overflow", name, self.apos * 4)
            ap = self.arena_t[0:shape[0], off:off + nfl]
            if dt != F32:
                ap = ap.bitcast(dt)
            if len(shape) == 3:
                ap = ap.rearrange("p (a b) -> p a b", a=shape[1])
            elif len(shape) == 4:
                ap = ap.rearrange("p (a b c) -> p a b c", a=shape[1], b=shape[2])
            return Buf("%s_%d" % (name, self.nbuf), ap)
        stk = self.pstack if self.phased else self.stack
        t = stk.enter_context(self.nc.sbuf_tensor("%s_%d" % (name, self.nbuf), list(shape), dt))
        return Buf("%s_%d" % (name, self.nbuf), t)

    def psum(self, name, shape, dt=F32):
        self.nbuf += 1
        if self.arena_t is not None or self.phased:
            b = self.pbanks[self.pcnt % 8]
            self.pcnt += 1
            if shape[0] < 128 or shape[1] < 512:
                return b.alias(b[0:shape[0], 0:shape[1]])
            return b
        t = self.stack.enter_context(self.nc.psum_tensor("%s_%d" % (name, self.nbuf), list(shape), dt))
        b = Buf("%s_%d" % (name, self.nbuf), t)
        b.is_psum = True
        return b

    def dram(self, name, shape, dt=F32, kind="Internal"):
        t = self.nc.dram_tensor(name, list(shape), dt, kind=kind).ap()
        b = Buf(name, t)
        if kind == "ExternalOutput":
            self.outs.append(b)
        return b

    def _waits(self, eng, r, w, dkey=None, selfsync=True):
        waits = {}
        for b in r:
            for k, v in b.w.items():
                waits[k] = max(waits.get(k, 0), v)
            if b.is_psum:
                for k, v in b.r.items():
                    if k != 'E_' + eng:
                        waits[k] = max(waits.get(k, 0), v)
        for b in w:
            for k, v in list(b.w.items()) + list(b.r.items()):
                if dkey is not None and k == dkey:
                    continue
                waits[k] = max(waits.get(k, 0), v)
        if eng == 'pe' or not selfsync:
            waits.pop('E_' + eng, None)
        wl = [(k, v) for k, v in waits.items() if v > self.seen[eng].get(k, 0)]
        for k, v in wl:
            self.seen[eng][k] = v
        return wl

    def op(self, eng, fn, r=(), w=(), selfsync=True):
        wl = self._waits(eng, r, w, selfsync=selfsync)
        key = 'E_' + eng
        self.sem(key)
        for k, _ in wl:
            self.sem(k)
        self.cnt[eng] += 1
        n = self.cnt[eng]
        self.stream[eng].append((wl, fn, (key, 1)))
        for b in r:
            b.r[key] = n
        for b in w:
            b.w = {key: n}
            b.r = {}
        return n

    def dma(self, q, out_ap, in_ap, r=(), w=(), **kw):
        assert len(w) == 1
        wb = w[0]
        lkey = 'D_' + wb.name
        if lkey not in self.dphys:
            if self.dfree:
                self.dphys[lkey] = self.dfree.pop()
            else:
                self.dphys[lkey] = 'DP%d' % len(self.dtot)
                self.dtot[self.dphys[lkey]] = 0
            wb.dcnt = self.dtot[self.dphys[lkey]]
        dkey = self.dphys[lkey]
        self.sem(dkey)
        full = dict(wb.pre)
        for b in r:
            for k, v in b.w.items():
                full[k] = max(full.get(k, 0), v)
        for k, v in list(wb.w.items()) + list(wb.r.items()):
            if k != dkey:
                full[k] = max(full.get(k, 0), v)
        wb.pre = dict(full)
        wl = [(k, v) for k, v in full.items() if v > self.seen[q].get(k, 0)]
        for k, v in wl:
            self.seen[q][k] = v
            self.sem(k)
        wb.dcnt += 16
        val = wb.dcnt
        self.dtot[dkey] = val
        self.stream[q].append((wl, (lambda e: e.dma_start(out=out_ap, in_=in_ap, **kw)), (dkey, 16)))
        for b in r:
            b.r[dkey] = val
        keep = {k: v for k, v in wb.w.items() if k == dkey}
        wb.w = keep
        wb.w[dkey] = val
        wb.r = {}

    def barrier(self):
        snap = {}
        for e in ENG:
            if self.cnt[e] > 0:
                snap['E_' + e] = self.cnt[e]
        snap.update(self.dtot)
        for e in ENG:
            wl = [(k, v) for k, v in snap.items() if k != 'E_' + e and v > self.seen[e].get(k, 0)]
            for k, v in wl:
                self.seen[e][k] = v
            if wl:
                self.stream[e].append((wl, None, None))
        self.dfree = sorted(set(self.dphys.values()) | set(self.dfree))
        self.dphys = {}

    def view(self, name, ap):
        self.nbuf += 1
        return Buf("%s_%d" % (name, self.nbuf), ap)

    def _emit_block(self):
        with self.nc.Block() as block:
            for e, attr in ENG.items():
                lst = self.stream[e]
                if not lst:
                    continue

                def f(eng, lst=lst):
                    for wl, fn, inc in lst:
                        for k, v in wl:
                            eng.wait_ge(self.sems[k], v)
                        if fn is None:
                            continue
                        ins = fn(eng)
                        if inc is not None:
                            ins.then_inc(self.sems[inc[0]], inc[1])
                getattr(block, attr)(f)
        self.stream = {e: [] for e in ENG}

    def end_phase(self):
        self.barrier()
        if self.phased:
            self._emit_block()
            self.pstack.close()
            self.pstack = contextlib.ExitStack()
        else:
            self.apos = 0
        self.pcnt = 0

    def finish(self):
        waits = {}
        for b in self.outs:
            for k, v in b.w.items():
                waits[k] = max(waits.get(k, 0), v)
        self.stream['sp'].append((list(waits.items()), None, None))
        self._emit_block()
        self.pstack.close()
        self.stack.close()
        return self.nc


D = 2048
NCORE = 8
import os
NL = int(os.environ.get('NL', '2'))
ADA_COLS = 6 * D // NCORE


def build_ada():
    P = Prog()
    nc = P.nc
    cT = P.dram("cT", [128, 16, 3], F32, kind="ExternalInput")
    w = P.dram("w", [2, 16, 128, ADA_COLS], F32, kind="ExternalInput")
    b = P.dram("b", [2, 1, ADA_COLS], F32, kind="ExternalInput")
    out = P.dram("out", [2, 3, ADA_COLS], F32, kind="ExternalOutput")
    c_sb = P.sbuf("c_sb", [128, 16, 3])
    s_sb = P.sbuf("s_sb", [128, 16, 3])
    ones = P.sbuf("ones", [1, 3])
    P.dma('sp', c_sb[:], cT[:], w=[c_sb])
    P.op('act', lambda e: e.activation(out=s_sb[:], in_=c_sb[:], func=AF.Silu), r=[c_sb], w=[s_sb])
    P.op('dve', lambda e: e.memset(ones[:], 1.0), w=[ones])
    w_sb = P.sbuf("w_sb", [128, 16, ADA_COLS])
    b_sb = P.sbuf("b_sb", [1, ADA_COLS])
    o_sb = P.sbuf("o_sb", [3, ADA_COLS])
    pss = [P.psum("ps", [3, 512]) for _ in range(3)]
    for i in range(NL):
        for kq in range(4):
            P.dma('sp' if kq % 2 == 0 else 'pool', w_sb[:, kq * 4:(kq + 1) * 4, :],
                  w[i, kq * 4:(kq + 1) * 4].rearrange("k p n -> p k n"), w=[w_sb])
        P.dma('sp', b_sb[:], b[i], w=[b_sb])
        for j in range(ADA_COLS // 512):
            ps = pss[j]
            for kc in range(16):
                P.op('pe', lambda e, kc=kc, j=j, ps=ps, w_sb=w_sb: e.matmul(
                    ps[:], lhsT=s_sb[:, kc, :], rhs=w_sb[:, kc, j * 512:(j + 1) * 512],
                    start=(kc == 0), stop=False), r=[s_sb, w_sb], w=[ps])
            P.op('pe', lambda e, j=j, ps=ps, b_sb=b_sb: e.matmul(
                ps[:], lhsT=ones[:], rhs=b_sb[:, j * 512:(j + 1) * 512], start=False, stop=True),
                r=[ones, b_sb], w=[ps])
            P.op('dve', lambda e, j=j, ps=ps, o_sb=o_sb: e.tensor_copy(out=o_sb[:, j * 512:(j + 1) * 512], in_=ps[:]),
                 r=[ps], w=[o_sb])
        P.dma('sp', out[i], o_sb[:], r=[o_sb], w=[out])
    return P.finish()


def run_ada(c, c_ctx, ada_w, ada_b):
    cvec = np.stack([c[0], c[1], c_ctx], axis=0)
    cT = np.ascontiguousarray(cvec.reshape(3, 16, 128).transpose(2, 1, 0))
    in_maps = []
    for k in range(NCORE):
        ws = ada_w[:, :, k * ADA_COLS:(k + 1) * ADA_COLS].reshape(2, 16, 128, ADA_COLS)
        bs = ada_b[:, k * ADA_COLS:(k + 1) * ADA_COLS].reshape(2, 1, ADA_COLS)
        in_maps.append({"cT": cT, "w": np.ascontiguousarray(ws), "b": np.ascontiguousarray(bs)})
    nc = build_ada()
    res = run_bass_kernel_spmd(nc, in_maps, core_ids=list(range(NCORE)))
    mod = np.concatenate([res.results[k]["out"] for k in range(NCORE)], axis=-1)
    return mod.reshape(2, 3, 6, D)


import os
TQ = os.environ.get('TQ', 'act')

NORM_EPS = 1e-6


def AP3(ap, dims):
    return bass.AP(ap.tensor, ap.offset, [list(ap.ap[0])] + [list(d) for d in dims])


def build_moe(NT, tile_set, D=2048, FF=512, E=32, NG=4, stage='all', P=None, ext=None, sfx='', out_hook=None):
    own = P is None
    if own:
        P = Prog()
    ext = ext or {}

    def DR(name, shape, dt, kind):
        if name in ext:
            return ext[name]
        return P.dram(name + sfx, shape, dt, kind=kind)
    KC = D // 128
    FFC = FF // 128
    DS = D // 512
    EG = E // NG
    NR = NG + E
    ntile = NT // 128
    ngroup = NT // 512
    nset = max(tile_set) + 1
    x0 = DR("x0", [NT, D], F32, "ExternalInput")
    z = DR("z", [NT, D], F32, "ExternalInput")
    wo = DR("wo", [D, D], F32, "ExternalInput")
    mods = DR("mods", [nset, 4, 128, D], F32, "ExternalInput")
    gnorm = DR("gnorm", [128, D], F32, "ExternalInput")
    wr = DR("wr", [D, NR], F32, "ExternalInput")
    w1 = DR("w1", [E, D, FF], F32, "ExternalInput")
    w3 = DR("w3", [E, D, FF], F32, "ExternalInput")
    w2 = DR("w2", [E, FF, D], F32, "ExternalInput")
    ident_d = DR("ident", [128, 128], F32, "ExternalInput")
    out = DR("out", [NT, D], F32, "ExternalOutput")
    x1s = P.dram("x1s" + sfx, [NT, D], F32)
    x0ap = ext.get("x0ap", lambda t_: x0[t_ * 128:(t_ + 1) * 128, :])
    modap = ext.get("modap", lambda s_, j_: mods[s_, j_])
    gnap = ext.get("gnap", lambda: gnorm[:])

    WSZ = max(KC * FF, FFC * D)
    wb = [[P.sbuf("w%d_%d" % (m, j), [128, WSZ], BF16) for j in range(2)] for m in range(3)]
    y_acc = P.sbuf("y_acc", [128, 4, D])
    hT_bf = P.sbuf("hT_bf", [128, KC, 512], BF16)
    tmpA = P.sbuf("tmpA", [128, D])
    xt = P.sbuf("xt", [128, D])
    htmp = P.sbuf("htmp", [128, D])
    m_a = P.sbuf("m_a", [128, D])
    m_b = P.sbuf("m_b", [128, D])
    PIECE = min(1024, KC * FF)
    stg = [P.sbuf("stg", [128, PIECE]) for _ in range(2)]
    stgc = [0]

    def load_cast(dst_buf, dst_off, src_ap):
        sb = stg[stgc[0] % 2]
        stgc[0] += 1
        P.dma('sp', sb[:] if len(src_ap.shape) == 2 else sb[:].rearrange("p (a b) -> p a b", a=src_ap.shape[1]),
              src_ap, w=[sb])
        P.op('pool', lambda e: e.tensor_copy(out=dst_buf[:, dst_off:dst_off + PIECE], in_=sb[:]), r=[sb], w=[dst_buf])
    s_sb = [P.sbuf("s_sb", [128, 512]) for _ in range(1)]
    actT = [P.sbuf("actT", [128, FFC, 512], BF16) for _ in range(2)]
    assert FFC * 512 == KC * 128
    zT_bf = actT[0].alias(actT[0][:].rearrange("p a (b c) -> p (a b) c", c=128))
    ident = P.sbuf("ident", [128, 128])
    wr_sb = P.sbuf("wr_sb", [128, KC, NR])
    G = P.sbuf("G", [128, 4, E])
    sm = P.sbuf("sm", [128, 16])
    lg = P.sbuf("lg", [128, NR])
    r1 = P.sbuf("r1", [128, E])
    r2 = P.sbuf("r2", [128, E])
    r3 = P.sbuf("r3", [128, E])
    r4 = P.sbuf("r4", [128, NG])
    TB = [P.psum("TB", [128, 512]) for _ in range(4)]
    YB = [P.psum("YB", [128, 512]) for _ in range(4)]

    epsb = P.sbuf("epsb", [128, 1])
    P.op('dve', lambda e: e.memset(epsb[:], NORM_EPS), w=[epsb])
    P.dma('sp', ident[:], ident_d[:], w=[ident])
    P.dma('sp', wr_sb[:], wr.t.rearrange("(kc p) n -> p kc n", p=128), w=[wr_sb])

    def transpose_to(src, dst_fn, dst_bufs, ncol=128):
        for q in range(KC // 4):
            bank = TB[q % 4]
            for j in range(4):
                kc = q * 4 + j
                P.op('pe', lambda e, bank=bank, j=j, kc=kc: e.transpose(
                    bank[:, j * 128:(j + 1) * 128], src[:, kc * 128:(kc + 1) * 128], ident[:]),
                    r=[src, ident], w=[bank])
            dst_fn(q, bank)

    per = WSZ // D
    wo_loc = []
    flat = [wb[m][j] for m in range(3) for j in range(2)]
    assert per * len(flat) >= KC
    for kc in range(KC):
        b = flat[kc // per]
        off = (kc % per) * D
        wo_loc.append((b, off))
        for pc in range(D // PIECE):
            load_cast(b, off + pc * PIECE, wo[kc * 128:(kc + 1) * 128, pc * PIECE:(pc + 1) * PIECE])
    cur_set = -1
    for t in range(ntile):
        if tile_set[t] != cur_set:
            cur_set = tile_set[t]
            P.dma(TQ, m_a[:], modap(cur_set, 0), w=[m_a])
        P.dma(TQ, tmpA[:], z[t * 128:(t + 1) * 128, :], w=[tmpA])
        P.dma(TQ, xt[:], x0ap(t), w=[xt])

        def ev(q, bank):
            eng = 'act' if q % 2 == 0 else 'dve'
            if eng == 'act':
                P.op('act', lambda e, q=q, bank=bank: e.copy(
                    out=zT_bf[:, q * 4:(q + 1) * 4, :], in_=bank[:].rearrange("p (a b) -> p a b", a=4)),
                    r=[bank], w=[zT_bf])
            else:
                P.op('dve', lambda e, q=q, bank=bank: e.tensor_copy(
                    out=zT_bf[:, q * 4:(q + 1) * 4, :], in_=bank[:].rearrange("p (a b) -> p a b", a=4)),
                    r=[bank], w=[zT_bf])
        transpose_to(tmpA, ev, [zT_bf])
        for ds in range(DS):
            for kc in range(KC):
                b, off = wo_loc[kc]
                P.op('pe', lambda e, ds=ds, kc=kc, b=b, off=off: e.matmul(
                    YB[ds][:], lhsT=zT_bf[:, kc, :], rhs=b[:, off + ds * 512: off + (ds + 1) * 512],
                    start=(kc == 0), stop=(kc == KC - 1)), r=[zT_bf, b], w=[YB[ds]])
            P.op('dve', lambda e, ds=ds: e.tensor_tensor(
                out=htmp[:, ds * 512:(ds + 1) * 512], in0=YB[ds][:], in1=m_a[:, ds * 512:(ds + 1) * 512],
                op=ALU.mult), r=[YB[ds], m_a], w=[htmp])
        P.op('pool', lambda e: e.tensor_tensor(out=htmp[:], in0=htmp[:], in1=xt[:], op=ALU.add),
             r=[htmp, xt], w=[htmp])
        P.dma(TQ, x1s[t * 128:(t + 1) * 128, :], htmp[:], r=[htmp], w=[x1s])
        if stage == 'A':
            P.dma(TQ, out[t * 128:(t + 1) * 128, :], htmp[:], r=[htmp], w=[out])
    if stage == 'A':
        return P.finish()

    cur_set = -1
    wcount = [0, 0, 0]

    wcache = [P.dram("wc%d%s" % (m, sfx), [E, 128, WSZ], BF16) for m in range(3)]
    for ex in range(E):
        for m in range(3):
            j = wcount[m] % 2
            wcount[m] += 1
            b = wb[m][j]
            tot = KC * FF
            for pc in range(tot // PIECE):
                if m < 2:
                    nk = PIECE // FF
                    src = (w1 if m == 0 else w3).t[ex, pc * nk * 128:(pc + 1) * nk * 128, :].rearrange(
                        "(kc p) f -> p kc f", p=128)
                else:
                    per_row = D // PIECE
                    fc_i, hf = pc // per_row, pc % per_row
                    src = w2.t[ex, fc_i * 128:(fc_i + 1) * 128, hf * PIECE:(hf + 1) * PIECE]
                load_cast(b, pc * PIECE, src)
            P.dma(TQ, wcache[m][ex], b[:, 0:WSZ], r=[b], w=[wcache[m]])

    def load_w(m, e_idx):
        j = wcount[m] % 2
        wcount[m] += 1
        b = wb[m][j]
        P.dma('sp', b[:, 0:WSZ], wcache[m][e_idx], r=[wcache[m]], w=[b])
        return b

    for g in range(ngroup):
        gset = tile_set[g * 4]
        assert all(tile_set[g * 4 + i] == gset for i in range(4))
        if gset != cur_set:
            cur_set = gset
            P.dma(TQ, m_a[:], modap(cur_set, 1), w=[m_a])
            P.dma(TQ, m_b[:], modap(cur_set, 2), w=[m_b])
            P.dma(TQ, htmp[:], gnap(), w=[htmp])
            P.op('dve', lambda e: e.scalar_tensor_tensor(out=m_a[:], in0=m_a[:], scalar=1.0, in1=htmp[:],
                                                          op0=ALU.add, op1=ALU.mult), r=[m_a, htmp], w=[m_a])
        for sub in range(4):
            t = g * 4 + sub
            P.dma(TQ, xt[:], x1s[t * 128:(t + 1) * 128, :], r=[x1s], w=[xt])
            P.op('act', lambda e: e.activation(out=htmp[:], in_=xt[:], func=AF.Square, accum_out=sm[:, 0:1]),
                 r=[xt], w=[htmp, sm])
            P.op('act', lambda e: e.activation(out=sm[:, 1:2], in_=sm[:, 0:1], func=AF.Sqrt, bias=epsb[:, 0:1],
                                               scale=1.0 / D), r=[sm, epsb], w=[sm])
            P.op('dve', lambda e: e.reciprocal(out=sm[:, 2:3], in_=sm[:, 1:2]), r=[sm], w=[sm])
            P.op('dve', lambda e: e.scalar_tensor_tensor(out=htmp[:], in0=xt[:], scalar=sm[:, 2:3], in1=m_a[:],
                                                         op0=ALU.mult, op1=ALU.mult), r=[xt, sm, m_a], w=[htmp])
            P.op('pool', lambda e: e.tensor_tensor(out=htmp[:], in0=htmp[:], in1=m_b[:], op=ALU.add),
                 r=[htmp, m_b], w=[htmp])
            if stage == 'R1':
                P.dma(TQ, out[t * 128:(t + 1) * 128, :], htmp[:], r=[htmp], w=[out])
                continue

            def ev(q, bank, sub=sub):
                P.op('act', lambda e, q=q, bank=bank: e.copy(
                    out=tmpA[:, q * 512:(q + 1) * 512], in_=bank[:]), r=[bank], w=[tmpA])
                P.op('dve', lambda e, q=q, bank=bank, sub=sub: e.tensor_copy(
                    out=hT_bf[:, q * 4:(q + 1) * 4, sub * 128:(sub + 1) * 128],
                    in_=tmpA[:, q * 512:(q + 1) * 512].rearrange("p (a b) -> p a b", a=4)), r=[tmpA], w=[hT_bf])
            transpose_to(htmp, ev, [tmpA, hT_bf])
            for kc in range(KC):
                P.op('pe', lambda e, kc=kc: e.matmul(TB[0][:, 0:NR], lhsT=tmpA[:, kc * 128:(kc + 1) * 128],
                                                     rhs=wr_sb[:, kc, :], start=(kc == 0), stop=(kc == KC - 1)),
                     r=[tmpA, wr_sb], w=[TB[0]])
            P.op('dve', lambda e: e.tensor_copy(out=lg[:], in_=TB[0][:, 0:NR]), r=[TB[0]], w=[lg])
            if stage == 'R2':
                P.op('dve', lambda e: e.memset(htmp[:], 0.0), w=[htmp])
                P.op('dve', lambda e: e.tensor_copy(out=htmp[:, 0:NR], in_=lg[:]), r=[lg], w=[htmp])
                P.dma(TQ, out[t * 128:(t + 1) * 128, :], htmp[:], r=[htmp], w=[out])
                continue
            V = lambda fn, r, w: P.op('dve', fn, r=r, w=w)
            V(lambda e: e.tensor_reduce(out=sm[:, 3:4], in_=lg[:, 0:NG], axis=AX.X, op=ALU.max), [lg], [sm])
            V(lambda e: e.tensor_scalar(out=sm[:, 4:5], in0=sm[:, 3:4], scalar1=-1.0, scalar2=None, op0=ALU.mult),
              [sm], [sm])
            P.op('act', lambda e: e.activation(out=r4[:], in_=lg[:, 0:NG], func=AF.Exp, bias=sm[:, 4:5], scale=1.0,
                                               accum_out=sm[:, 5:6]), r=[lg, sm], w=[r4, sm])
            V(lambda e: e.reciprocal(out=sm[:, 6:7], in_=sm[:, 5:6]), [sm], [sm])
            V(lambda e: e.tensor_scalar(out=r4[:], in0=lg[:, 0:NG], scalar1=sm[:, 3:4], scalar2=None,
                                        op0=ALU.is_equal), [lg, sm], [r4])
            V(lambda e: e.tensor_scalar(out=r4[:], in0=r4[:], scalar1=1.0, scalar2=1e30, op0=ALU.subtract,
                                        op1=ALU.mult), [r4], [r4])
            V(lambda e: e.tensor_tensor(out=r1[:].rearrange("p (g k) -> p g k", g=NG),
                                        in0=lg[:, NG:NR].rearrange("p (g k) -> p g k", g=NG),
                                        in1=AP3(r4[:], [[1, NG], [0, EG]]), op=ALU.add), [lg, r4], [r1])
            V(lambda e: e.tensor_reduce(out=sm[:, 7:8], in_=r1[:], axis=AX.X, op=ALU.max), [r1], [sm])
            V(lambda e: e.tensor_scalar(out=r2[:], in0=r1[:], scalar1=sm[:, 7:8], scalar2=None, op0=ALU.is_equal),
              [r1, sm], [r2])
            V(lambda e: e.scalar_tensor_tensor(out=r1[:], in0=r2[:], scalar=-1e30, in1=r1[:], op0=ALU.mult,
                                               op1=ALU.add), [r1, r2], [r1])
            V(lambda e: e.tensor_reduce(out=sm[:, 8:9], in_=r1[:], axis=AX.X, op=ALU.max), [r1], [sm])
            V(lambda e: e.tensor_scalar(out=r3[:], in0=r1[:], scalar1=sm[:, 8:9], scalar2=None, op0=ALU.is_equal),
              [r1, sm], [r3])
            V(lambda e: e.tensor_tensor(out=sm[:, 9:10], in0=sm[:, 8:9], in1=sm[:, 7:8], op=ALU.subtract),
              [sm], [sm])
            P.op('act', lambda e: e.activation(out=sm[:, 10:11], in_=sm[:, 9:10], func=AF.Exp), r=[sm], w=[sm])
            V(lambda e: e.tensor_scalar(out=sm[:, 11:12], in0=sm[:, 10:11], scalar1=1.0, scalar2=None, op0=ALU.add),
              [sm], [sm])
            V(lambda e: e.reciprocal(out=sm[:, 11:12], in_=sm[:, 11:12]), [sm], [sm])
            V(lambda e: e.tensor_tensor(out=sm[:, 12:13], in0=sm[:, 10:11], in1=sm[:, 11:12], op=ALU.mult),
              [sm], [sm])
            V(lambda e: e.tensor_scalar(out=sm[:, 11:13], in0=sm[:, 11:13], scalar1=sm[:, 6:7], scalar2=None,
                                        op0=ALU.mult), [sm], [sm])
            V(lambda e: e.tensor_scalar(out=r2[:], in0=r2[:], scalar1=sm[:, 11:12], scalar2=None, op0=ALU.mult),
              [r2, sm], [r2])
            V(lambda e, sub=sub: e.scalar_tensor_tensor(out=G[:, sub, :], in0=r3[:], scalar=sm[:, 12:13], in1=r2[:],
                                                        op0=ALU.mult, op1=ALU.add), [r3, r2, sm], [G])
        if stage in ('R1', 'R2'):
            continue
        if stage == 'R':
            for sub in range(4):
                t = g * 4 + sub
                P.op('dve', lambda e, sub=sub: e.memset(htmp[:], 0.0), w=[htmp])
                P.op('dve', lambda e, sub=sub: e.tensor_copy(out=htmp[:, 0:E], in_=G[:, sub, :]), r=[G], w=[htmp])
                P.dma(TQ, out[t * 128:(t + 1) * 128, :], htmp[:], r=[htmp], w=[out])
            continue
        for ex in range(E):
            w1b = load_w(0, ex)
            w3b = load_w(1, ex)
            w2b = load_w(2, ex)
            aT = actT[ex % 2]
            for fc in range(FFC):
                hp1 = TB[(fc % 2) * 2]
                hp3 = TB[(fc % 2) * 2 + 1]
                for (hp, wbuf) in ((hp1, w1b), (hp3, w3b)):
                    for kc in range(KC):
                        P.op('pe', lambda e, hp=hp, wbuf=wbuf, kc=kc, fc=fc: e.matmul(
                            hp[:], lhsT=wbuf[:, kc * FF + fc * 128: kc * FF + (fc + 1) * 128], rhs=hT_bf[:, kc, :],
                            start=(kc == 0), stop=(kc == KC - 1)), r=[wbuf, hT_bf], w=[hp])
                sb = s_sb[0]
                P.op('act', lambda e, hp1=hp1, sb=sb: e.activation(out=sb[:], in_=hp1[:], func=AF.Silu),
                     r=[hp1], w=[sb])
                P.op('dve', lambda e, hp3=hp3, sb=sb, aT=aT, fc=fc: e.tensor_tensor(
                    out=aT[:, fc, :], in0=hp3[:], in1=sb[:], op=ALU.mult), r=[hp3, sb], w=[aT])
            for sub in range(4):
                for ds in range(DS):
                    for fc in range(FFC):
                        P.op('pe', lambda e, sub=sub, ds=ds, fc=fc, aT=aT, w2b=w2b: e.matmul(
                            YB[ds][:], lhsT=aT[:, fc, sub * 128:(sub + 1) * 128],
                            rhs=w2b[:, fc * D + ds * 512: fc * D + (ds + 1) * 512],
                            start=(fc == 0), stop=(fc == FFC - 1)), r=[aT, w2b], w=[YB[ds]])
                    if ex == 0:
                        P.op('dve', lambda e, sub=sub, ds=ds, ex=ex: e.tensor_scalar(
                            out=y_acc[:, sub, ds * 512:(ds + 1) * 512], in0=YB[ds][:], scalar1=G[:, sub, ex:ex + 1],
                            scalar2=None, op0=ALU.mult), r=[YB[ds], G], w=[y_acc])
                    else:
                        P.op('dve', lambda e, sub=sub, ds=ds, ex=ex: e.scalar_tensor_tensor(
                            out=y_acc[:, sub, ds * 512:(ds + 1) * 512], in0=YB[ds][:], scalar=G[:, sub, ex:ex + 1],
                            in1=y_acc[:, sub, ds * 512:(ds + 1) * 512], op0=ALU.mult, op1=ALU.add),
                            r=[YB[ds], G, y_acc], w=[y_acc])
        P.dma(TQ, tmpA[:], modap(cur_set, 3), w=[tmpA])
        m_c = tmpA
        for sub in range(4):
            t = g * 4 + sub
            P.dma(TQ, xt[:], x1s[t * 128:(t + 1) * 128, :], r=[x1s], w=[xt])
            P.op('pool', lambda e, sub=sub: e.tensor_tensor(out=htmp[:], in0=y_acc[:, sub, :], in1=m_c[:], op=ALU.mult),
                 r=[y_acc, m_c], w=[htmp])
            P.op('pool', lambda e: e.tensor_tensor(out=htmp[:], in0=htmp[:], in1=xt[:], op=ALU.add),
                 r=[htmp, xt], w=[htmp])
            P.dma(TQ, out[t * 128:(t + 1) * 128, :], htmp[:], r=[htmp], w=[out])
            xd = ext["xT_dst"](t) if "xT_dst" in ext else None
            if xd is not None:
                dbuf, dap = xd
                for q in range(KC // 4):
                    bank = TB[q % 4]
                    for j in range(4):
                        kc = q * 4 + j
                        P.op('pe', lambda e, bank=bank, j=j, kc=kc: e.transpose(
                            bank[:, j * 128:(j + 1) * 128], htmp[:, kc * 128:(kc + 1) * 128], ident[:]),
                            r=[htmp, ident], w=[bank])
                    P.op('act', lambda e, bank=bank, q=q: e.copy(out=xt[:, q * 512:(q + 1) * 512], in_=bank[:]),
                         r=[bank], w=[xt])
                P.dma(TQ, dap, xt[:].rearrange("p (k t) -> p k t", k=KC), r=[xt], w=[dbuf])
    return P.finish() if own else None


def bc128(v):
    return np.ascontiguousarray(np.broadcast_to(np.asarray(v, np.float32)[None, :], (128, v.shape[-1])))


import os

NORM_EPS = 1e-6
HD = 128
GRID_W = 64
NA_ROWS, NA_COLS = 8, 16
A_WINDOW = 128


def AP3(ap, dims):
    return bass.AP(ap.tensor, ap.offset, [list(ap.ap[0])] + [list(d) for d in dims])


def build_att(NQT=32, D=2048, stage='all', P=None, ext=None):
    own = P is None
    if own:
        P = Prog()
    ext = ext or {}

    def DR(name, shape, dt, kind):
        if name in ext:
            return ext[name]
        return P.dram(name, shape, dt, kind=kind)
    KC = D // 128
    NKT = NQT + 4
    NT = NKT + 2
    NTOK = NT * 128
    NQKV = 4608
    scale = HD ** -0.5
    xh = DR("xh", [NT * 128, D], F32, "ExternalInput")
    mods = DR("mods", [2, 2, 128, D], F32, "ExternalInput")
    gmix = DR("gmix", [128, D], F32, "ExternalInput")
    w_in = DR("w_in", [D, NQKV], F32, "ExternalInput")
    gains = DR("gains", [128, 4], F32, "ExternalInput")
    ropeT = DR("ropeT", [2, 128, NTOK], F32, "ExternalInput")
    perm_d = DR("perm", [128, 128], F32, "ExternalInput")
    ident_d = DR("ident", [128, 128], F32, "ExternalInput")
    sinkbc = DR("sinkbc", [128, 8], F32, "ExternalInput")
    biasT = DR("biasT", [5, 128, 6, 8, 128], F32, "ExternalInput")
    maskA = DR("maskA", [3, 128, 3, 128], F32, "ExternalInput")
    zout = DR("zout", [(NQT + 2) * 128, D], F32, "ExternalOutput")
    modap = ext.get("modap", lambda s_, j_: mods[s_, j_])
    gmap = ext.get("gmap", lambda: gmix[:])
    QT_s = P.dram("QT_s", [16, 128, NTOK], BF16)
    KT_s = P.dram("KT_s", [10, 128, NTOK], BF16)
    V_s = P.dram("V_s", [NTOK, 1280], BF16)

    w_blk = [P.sbuf("w_blk", [128, KC * 512], BF16) for _ in range(2)]
    stg = [P.sbuf("stg", [128, 1024]) for _ in range(2)]
    hT_bf = P.sbuf("hT_bf", [128, KC * 512], BF16)
    xt = P.sbuf("xt", [128, D])
    htmp = P.sbuf("htmp", [128, D])
    m_a = P.sbuf("m_a", [128, D])
    m_b = P.sbuf("m_b", [128, D])
    ident = P.sbuf("ident", [128, 128])
    perm = P.sbuf("perm", [128, 128])
    ones_f = P.sbuf("ones_f", [128, 128])
    ones_b = P.sbuf("ones_b", [128, 128], BF16)
    zeros_b = P.sbuf("zeros_b", [128, 128], BF16)
    ones512 = P.sbuf("ones512", [128, 512], BF16)
    gsb = P.sbuf("gsb", [128, 4])
    epsb = P.sbuf("epsb", [128, 1])
    sm = P.sbuf("sm", [128, 8])
    qk = P.sbuf("qk", [128, 512])
    sq = P.sbuf("sq", [128, 512])
    rstd = P.sbuf("rstd", [128, 512])
    qn = P.sbuf("qn", [128, 512])
    t1 = P.sbuf("t1", [128, 512])
    ropeC = P.sbuf("ropeC", [128, 512])
    ropeS = P.sbuf("ropeS", [128, 512])
    ob = [P.sbuf("ob", [128, 512], BF16) for _ in range(2)]
    vob = [P.sbuf("vob", [128, 1280], BF16) for _ in range(2)]
    QTt = [P.sbuf("QTt", [128, 2048], BF16) for _ in range(2)]
    KTw = [w_blk[i].alias(w_blk[i][:, 0:10 * 768].rearrange("p (h t) -> p h t", h=10)) for i in range(2)]
    Vw0 = hT_bf.alias(hT_bf[:, 0:6 * 1280].rearrange("p (c f) -> p c f", c=6))
    Vw1 = P.sbuf("Vw1", [128, 6, 1280], BF16)
    Vw = [Vw0, Vw1]
    KTc = P.sbuf("KTc", [128, 10, 256], BF16)
    Vc = P.sbuf("Vc", [128, 2, 1280], BF16)
    bias_std = P.sbuf("bias_std", [128, 5, 8, 128])
    bias_sp = P.sbuf("bias_sp", [128, 6, 8, 128])
    mA = P.sbuf("mA", [128, 3, 128], BF16)
    mA_f = P.sbuf("mA_f", [128, 3, 128])
    mA_cur = P.sbuf("mA_cur", [128, 3, 128], BF16)
    PT = [P.sbuf("PT", [128, 512], BF16) for _ in range(3)]
    stmp = [P.sbuf("stmp", [128, 512]) for _ in range(2)]
    sinkA = P.sbuf("sinkA", [128, 2, 512])
    sk = P.sbuf("sk", [128, 8])
    den = P.sbuf("den", [128, 512])
    zT = xt.alias(xt[:])
    zo = htmp.alias(htmp[:])
    PB = [P.psum("PB", [128, 512]) for _ in range(8)]

    P.dma('sp', ident[:], ident_d[:], w=[ident])
    P.dma('sp', perm[:], perm_d[:], w=[perm])
    P.dma('sp', gsb[:], gains[:], w=[gsb])
    P.dma('sp', sk[:], sinkbc[:], w=[sk])
    P.op('dve', lambda e: e.memset(ones_f[:], 1.0), w=[ones_f])
    P.op('dve', lambda e: e.memset(ones_b[:], 1.0), w=[ones_b])
    P.op('dve', lambda e: e.memset(zeros_b[:], 0.0), w=[zeros_b])
    P.op('dve', lambda e: e.memset(ones512[:], 1.0), w=[ones512])
    P.op('dve', lambda e: e.memset(epsb[:], NORM_EPS), w=[epsb])
    P.op('act', lambda e: e.activation(out=sk[:], in_=sk[:], func=AF.Exp), r=[sk], w=[sk])
    for g in range(2):
        for hh in range(4):
            P.op('dve', lambda e, g=g, hh=hh: e.tensor_scalar(
                out=sinkA[:, g, hh * 128:(hh + 1) * 128], in0=ones_f[:], scalar1=sk[:, g * 4 + hh: g * 4 + hh + 1],
                scalar2=None, op0=ALU.mult), r=[ones_f, sk], w=[sinkA])

    stgc = [0]

    def load_cast(dst_buf, dst_off, src_ap, n):
        sb = stg[stgc[0] % 2]
        stgc[0] += 1
        if len(src_ap.shape) == 3:
            P.dma('sp', sb[:, 0:n].rearrange("p (a b) -> p a b", a=src_ap.shape[1]), src_ap, w=[sb])
        else:
            P.dma('sp', sb[:, 0:n], src_ap, w=[sb])
        P.op('pool', lambda e: e.tensor_copy(out=dst_buf[:, dst_off:dst_off + n], in_=sb[:, 0:n]), r=[sb], w=[dst_buf])

    groups = [(g * 4, 4) for g in range(NKT // 4)] + [(NKT, 2)]
    cur_set = -1
    wcnt = [0]
    obc = [0]
    for (tile0, ntl) in groups:
        gN = ntl * 128
        tok0 = tile0 * 128
        gset = 0 if tile0 < NKT else 1
        if gset != cur_set:
            cur_set = gset
            P.dma('act', m_a[:], modap(gset, 0), w=[m_a])
            P.dma('act', m_b[:], modap(gset, 1), w=[m_b])
            P.dma('act', htmp[:], gmap(), w=[htmp])
            P.op('dve', lambda e: e.scalar_tensor_tensor(out=m_a[:], in0=m_a[:], scalar=1.0, in1=htmp[:],
                                                         op0=ALU.add, op1=ALU.mult), r=[m_a, htmp], w=[m_a])
        P.dma('act', ropeC[:, 0:gN], ropeT[0, :, tok0:tok0 + gN], w=[ropeC])
        P.dma('act', ropeS[:, 0:gN], ropeT[1, :, tok0:tok0 + gN], w=[ropeS])
        for sub in range(ntl):
            t = tile0 + sub
            P.dma('act', xt[:], xh[t * 128:(t + 1) * 128, :], w=[xt])
            P.op('act', lambda e: e.activation(out=htmp[:], in_=xt[:], func=AF.Square, accum_out=sm[:, 0:1]),
                 r=[xt], w=[htmp, sm])
            P.op('act', lambda e: e.activation(out=sm[:, 1:2], in_=sm[:, 0:1], func=AF.Sqrt, bias=epsb[:, 0:1],
                                               scale=1.0 / D), r=[sm, epsb], w=[sm])
            P.op('dve', lambda e: e.reciprocal(out=sm[:, 2:3], in_=sm[:, 1:2]), r=[sm], w=[sm])
            P.op('dve', lambda e: e.scalar_tensor_tensor(out=htmp[:], in0=xt[:], scalar=sm[:, 2:3], in1=m_a[:],
                                                         op0=ALU.mult, op1=ALU.mult), r=[xt, sm, m_a], w=[htmp])
            P.op('pool', lambda e: e.tensor_tensor(out=htmp[:], in0=htmp[:], in1=m_b[:], op=ALU.add),
                 r=[htmp, m_b], w=[htmp])
            for q in range(KC // 4):
                bank = PB[q % 4]
                for j in range(4):
                    kc = q * 4 + j
                    P.op('pe', lambda e, bank=bank, j=j, kc=kc: e.transpose(
                        bank[:, j * 128:(j + 1) * 128], htmp[:, kc * 128:(kc + 1) * 128], ident[:]),
                        r=[htmp, ident], w=[bank])
                eng = 'act' if q % 2 == 0 else 'dve'
                dst = hT_bf[:, q * 4 * 512:(q + 1) * 4 * 512].rearrange("p (a b) -> p a b", a=4)[:, :, sub * 128:(sub + 1) * 128]
                src = bank[:].rearrange("p (a b) -> p a b", a=4)
                if eng == 'act':
                    P.op('act', lambda e, dst=dst, src=src: e.copy(out=dst, in_=src), r=[bank], w=[hT_bf])
                else:
                    P.op('dve', lambda e, dst=dst, src=src: e.tensor_copy(out=dst, in_=src), r=[bank], w=[hT_bf])
        for blk in range(9):
            wb_ = w_blk[wcnt[0] % 2]
            wcnt[0] += 1
            for pc in range(KC // 2):
                load_cast(wb_, pc * 1024, w_in.t[pc * 256:(pc + 1) * 256, blk * 512:(blk + 1) * 512].rearrange(
                    "(kc p) f -> p kc f", p=128), 1024)
            for cc in range(4):
                col = blk * 512 + cc * 128
                if col < 1024:
                    kind, hidx, gi, rope = 'q', col // 128, 0, True
                elif col < 1280:
                    kind, hidx, gi, rope = 'k', (col - 1024) // 128, 1, True
                elif col < 1536:
                    kind = 'v'
                elif col < 2560:
                    kind, hidx, gi, rope = 'q', 8 + (col - 1536) // 128, 2, False
                elif col < 3584:
                    kind, hidx, gi, rope = 'k', 2 + (col - 2560) // 128, 3, False
                else:
                    kind = 'v'
                if kind == 'v':
                    continue
                pb = PB[4 + (cc % 2)]
                for kc in range(KC):
                    P.op('pe', lambda e, pb=pb, wb_=wb_, kc=kc, cc=cc, gN=gN: e.matmul(
                        pb[:, 0:gN], lhsT=wb_[:, kc * 512 + cc * 128: kc * 512 + (cc + 1) * 128],
                        rhs=hT_bf[:, kc * 512: kc * 512 + gN], start=(kc == 0), stop=(kc == KC - 1)),
                        r=[wb_, hT_bf], w=[pb])
                P.op('act', lambda e, pb=pb, gN=gN: e.copy(out=qk[:, 0:gN], in_=pb[:, 0:gN]), r=[pb], w=[qk])
                P.op('pool', lambda e, gN=gN: e.tensor_tensor(out=sq[:, 0:gN], in0=qk[:, 0:gN], in1=qk[:, 0:gN],
                                                              op=ALU.mult), r=[qk], w=[sq])
                P.op('pe', lambda e, gN=gN: e.matmul(PB[6][:, 0:gN], lhsT=ones_f[:], rhs=sq[:, 0:gN], start=True,
                                                     stop=True), r=[ones_f, sq], w=[PB[6]])
                P.op('act', lambda e, gN=gN: e.activation(out=rstd[:, 0:gN], in_=PB[6][:, 0:gN], func=AF.Sqrt,
                                                          bias=epsb[:, 0:1], scale=1.0 / HD), r=[PB[6], epsb], w=[rstd])
                P.op('dve', lambda e, gN=gN: e.reciprocal(out=rstd[:, 0:gN], in_=rstd[:, 0:gN]), r=[rstd], w=[rstd])
                P.op('dve', lambda e, gN=gN, gi=gi: e.scalar_tensor_tensor(
                    out=qn[:, 0:gN], in0=qk[:, 0:gN], scalar=gsb[:, gi:gi + 1], in1=rstd[:, 0:gN], op0=ALU.mult,
                    op1=ALU.mult), r=[qk, gsb, rstd], w=[qn])
                o_ = ob[obc[0] % 2]
                obc[0] += 1
                if rope:
                    P.op('pe', lambda e, gN=gN: e.matmul(PB[7][:, 0:gN], lhsT=perm[:], rhs=qn[:, 0:gN], start=True,
                                                         stop=True), r=[perm, qn], w=[PB[7]])
                    P.op('dve', lambda e, gN=gN: e.tensor_tensor(out=t1[:, 0:gN], in0=PB[7][:, 0:gN], in1=ropeS[:, 0:gN],
                                                                 op=ALU.mult), r=[PB[7], ropeS], w=[t1])
                    P.op('pool', lambda e, gN=gN: e.tensor_tensor(out=qn[:, 0:gN], in0=qn[:, 0:gN], in1=ropeC[:, 0:gN],
                                                                  op=ALU.mult), r=[qn, ropeC], w=[qn])
                    P.op('pool', lambda e, gN=gN, o_=o_: e.tensor_tensor(out=o_[:, 0:gN], in0=qn[:, 0:gN], in1=t1[:, 0:gN],
                                                                         op=ALU.add), r=[qn, t1], w=[o_])
                else:
                    P.op('pool', lambda e, gN=gN, o_=o_: e.tensor_copy(out=o_[:, 0:gN], in_=qn[:, 0:gN]), r=[qn], w=[o_])
                dst_s = QT_s if kind == 'q' else KT_s
                P.dma('act', dst_s[hidx, :, tok0:tok0 + gN], o_[:, 0:gN], r=[o_], w=[dst_s])
            vr = None
            if blk == 2:
                vr = (256, 512, 0)
            elif blk in (7, 8):
                vr = (0, 512, 256 + (blk - 7) * 512)
            if vr is not None:
                c0, c1, vcol = vr
                for sub in range(ntl):
                    pb = PB[4 + (sub % 2)]
                    for kc in range(KC):
                        P.op('pe', lambda e, pb=pb, wb_=wb_, kc=kc, sub=sub, c0=c0, c1=c1: e.matmul(
                            pb[:, 0:c1 - c0], lhsT=hT_bf[:, kc * 512 + sub * 128: kc * 512 + (sub + 1) * 128],
                            rhs=wb_[:, kc * 512 + c0: kc * 512 + c1], start=(kc == 0), stop=(kc == KC - 1)),
                            r=[wb_, hT_bf], w=[pb])
                    vb_ = vob[sub % 2]
                    P.op('act', lambda e, pb=pb, vb_=vb_, c0=c0, c1=c1: e.copy(out=vb_[:, 0:c1 - c0], in_=pb[:, 0:c1 - c0]),
                         r=[pb], w=[vb_])
                    t = tile0 + sub
                    P.dma('act', V_s[t * 128:(t + 1) * 128, vcol:vcol + (c1 - c0)], vb_[:, 0:c1 - c0], r=[vb_], w=[V_s])


    P.dma('sp', KTc[:], KT_s.t[:, :, NKT * 128:NTOK].rearrange("h d t -> d h t"), r=[KT_s], w=[KTc])
    P.dma('sp', Vc[:], V_s.t[NKT * 128:NTOK, :].rearrange("(c p) f -> p c f", p=128), r=[V_s], w=[Vc])
    P.dma('sp', bias_std[:], biasT[0, :, 0:5], w=[bias_std])
    P.dma('sp', mA_f[:], maskA[0], w=[mA_f])
    P.op('dve', lambda e: e.tensor_copy(out=mA[:], in_=mA_f[:]), r=[mA_f], w=[mA])
    ptc = [0]

    def next_pt():
        p_ = PT[ptc[0] % 3]
        ptc[0] += 1
        return p_

    for qi in range(NQT + 2):
        is_ctx = qi >= NQT
        j = qi + 2 if not is_ctx else NKT + (qi - NQT)
        bi = qi % 2
        P.dma('sp', QTt[bi][:].rearrange("p (h t) -> p h t", h=16), QT_s.t[:, :, j * 128:(j + 1) * 128].rearrange("h d t -> d h t"), r=[QT_s], w=[QTt[bi]])
        if not is_ctx:
            if qi == 0:
                b0, nch = j - 2, 6
            elif qi == NQT - 1:
                b0, nch = j - 3, 6
            else:
                b0, nch = j - 2, 5
            a0 = j - 1
            ktw, vw = KTw[bi], Vw[bi]
            P.dma('sp', ktw[:, 0:2, 0:384], KT_s.t[0:2, :, a0 * 128:(a0 + 3) * 128].rearrange("h d t -> d h t"),
                  r=[KT_s], w=[ktw])
            P.dma('sp', ktw[:, 2:10, 0:nch * 128], KT_s.t[2:10, :, b0 * 128:(b0 + nch) * 128].rearrange("h d t -> d h t"),
                  r=[KT_s], w=[ktw])
            P.dma('sp', vw[:, 0:3, 0:256], V_s.t[a0 * 128:(a0 + 3) * 128, 0:256].rearrange("(c p) f -> p c f", p=128),
                  r=[V_s], w=[vw])
            P.dma('sp', vw[:, 0:nch, 256:1280], V_s.t[b0 * 128:(b0 + nch) * 128, 256:1280].rearrange("(c p) f -> p c f", p=128),
                  r=[V_s], w=[vw])
            spi = {0: 1, 1: 2, NQT - 2: 3, NQT - 1: 4}.get(qi)
            if spi is not None:
                P.dma('sp', bias_sp[:], biasT[spi], w=[bias_sp])
                btab = bias_sp
            else:
                btab = bias_std
            if qi in (0, NQT - 1):
                P.dma('sp', mA_f[:], maskA[1 if qi == 0 else 2], w=[mA_f])
                P.op('dve', lambda e: e.tensor_copy(out=mA_cur[:], in_=mA_f[:]), r=[mA_f], w=[mA_cur])
                mtab = mA_cur
            else:
                mtab = mA
        qt = QTt[bi]
        for g in range(2):
            OB, DB = PB[4 + g], PB[6 + g]
            chunks = ([('w', c) for c in range(3)] if not is_ctx else []) + [('c', 0), ('c', 1)]
            for ci, (kind, c) in enumerate(chunks):
                SB = PB[ci % 2]
                if kind == 'w':
                    lhs = ktw[:, g, c * 128:(c + 1) * 128]
                    kbuf = ktw
                else:
                    lhs = KTc[:, g, c * 128:(c + 1) * 128]
                    kbuf = KTc
                P.op('pe', lambda e, SB=SB, lhs=lhs, qt=qt, g=g: e.matmul(
                    SB[:], lhsT=lhs, rhs=qt[:, g * 512:(g + 1) * 512], start=True, stop=True), r=[kbuf, qt], w=[SB])
                pt = next_pt()
                P.op('act', lambda e, SB=SB, pt=pt: e.activation(out=pt[:], in_=SB[:], func=AF.Exp, scale=scale),
                     r=[SB], w=[pt])
                if kind == 'w':
                    P.op('pool', lambda e, pt=pt, mtab=mtab, c=c: e.tensor_tensor(
                        out=pt[:].rearrange("p (h q) -> p h q", h=4), in0=pt[:].rearrange("p (h q) -> p h q", h=4),
                        in1=AP3(mtab[:, c, :], [[0, 4], [1, 128]]), op=ALU.mult), r=[pt, mtab], w=[pt])
                    vl = vw[:, c, g * 128:(g + 1) * 128]
                    vbuf = vw
                else:
                    vl = Vc[:, c, g * 128:(g + 1) * 128]
                    vbuf = Vc
                first, last = (ci == 0), (ci == len(chunks) - 1)
                P.op('pe', lambda e, OB=OB, vl=vl, pt=pt, first=first, last=last: e.matmul(
                    OB[:], lhsT=vl, rhs=pt[:], start=first, stop=last), r=[vbuf, pt], w=[OB])
                P.op('pe', lambda e, DB=DB, pt=pt, first=first, last=last: e.matmul(
                    DB[:], lhsT=ones_b[:], rhs=pt[:], start=first, stop=last), r=[ones_b, pt], w=[DB])
            P.op('dve', lambda e, DB=DB, g=g: e.tensor_tensor(out=den[:], in0=DB[:], in1=sinkA[:, g, :], op=ALU.add),
                 r=[DB, sinkA], w=[den])
            P.op('dve', lambda e: e.reciprocal(out=den[:], in_=den[:]), r=[den], w=[den])
            P.op('dve', lambda e, OB=OB, g=g: e.tensor_tensor(out=zT[:, g * 512:(g + 1) * 512], in0=OB[:], in1=den[:],
                                                              op=ALU.mult), r=[OB, den], w=[zT])
        for hf in range(2):
            OB, DB = PB[4 + hf], PB[6 + hf]
            chunks = ([('w', c) for c in range(nch)] if not is_ctx else []) + [('c', 0), ('c', 1)]
            P.op('pe', lambda e, OB=OB: e.matmul(OB[:], lhsT=zeros_b[:], rhs=ones512[:], start=True, stop=False),
                 r=[zeros_b, ones512], w=[OB])
            for ci, (kind, c) in enumerate(chunks):
                SB = PB[ci % 2]
                for hh in range(4):
                    h = hf * 4 + hh
                    if kind == 'w':
                        lhs, kbuf = ktw[:, 2 + h, c * 128:(c + 1) * 128], ktw
                    else:
                        lhs, kbuf = KTc[:, 2 + h, c * 128:(c + 1) * 128], KTc
                    P.op('pe', lambda e, SB=SB, lhs=lhs, qt=qt, h=h, hh=hh: e.matmul(
                        SB[:, hh * 128:(hh + 1) * 128], lhsT=lhs, rhs=qt[:, (8 + h) * 128:(9 + h) * 128], start=True, stop=True),
                        r=[kbuf, qt], w=[SB])
                pt = next_pt()
                if kind == 'w':
                    st_ = stmp[ci % 2]
                    P.op('dve', lambda e, SB=SB, st_=st_, btab=btab, c=c, hf=hf: e.scalar_tensor_tensor(
                        out=st_[:].rearrange("p (h q) -> p h q", h=4), in0=SB[:].rearrange("p (h q) -> p h q", h=4),
                        scalar=scale, in1=btab[:, c, hf * 4:(hf + 1) * 4, :], op0=ALU.mult, op1=ALU.add),
                        r=[SB, btab], w=[st_])
                    P.op('act', lambda e, st_=st_, pt=pt: e.activation(out=pt[:], in_=st_[:], func=AF.Exp),
                         r=[st_], w=[pt])
                else:
                    P.op('act', lambda e, SB=SB, pt=pt: e.activation(out=pt[:], in_=SB[:], func=AF.Exp, scale=scale),
                         r=[SB], w=[pt])
                first, last = (ci == 0), (ci == len(chunks) - 1)
                for hh in range(4):
                    h = hf * 4 + hh
                    if kind == 'w':
                        vl, vbuf = vw[:, c, 256 + h * 128: 256 + (h + 1) * 128], vw
                    else:
                        vl, vbuf = Vc[:, c, 256 + h * 128: 256 + (h + 1) * 128], Vc
                    P.op('pe', lambda e, OB=OB, vl=vl, pt=pt, hh=hh, first=first, last=last: e.matmul(
                        OB[:, hh * 128:(hh + 1) * 128], lhsT=vl, rhs=pt[:, hh * 128:(hh + 1) * 128], start=False,
                        stop=last), r=[vbuf, pt], w=[OB])
                P.op('pe', lambda e, DB=DB, pt=pt, first=first, last=last: e.matmul(
                    DB[:], lhsT=ones_b[:], rhs=pt[:], start=first, stop=last), r=[ones_b, pt], w=[DB])
            P.op('dve', lambda e, DB=DB: e.reciprocal(out=den[:], in_=DB[:]), r=[DB], w=[den])
            P.op('dve', lambda e, OB=OB, hf=hf: e.tensor_tensor(out=zT[:, 1024 + hf * 512: 1024 + (hf + 1) * 512],
                                                                in0=OB[:], in1=den[:], op=ALU.mult), r=[OB, den], w=[zT])
        for q4 in range(4):
            bank = PB[q4 % 4]
            for jj in range(4):
                hd = q4 * 4 + jj
                P.op('pe', lambda e, bank=bank, jj=jj, hd=hd: e.transpose(
                    bank[:, jj * 128:(jj + 1) * 128], zT[:, hd * 128:(hd + 1) * 128], ident[:]),
                    r=[zT, ident], w=[bank])
            if q4 % 2 == 0:
                P.op('act', lambda e, bank=bank, q4=q4: e.copy(out=zo[:, q4 * 512:(q4 + 1) * 512], in_=bank[:]),
                     r=[bank], w=[zo])
            else:
                P.op('dve', lambda e, bank=bank, q4=q4: e.tensor_copy(out=zo[:, q4 * 512:(q4 + 1) * 512], in_=bank[:]),
                     r=[bank], w=[zo])
        P.dma('act', zout[qi * 128:(qi + 1) * 128, :], zo[:], r=[zo], w=[zout])
    return P.finish() if own else None


def att_tables(rpb, L, q0_list_b, q0_first, q0_last):
    rows = L // GRID_W

    def btab(q0, k0s):
        q_tok = q0 + np.arange(128)
        rq, cq = q_tok // GRID_W, q_tok % GRID_W
        r0 = np.clip(rq - NA_ROWS // 2, 0, rows - NA_ROWS)
        c0 = np.clip(cq - NA_COLS // 2, 0, GRID_W - NA_COLS)
        out = np.full((128, 6, 8, 128), -1e30, np.float32)
        for c, k0 in enumerate(k0s):
            k_tok = k0 + np.arange(128)
            valid = (k_tok >= 0) & (k_tok < L)
            rk, ck = np.floor_divide(k_tok, GRID_W), np.mod(k_tok, GRID_W)
            ok = (valid[:, None] & (rk[:, None] >= r0[None]) & (rk[:, None] < r0[None] + NA_ROWS)
                  & (ck[:, None] >= c0[None]) & (ck[:, None] < c0[None] + NA_COLS))
            dr = np.clip(rk[:, None] - rq[None] + NA_ROWS - 1, 0, 2 * NA_ROWS - 2)
            dc = np.clip(ck[:, None] - cq[None] + NA_COLS - 1, 0, 2 * NA_COLS - 2)
            b = rpb[:, dr, dc]
            b = np.where(ok[None], b, np.float32(-1e30))
            out[:, c] = b.transpose(1, 0, 2)
        return out

    def mtab(q0):
        q_tok = q0 + np.arange(128)
        out = np.zeros((128, 3, 128), np.float32)
        for c in range(3):
            k_tok = q0 + (c - 1) * 128 + np.arange(128)
            ok = (np.abs(q_tok[None] - k_tok[:, None]) <= A_WINDOW) & (k_tok[:, None] >= 0) & (k_tok[:, None] < L)
            out[:, c] = ok.astype(np.float32)
        return out
    std_q0 = (rows // 2) * GRID_W
    tabs = [btab(std_q0, [std_q0 + (c - 2) * 128 for c in range(5)])]
    q0, q1, q30, q31 = q0_list_b
    tabs.append(btab(q0, [q0 + (c - 2) * 128 for c in range(6)]))
    tabs.append(btab(q1, [q1 + (c - 2) * 128 for c in range(5)]))
    tabs.append(btab(q30, [q30 + (c - 2) * 128 for c in range(5)]))
    tabs.append(btab(q31, [q31 + (c - 3) * 128 for c in range(6)]))
    biasT = np.stack(tabs)
    maskA = np.stack([mtab(std_q0), mtab(q0_first), mtab(q0_last)])
    return biasT, maskA


def rope_tables(tok_global, is_ctx):
    n = len(tok_global)
    quarter = 32
    inv_freq = (10000.0 ** (-np.arange(quarter, dtype=np.float32) / quarter)).astype(np.float32)
    row = (tok_global // GRID_W).astype(np.float32)
    col = (tok_global % GRID_W).astype(np.float32)
    C = np.ones((128, n), np.float32)
    S = np.zeros((128, n), np.float32)
    for d in range(128):
        half, idx = d // 64, d % 64
        pos = row if half == 0 else col
        ang = (pos * inv_freq[idx % quarter]).astype(np.float32)
        C[d] = np.cos(ang)
        S[d] = -np.sin(ang) if idx < quarter else np.sin(ang)
    C[:, is_ctx] = 1.0
    S[:, is_ctx] = 0.0
    perm = np.zeros((128, 128), np.float32)
    for d in range(128):
        idx = d % 64
        partner = d + 32 if idx < 32 else d - 32
        perm[partner, d] = 1.0
    return np.stack([C, S]), perm


def bc128(v):
    return np.ascontiguousarray(np.broadcast_to(np.asarray(v, np.float32)[None, :], (128, v.shape[-1])))


def att_inputs(NQT, L, tok_start, xb, ctxb, modlat, modctx, gmix, w_in, gains4, sink, rpb):
    D = xb.shape[1]
    NKT = NQT + 4
    tokg = tok_start - 256 + np.arange(NKT * 128)
    xh = np.zeros((NKT * 128 + 256, D), np.float32)
    ok = (tokg >= 0) & (tokg < L)
    xh[:NKT * 128][ok] = xb[tokg[ok]]
    xh[NKT * 128:] = ctxb
    allg = np.concatenate([tokg, np.zeros(256, np.int64)])
    is_ctx = np.concatenate([np.zeros(NKT * 128, bool), np.ones(256, bool)])
    ropeT, perm = rope_tables(allg, is_ctx)
    qs = [tok_start + i * 128 for i in (0, 1, NQT - 2, NQT - 1)]
    biasT, maskA = att_tables(rpb, L, qs, qs[0], qs[3])
    mods = np.stack([np.stack([bc128(modlat[0]), bc128(modlat[1])]), np.stack([bc128(modctx[0]), bc128(modctx[1])])])
    return {"xh": xh, "mods": mods, "gmix": bc128(gmix), "w_in": w_in,
            "gains": np.ascontiguousarray(np.stack(gains4, axis=1).astype(np.float32)),
            "ropeT": np.ascontiguousarray(ropeT), "perm": perm, "ident": np.eye(128, dtype=np.float32),
            "sinkbc": bc128(sink), "biasT": biasT, "maskA": maskA}


import os

NORM_EPS = 1e-6
RW_LNX_EPS = 64e-5
NCTX = 256
DEC_C = float(np.exp(-0.5))


def AP3(ap, dims):
    return bass.AP(ap.tensor, ap.offset, [list(ap.ap[0])] + [list(d) for d in dims])


def build_rwkv(NLAT=16384, D=2048, stage='all', P=None, ext=None, C0=0, hg_i=None):
    SS = int(os.environ.get('SCAN_STOP', '99'))
    own = P is None
    if own:
        P = Prog()
    ext = ext or {}

    def DR(name, shape, dt, kind):
        if name in ext:
            return ext[name]
        return P.dram(name, shape, dt, kind=kind)
    KC = D // 128
    NC_C = NCTX // 128
    NC_L = NLAT // 128
    NCH = NC_C + NC_L
    NTOK = NCTX + NLAT
    xcT = DR("xcT", [KC, 128, NCTX + 2], F32, "ExternalInput")
    xlT = DR("xlT", [KC, 128, NLAT + 2], F32, "ExternalInput")
    modv = DR("modv", [128, 2, 2, KC], F32, "ExternalInput")
    gmixv = DR("gmixv", [128, KC], F32, "ExternalInput")
    muv = DR("muv", [128, 6, KC], F32, "ExternalInput")
    wr_d = DR("wr", [D, 512], F32, "ExternalInput")
    wk_d = DR("wk", [D, 512], F32, "ExternalInput")
    wv_d = DR("wv", [D, 512], F32, "ExternalInput")
    g1_d = DR("g1", [D, 256], F32, "ExternalInput")
    g2_d = DR("g2", [256, 512], F32, "ExternalInput")
    w1_d = DR("w1", [D, 192], F32, "ExternalInput")
    a1_d = DR("a1", [D, 192], F32, "ExternalInput")
    w2_d = DR("w2", [96, 2, 512], F32, "ExternalInput")
    a2_d = DR("a2", [96, 2, 512], F32, "ExternalInput")
    rows_d = DR("rows", [9, 128, 512], F32, "ExternalInput")
    masks_d = DR("masks", [4, 128, 128], F32, "ExternalInput")
    ident_d = DR("ident", [128, 128], F32, "ExternalInput")
    zout = DR("zout", [NLAT, 512], F32, "ExternalOutput")
    names = ["Rr", "Vv", "Gg", "KK", "LW0", "LW1", "KD0", "KD1", "BB0", "BB1"]
    S = ext["S"] if "S" in ext else {n: P.dram("s_" + n, [NTOK, 512], F32) for n in names}
    Yf = ext["Yf"] if "Yf" in ext else P.dram("s_Yf", [NLAT, 512], F32)
    zap = ext.get("zap", lambda lrow: zout[lrow:lrow + 128, :])
    rows_src = ext.get("rows_src", lambda: rows_d.t.rearrange("n p f -> p n f"))

    ident = P.sbuf("ident", [128, 128])
    ones_f = P.sbuf("ones_f", [128, 128])
    epsb = P.sbuf("epsb", [128, 4])
    rows = P.sbuf("rows", [128, 9, 512])
    masks = P.sbuf("masks", [128, 4, 128])
    P.dma('sp', ident[:], ident_d[:], w=[ident])
    P.dma('sp', rows[:], rows_src(), w=[rows])
    P.dma('sp', masks[:], masks_d.t.rearrange("n p f -> p n f"), w=[masks])
    P.op('dve', lambda e: e.memset(ones_f[:], 1.0), w=[ones_f])
    P.op('dve', lambda e: e.memset(epsb[:, 0:1], NORM_EPS), w=[epsb])
    P.op('dve', lambda e: e.memset(epsb[:, 1:2], 1e-12), w=[epsb])
    P.op('dve', lambda e: e.memset(epsb[:, 2:3], RW_LNX_EPS), w=[epsb])
    PB = [P.psum("PB", [128, 512]) for _ in range(8)]
    pbc = [0]

    def nb():
        b = PB[pbc[0] % 8]
        pbc[0] += 1
        return b

    wr = P.sbuf("wr", [128, KC * 512], BF16)
    wk = P.sbuf("wk", [128, KC * 512], BF16)
    wv = P.sbuf("wv", [128, KC * 512], BF16)
    g1 = P.sbuf("g1", [128, KC * 256], BF16)
    w1 = P.sbuf("w1", [128, KC * 192], BF16)
    a1 = P.sbuf("a1", [128, KC * 192], BF16)
    g2 = P.sbuf("g2", [128, 2 * 512], BF16)
    w2 = P.sbuf("w2", [96, 2 * 512], BF16)
    a2 = P.sbuf("a2", [96, 2 * 512], BF16)
    stg = [P.sbuf("stg", [128, 1024]) for _ in range(2)]
    stgc = [0]

    def load_cast(dst_buf, np_, dst_off, src_ap, n):
        sb = stg[stgc[0] % 2]
        stgc[0] += 1
        if len(src_ap.shape) == 3:
            P.dma('sp', sb[0:np_, 0:n].rearrange("p (a b) -> p a b", a=src_ap.shape[1]), src_ap, w=[sb])
        else:
            P.dma('sp', sb[0:np_, 0:n], src_ap, w=[sb])
        P.op('pool', lambda e: e.tensor_copy(out=dst_buf[0:np_, dst_off:dst_off + n], in_=sb[0:np_, 0:n]), r=[sb], w=[dst_buf])

    for (dst, src, ncol) in ((wr, wr_d, 512), (wk, wk_d, 512), (wv, wv_d, 512), (g1, g1_d, 256), (w1, w1_d, 192),
                             (a1, a1_d, 192)):
        per = 1024 // ncol
        for pc in range((KC + per - 1) // per):
            k0 = pc * per
            nk = min(per, KC - k0)
            cc0 = C0 if ncol == 512 else 0
            load_cast(dst, 128, k0 * ncol, src.t[k0 * 128:(k0 + nk) * 128, cc0:cc0 + ncol].rearrange("(kc p) f -> p kc f", p=128),
                      nk * ncol)
    load_cast(g2, 128, 0, g2_d.t[:, C0:C0 + 512].rearrange("(c p) f -> p c f", p=128), 1024)
    load_cast(w2, 96, 0, w2_d[:, :, C0:C0 + 512], 1024)
    load_cast(a2, 96, 0, a2_d[:, :, C0:C0 + 512], 1024)
    mods = P.sbuf("mods", [128, 2, 2, KC])
    gmx = P.sbuf("gmx", [128, KC])
    mu = P.sbuf("mu", [128, 6, KC])
    if "modv_loader" in ext:
        ext["modv_loader"](P, mods)
    else:
        P.dma('sp', mods[:], modv[:], w=[mods])
    P.dma('sp', gmx[:], gmixv[:], w=[gmx])
    P.dma('sp', mu[:], muv[:], w=[mu])
    G1 = P.sbuf("G1", [128, 2, KC])
    for st_ in range(2):
        P.op('dve', lambda e, st_=st_: e.scalar_tensor_tensor(out=G1[:, st_, :], in0=mods[:, st_, 0, :], scalar=1.0,
                                                              in1=gmx[:], op0=ALU.add, op1=ALU.mult),
             r=[mods, gmx], w=[G1])
    xe = P.sbuf("xe", [128, KC, 130])
    sq = P.sbuf("sq", [128, KC, 130])
    hh = P.sbuf("hh", [128, KC, 130])
    rs = P.sbuf("rs", [128, 130])
    xx = P.sbuf("xx", [128, KC, 128])
    tm = P.sbuf("tm", [128, KC, 128])
    xm = [P.sbuf("xm", [128, KC, 128], BF16) for _ in range(6)]
    ggT = [P.sbuf("ggT", [128, 128], BF16) for _ in range(2)]
    lrT = [P.sbuf("lrT", [96, 128], BF16) for _ in range(2)]
    E = {n: P.sbuf("e_" + n, [128, 512]) for n in ["r", "k", "v", "g", "kx", "kk", "t0", "t1", "as0", "as1", "lw0", "lw1",
                                                   "kd0", "kd1", "b0", "b1"]}
    sm = P.sbuf("sm", [128, 32])

    def mm_tm(dst_ps, xsrc, wsb, ncol, c0=0, cw=None):
        cw = ncol if cw is None else cw
        for kc in range(KC):
            P.op('pe', lambda e, kc=kc: e.matmul(dst_ps[:, 0:cw], lhsT=xsrc[:, kc, :],
                                                 rhs=wsb[:, kc * ncol + c0: kc * ncol + c0 + cw],
                                                 start=(kc == 0), stop=(kc == KC - 1)), r=[xsrc, wsb], w=[dst_ps])

    for c in range(NCH):
        is_ctx = c < NC_C
        st_ = 1 if is_ctx else 0
        src = xcT if is_ctx else xlT
        lc = c if is_ctx else c - NC_C
        nloc = NC_C if is_ctx else NC_L
        tok0 = c * 128
        P.dma('act', xe[:], src.t[:, :, lc * 128: lc * 128 + 130].rearrange("k p t -> p k t"), w=[xe])
        P.op('pool', lambda e: e.tensor_tensor(out=sq[:], in0=xe[:], in1=xe[:], op=ALU.mult), r=[xe], w=[sq])
        ssb = nb()
        for kc in range(KC):
            P.op('pe', lambda e, kc=kc, ssb=ssb: e.matmul(ssb[:, 0:130], lhsT=ones_f[:], rhs=sq[:, kc, :], start=(kc == 0),
                                                          stop=(kc == KC - 1)), r=[ones_f, sq], w=[ssb])
        P.op('act', lambda e, ssb=ssb: e.activation(out=rs[:], in_=ssb[:, 0:130], func=AF.Sqrt, bias=epsb[:, 0:1],
                                                    scale=1.0 / D), r=[ssb, epsb], w=[rs])
        P.op('dve', lambda e: e.reciprocal(out=rs[:], in_=rs[:]), r=[rs], w=[rs])
        P.op('dve', lambda e: e.tensor_tensor(out=hh[:], in0=xe[:], in1=AP3(rs[:], [[0, KC], [1, 130]]), op=ALU.mult),
             r=[xe, rs], w=[hh])
        P.op('pool', lambda e, st_=st_: e.tensor_tensor(out=hh[:], in0=hh[:], in1=AP3(G1[:, st_, :], [[1, KC], [0, 130]]),
                                                        op=ALU.mult), r=[hh, G1], w=[hh])
        P.op('pool', lambda e, st_=st_: e.tensor_tensor(out=hh[:], in0=hh[:],
                                                        in1=AP3(mods[:, st_, 1, :], [[1, KC], [0, 130]]), op=ALU.add),
             r=[hh, mods], w=[hh])
        if lc == 0:
            P.op('pool', lambda e: e.memset(hh[:, :, 0:1], 0.0), w=[hh])
        if lc == nloc - 1:
            P.op('pool', lambda e: e.memset(hh[:, :, 129:130], 0.0), w=[hh])
        P.op('dve', lambda e: e.tensor_tensor(out=xx[:], in0=hh[:, :, 0:128], in1=hh[:, :, 2:130], op=ALU.add),
             r=[hh], w=[xx])
        P.op('dve', lambda e: e.scalar_tensor_tensor(out=xx[:], in0=xx[:], scalar=0.5, in1=hh[:, :, 1:129], op0=ALU.mult,
                                                     op1=ALU.subtract), r=[xx, hh], w=[xx])
        for m in range(6):
            P.op('pool', lambda e, m=m: e.tensor_tensor(out=tm[:], in0=xx[:], in1=AP3(mu[:, m, :], [[1, KC], [0, 128]]),
                                                        op=ALU.mult), r=[xx, mu], w=[tm])
            P.op('dve', lambda e, m=m: e.tensor_tensor(out=xm[m][:], in0=tm[:], in1=hh[:, :, 1:129], op=ALU.add),
                 r=[tm, hh], w=[xm[m]])
        for (nm, m, wsb) in (("r", 0, wr), ("k", 2, wk), ("v", 3, wv)):
            pb = nb()
            mm_tm(pb, xm[m], wsb, 512)
            P.op('act', lambda e, pb=pb, nm=nm: e.copy(out=E[nm][:], in_=pb[:]), r=[pb], w=[E[nm]])
        for jc in range(2):
            pb = nb()
            for kc in range(KC):
                P.op('pe', lambda e, kc=kc, jc=jc, pb=pb: e.matmul(
                    pb[:, 0:128], lhsT=g1[:, kc * 256 + jc * 128: kc * 256 + (jc + 1) * 128], rhs=xm[5][:, kc, :],
                    start=(kc == 0), stop=(kc == KC - 1)), r=[g1, xm[5]], w=[pb])
            P.op('act', lambda e, pb=pb, jc=jc: e.activation(out=ggT[jc][:], in_=pb[:, 0:128], func=AF.Sigmoid),
                 r=[pb], w=[ggT[jc]])
        pb = nb()
        for jc in range(2):
            P.op('pe', lambda e, jc=jc, pb=pb: e.matmul(pb[:], lhsT=ggT[jc][:], rhs=g2[:, jc * 512:(jc + 1) * 512],
                                                        start=(jc == 0), stop=(jc == 1)), r=[ggT[jc], g2], w=[pb])
        P.op('act', lambda e, pb=pb: e.copy(out=E["g"][:], in_=pb[:]), r=[pb], w=[E["g"]])
        for d in range(2):
            for (kind, m, l1, l2, rowi, dst) in (("w", 1, w1, w2, d, "lw%d" % d), ("a", 4, a1, a2, 2 + d, "as%d" % d)):
                pb = nb()
                for kc in range(KC):
                    P.op('pe', lambda e, kc=kc, pb=pb, l1=l1, m=m, d=d: e.matmul(
                        pb[0:96, 0:128], lhsT=l1[:, kc * 192 + d * 96: kc * 192 + (d + 1) * 96], rhs=xm[m][:, kc, :],
                        start=(kc == 0), stop=(kc == KC - 1)), r=[l1, xm[m]], w=[pb])
                lt = lrT[0 if kind == "w" else 1]
                if kind == "w":
                    P.op('act', lambda e, pb=pb, lt=lt: e.activation(out=lt[:], in_=pb[0:96, 0:128], func=AF.Tanh),
                         r=[pb], w=[lt])
                else:
                    P.op('act', lambda e, pb=pb, lt=lt: e.copy(out=lt[:], in_=pb[0:96, 0:128]), r=[pb], w=[lt])
                pb2 = nb()
                P.op('pe', lambda e, pb2=pb2, lt=lt, l2=l2, d=d: e.matmul(
                    pb2[:], lhsT=lt[:], rhs=l2[:, d * 512:(d + 1) * 512], start=True, stop=True), r=[lt, l2], w=[pb2])
                P.op('dve', lambda e, pb2=pb2, rowi=rowi: e.tensor_tensor(out=E["t0"][:], in0=pb2[:], in1=rows[:, rowi, :],
                                                                          op=ALU.add), r=[pb2, rows], w=[E["t0"]])
                P.op('act', lambda e, dst=dst: e.activation(out=E[dst][:], in_=E["t0"][:], func=AF.Sigmoid),
                     r=[E["t0"]], w=[E[dst]])
                if kind == "w":
                    P.op('pool', lambda e, dst=dst: e.tensor_scalar(out=E[dst][:], in0=E[dst][:], scalar1=-DEC_C,
                                                                    scalar2=None, op0=ALU.mult), r=[E[dst]], w=[E[dst]])
        P.op('dve', lambda e: e.tensor_tensor(out=E["kx"][:], in0=E["k"][:], in1=rows[:, 4, :], op=ALU.mult),
             r=[E["k"], rows], w=[E["kx"]])
        P.op('pool', lambda e: e.tensor_tensor(out=E["t1"][:], in0=E["kx"][:], in1=E["kx"][:], op=ALU.mult),
             r=[E["kx"]], w=[E["t1"]])
        P.op('dve', lambda e: e.tensor_reduce(out=sm[:, 0:8], in_=E["t1"][:].rearrange("p (h n) -> p h n", h=8),
                                              axis=AX.X, op=ALU.add), r=[E["t1"]], w=[sm])
        P.op('act', lambda e: e.activation(out=sm[:, 8:16], in_=sm[:, 0:8], func=AF.Sqrt, bias=epsb[:, 1:2], scale=1.0),
             r=[sm, epsb], w=[sm])
        P.op('dve', lambda e: e.reciprocal(out=sm[:, 8:16], in_=sm[:, 8:16]), r=[sm], w=[sm])
        P.op('dve', lambda e: e.tensor_tensor(out=E["kk"][:].rearrange("p (h n) -> p h n", h=8),
                                              in0=E["kx"][:].rearrange("p (h n) -> p h n", h=8),
                                              in1=AP3(sm[:, 8:16], [[1, 8], [0, 64]]), op=ALU.mult),
             r=[E["kx"], sm], w=[E["kk"]])
        for d in range(2):
            a_ = E["as%d" % d]
            P.op('dve', lambda e, a_=a_: e.scalar_tensor_tensor(out=E["t1"][:], in0=a_[:], scalar=-1.0, in1=rows[:, 5, :],
                                                                op0=ALU.add, op1=ALU.mult), r=[a_, rows], w=[E["t1"]])
            P.op('dve', lambda e, d=d: e.scalar_tensor_tensor(out=E["kd%d" % d][:], in0=E["t1"][:], scalar=1.0,
                                                              in1=E["k"][:], op0=ALU.add, op1=ALU.mult),
                 r=[E["t1"], E["k"]], w=[E["kd%d" % d]])
            P.op('pool', lambda e, d=d, a_=a_: e.tensor_tensor(out=E["b%d" % d][:], in0=E["kk"][:], in1=a_[:], op=ALU.mult),
                 r=[E["kk"], a_], w=[E["b%d" % d]])
        for (sn, en) in (("Rr", "r"), ("Vv", "v"), ("Gg", "g"), ("KK", "kk"), ("LW0", "lw0"), ("LW1", "lw1"),
                         ("KD0", "kd0"), ("KD1", "kd1"), ("BB0", "b0"), ("BB1", "b1")):
            P.dma('act', S[sn][tok0:tok0 + 128, :], E[en][:], r=[E[en]], w=[S[sn]])

    if stage == 'prep':
        dbg = os.environ.get('DBG', 'LW0')
        for c in range(NC_L):
            P.dma('sp', E["t0"][:], S[dbg][(NC_C + c) * 128:(NC_C + c + 1) * 128, :], r=[S[dbg]], w=[E["t0"]])
            P.dma('sp', zout[c * 128:(c + 1) * 128, :], E["t0"][:], r=[E["t0"]], w=[zout])
        return P.finish()

    P.barrier()
    def carve(buf, n_views, shape, is_bf=True):
        nel = int(np.prod(shape[1:]))
        outl = []
        base = buf[:]
        if len(base.shape) == 3:
            base = base.rearrange("p a b -> p (a b)")
        for i in range(n_views):
            if is_bf:
                ap = base[:, i * nel * 2:(i + 1) * nel * 2].bitcast(F32)
            else:
                ap = base[:, i * nel:(i + 1) * nel]
            if len(shape) == 3:
                ap = ap.rearrange("p (a b) -> p a b", a=shape[1])
            outl.append(P.view("cv", ap))
        return outl
    gr_a = carve(wr, 4, [128, 8, 128])
    gr_b = carve(wk, 4, [128, 8, 128])
    gr_c = carve(wv, 4, [128, 8, 128])
    LabT, Lab, LakT, MrbT = gr_a
    MrkT, LT_b, Lm_b, XT_b = gr_b
    LT_a, Lm_a, XT_a, _sp = gr_c
    fm = carve(g1, 4, [128, 4, 128]) + carve(w1, 2, [128, 4, 128])
    KT, BT, RT0, RT1, AT0, AT1 = fm
    RTj = [RT0, RT1]
    ATj = [AT0, AT1]
    for b_ in (RT0, AT0):
        P.op('pool', lambda e, b_=b_: e.memset(b_[64:128, :, :], 0.0), w=[b_])
    for b_ in (RT1, AT1):
        P.op('pool', lambda e, b_=b_: e.memset(b_[0:64, :, :], 0.0), w=[b_])
    pool_ = []
    for b_ in (xe, sq, hh, xx, tm):
        pool_ += carve(b_, 4, [128, 512], is_bf=False)
    for b_ in xm:
        pool_ += carve(b_, 2, [128, 512], is_bf=True)
    pool_ = pool_[::-1]
    inb = [{n: pool_.pop() for n in ["r", "v", "kk", "lw", "kd", "b"]} for _ in range(2)]
    LPx, e1, e2, Rt, Kt, Bt, At, Wsb, Usb, ysb = [pool_.pop() for _ in range(10)]
    ST = P.sbuf("ST", [128, 4, 64])
    PTs = P.sbuf("PTs", [128, 4])
    identb = P.sbuf("identb", [128, 8, 128])
    for h in range(8):
        P.op('pool', lambda e, h=h: e.tensor_copy(out=identb[:, h, :], in_=ident[:]), r=[ident], w=[identb])
    ob = {n: pool_.pop() for n in ["yf", "g", "kd0", "t", "u"]}

    def scan_chunk(c, d, cb):
        reverse = (d == 1)
        mSU, mSL, mIU, mIL = 0, 1, 2, 3
        m_strict_st = mSL if reverse else mSU
        m_strict_ts = mSU if reverse else mSL
        m_incl_st = mIL if reverse else mIU
        I = inb[cb % 2]
        r0 = c * 128
        for (nm, sn) in (("r", "Rr"), ("v", "Vv"), ("kk", "KK"), ("lw", "LW%d" % d), ("kd", "KD%d" % d), ("b", "BB%d" % d)):
            P.dma('sp', I[nm][:], S[sn][r0:r0 + 128, :], r=[S[sn]], w=[I[nm]])
        lpb = nb()
        P.op('pe', lambda e, lpb=lpb: e.matmul(lpb[:], lhsT=masks[:, m_incl_st, :], rhs=I["lw"][:], start=True, stop=True),
             r=[masks, I["lw"]], w=[lpb])
        P.op('act', lambda e, lpb=lpb: e.activation(out=e1[:], in_=lpb[:], func=AF.Exp), r=[lpb], w=[e1])
        P.op('act', lambda e, lpb=lpb: e.activation(out=e2[:], in_=lpb[:], func=AF.Exp, scale=-1.0), r=[lpb], w=[e2])
        P.op('dve', lambda e, lpb=lpb: e.tensor_tensor(out=LPx[:], in0=lpb[:], in1=I["lw"][:], op=ALU.subtract),
             r=[lpb, I["lw"]], w=[LPx])
        P.op('act', lambda e: e.activation(out=LPx[:], in_=LPx[:], func=AF.Exp), r=[LPx], w=[LPx])
        P.op('dve', lambda e: e.tensor_tensor(out=Rt[:], in0=I["r"][:], in1=e1[:], op=ALU.mult), r=[I["r"], e1], w=[Rt])
        P.op('pool', lambda e: e.tensor_tensor(out=Kt[:], in0=I["kd"][:], in1=e2[:], op=ALU.mult), r=[I["kd"], e2], w=[Kt])
        P.op('pool', lambda e: e.tensor_tensor(out=Bt[:], in0=I["b"][:], in1=e2[:], op=ALU.mult), r=[I["b"], e2], w=[Bt])
        P.op('dve', lambda e: e.scalar_tensor_tensor(out=At[:], in0=I["kk"][:], scalar=-1.0, in1=LPx[:], op0=ALU.mult,
                                                     op1=ALU.mult), r=[I["kk"], LPx], w=[At])
        if SS <= 1:
            return None, I
        ptb = nb()
        for g in range(4):
            P.op('pe', lambda e, g=g, ptb=ptb: e.matmul(ptb[:, g:g + 1], lhsT=I["lw"][:, g * 128:(g + 1) * 128],
                                                        rhs=ones_f[:, 0:1], start=True, stop=True),
                 r=[I["lw"], ones_f], w=[ptb])
        P.op('act', lambda e, ptb=ptb: e.activation(out=PTs[:], in_=ptb[:, 0:4], func=AF.Exp), r=[ptb], w=[PTs])
        if SS <= 2:
            return None, I
        for (src, dsts) in ((Rt, RTj), (Kt, [KT]), (Bt, [BT]), (At, ATj)):
            tb = nb()
            for g in range(4):
                P.op('pe', lambda e, g=g, tb=tb, src=src: e.transpose(tb[:, g * 128:(g + 1) * 128],
                                                                      src[:, g * 128:(g + 1) * 128], ident[:]),
                     r=[src, ident], w=[tb])
            if len(dsts) == 1:
                dst = dsts[0]
                P.op('act', lambda e, tb=tb, dst=dst: e.copy(out=dst[:], in_=tb[:].rearrange("p (a b) -> p a b", a=4)),
                     r=[tb], w=[dst])
            else:
                for j in range(2):
                    dst = dsts[j]
                    P.op('dve', lambda e, tb=tb, dst=dst, j=j: e.tensor_copy(
                        out=dst[j * 64:(j + 1) * 64, :, :],
                        in_=tb[j * 64:(j + 1) * 64, :].rearrange("p (a b) -> p a b", a=4)), r=[tb], w=[dst])
        if SS <= 3:
            return None, I

        def fmv(buf, h):
            g, j = h // 2, h % 2
            if isinstance(buf, list):
                return buf[j][:, g, :]
            return buf[:, g, :]

        def bl(buf):
            return buf if isinstance(buf, list) else [buf]
        for (dst, lh, rh, mk) in ((LabT, BT, ATj, m_strict_st), (Lab, ATj, BT, m_strict_ts), (LakT, KT, ATj, m_strict_st),
                                  (MrbT, BT, RTj, m_incl_st), (MrkT, KT, RTj, m_incl_st)):
            for hq in range(2):
                gb = nb()
                for hi in range(4):
                    h = hq * 4 + hi
                    P.op('pe', lambda e, gb=gb, hi=hi, h=h, lh=lh, rh=rh: e.matmul(
                        gb[:, hi * 128:(hi + 1) * 128], lhsT=fmv(lh, h), rhs=fmv(rh, h), start=True, stop=True),
                        r=bl(lh) + bl(rh), w=[gb])
                P.op('dve', lambda e, gb=gb, hq=hq, dst=dst, mk=mk: e.tensor_tensor(
                    out=dst[:, hq * 4:(hq + 1) * 4, :], in0=gb[:].rearrange("p (a b) -> p a b", a=4),
                    in1=AP3(masks[:, mk, :], [[0, 4], [1, 128]]), op=ALU.mult), r=[gb, masks], w=[dst])
        if SS <= 4:
            return None, I
        P.op('pool', lambda e: e.tensor_tensor(out=XT_a[:], in0=LabT[:], in1=identb[:], op=ALU.add), r=[LabT, identb], w=[XT_a])
        LT, Lm, XT = LabT, Lab, XT_a
        pp = [(LT_a, Lm_a, XT_b), (LT_b, Lm_b, XT_a)]
        for it in range(6):
            LTn, Lmn, XTn = pp[it % 2]
            last = (it == 5)
            for hq in range(2):
                if not last:
                    b1 = nb()
                    for hi in range(4):
                        h = hq * 4 + hi
                        P.op('pe', lambda e, b1=b1, hi=hi, h=h, Lm=Lm, LT=LT: e.matmul(
                            b1[:, hi * 128:(hi + 1) * 128], lhsT=Lm[:, h, :], rhs=LT[:, h, :], start=True, stop=True),
                            r=[Lm, LT], w=[b1])
                    P.op('act', lambda e, b1=b1, LTn=LTn, hq=hq: e.copy(
                        out=LTn[:, hq * 4:(hq + 1) * 4, :], in_=b1[:].rearrange("p (a b) -> p a b", a=4)), r=[b1], w=[LTn])
                b2 = nb()
                for hi in range(4):
                    h = hq * 4 + hi
                    P.op('pe', lambda e, b2=b2, hi=hi, h=h, Lm=Lm, LT=LT: e.matmul(
                        b2[:, hi * 128:(hi + 1) * 128], lhsT=LT[:, h, :], rhs=Lm[:, h, :], start=True, stop=True),
                        r=[Lm, LT], w=[b2])
                P.op('act' if last else 'dve', (lambda e, b2=b2, Lmn=Lmn, hq=hq: e.copy(
                    out=Lmn[:, hq * 4:(hq + 1) * 4, :], in_=b2[:].rearrange("p (a b) -> p a b", a=4))) if last else
                    (lambda e, b2=b2, Lmn=Lmn, hq=hq: e.tensor_copy(
                        out=Lmn[:, hq * 4:(hq + 1) * 4, :], in_=b2[:].rearrange("p (a b) -> p a b", a=4))),
                    r=[b2], w=[Lmn])
                b3 = nb()
                for hi in range(4):
                    h = hq * 4 + hi
                    P.op('pe', lambda e, b3=b3, hi=hi, h=h, Lmn=Lmn, XT=XT: e.matmul(
                        b3[:, hi * 128:(hi + 1) * 128], lhsT=Lmn[:, h, :], rhs=XT[:, h, :], start=True, stop=True),
                        r=[Lmn, XT], w=[b3])
                P.op('dve', lambda e, b3=b3, XTn=XTn, XT=XT, hq=hq: e.tensor_tensor(
                    out=XTn[:, hq * 4:(hq + 1) * 4, :], in0=b3[:].rearrange("p (a b) -> p a b", a=4),
                    in1=XT[:, hq * 4:(hq + 1) * 4, :], op=ALU.add), r=[b3, XT], w=[XTn])
            LT, Lm, XT = LTn, Lmn, XTn
        if SS <= 5:
            return None, I
        wbk = nb()
        for h in range(8):
            g, j = h // 2, h % 2
            P.op('pe', lambda e, h=h, g=g, j=j, wbk=wbk: e.matmul(
                wbk[:, h * 64:(h + 1) * 64], lhsT=fmv(ATj, h), rhs=ST[:, g, :], start=True, stop=False),
                r=ATj + [ST], w=[wbk])
            P.op('pe', lambda e, h=h, wbk=wbk: e.matmul(
                wbk[:, h * 64:(h + 1) * 64], lhsT=LakT[:, h, :], rhs=I["v"][:, h * 64:(h + 1) * 64], start=False, stop=True),
                r=[LakT, I["v"]], w=[wbk])
        P.op('act', lambda e, wbk=wbk: e.copy(out=Wsb[:], in_=wbk[:]), r=[wbk], w=[Wsb])
        if SS <= 6:
            return None, I
        ubk = nb()
        for h in range(8):
            P.op('pe', lambda e, h=h, ubk=ubk, XT=XT: e.matmul(
                ubk[:, h * 64:(h + 1) * 64], lhsT=XT[:, h, :], rhs=Wsb[:, h * 64:(h + 1) * 64], start=True, stop=True),
                r=[XT, Wsb], w=[ubk])
        P.op('act', lambda e, ubk=ubk: e.copy(out=Usb[:], in_=ubk[:]), r=[ubk], w=[Usb])
        ybk = nb()
        for h in range(8):
            g, j = h // 2, h % 2
            P.op('pe', lambda e, h=h, g=g, j=j, ybk=ybk: e.matmul(
                ybk[:, h * 64:(h + 1) * 64], lhsT=fmv(RTj, h), rhs=ST[:, g, :], start=True, stop=False),
                r=RTj + [ST], w=[ybk])
            P.op('pe', lambda e, h=h, ybk=ybk: e.matmul(
                ybk[:, h * 64:(h + 1) * 64], lhsT=MrbT[:, h, :], rhs=Usb[:, h * 64:(h + 1) * 64], start=False, stop=False),
                r=[MrbT, Usb], w=[ybk])
            P.op('pe', lambda e, h=h, ybk=ybk: e.matmul(
                ybk[:, h * 64:(h + 1) * 64], lhsT=MrkT[:, h, :], rhs=I["v"][:, h * 64:(h + 1) * 64], start=False, stop=True),
                r=[MrkT, I["v"]], w=[ybk])
        if SS <= 7:
            return None, I
        sbk = nb()
        for h in range(8):
            g = h // 2
            P.op('pe', lambda e, h=h, g=g, sbk=sbk: e.matmul(
                sbk[:, h * 64:(h + 1) * 64], lhsT=Bt[:, g * 128:(g + 1) * 128], rhs=Usb[:, h * 64:(h + 1) * 64],
                start=True, stop=False), r=[Bt, Usb], w=[sbk])
            P.op('pe', lambda e, h=h, g=g, sbk=sbk: e.matmul(
                sbk[:, h * 64:(h + 1) * 64], lhsT=Kt[:, g * 128:(g + 1) * 128], rhs=I["v"][:, h * 64:(h + 1) * 64],
                start=False, stop=True), r=[Kt, I["v"]], w=[sbk])
        for j in range(2):
            ps_v = AP3(sbk[j * 64:(j + 1) * 64, j * 64:(j + 1) * 64], [[128, 4], [1, 64]])
            P.op('dve', lambda e, j=j, ps_v=ps_v: e.tensor_tensor(out=ST[j * 64:(j + 1) * 64, :, :], in0=ps_v,
                                                                 in1=ST[j * 64:(j + 1) * 64, :, :], op=ALU.add),
                 r=[sbk, ST], w=[ST])
        P.op('dve', lambda e: e.tensor_tensor(out=ST[:], in0=ST[:], in1=AP3(PTs[:], [[1, 4], [0, 64]]), op=ALU.mult),
             r=[ST, PTs], w=[ST])
        return ybk, I

    cbc = [0]
    for d in range(2):
        P.op('dve', lambda e: e.memset(ST[:], 0.0), w=[ST])
        ctx_order = list(range(NC_C)) if d == 0 else list(range(NC_C - 1, -1, -1))
        lat_order = list(range(NC_C, NCH)) if d == 0 else list(range(NCH - 1, NC_C - 1, -1))
        for c in ctx_order + lat_order:
            ybk, I = scan_chunk(c, d, cbc[0])
            cbc[0] += 1
            if c < NC_C or ybk is None:
                continue
            lrow = (c - NC_C) * 128
            if d == 0:
                P.op('act', lambda e, ybk=ybk: e.copy(out=ysb[:], in_=ybk[:]), r=[ybk], w=[ysb])
                P.dma('act', Yf[lrow:lrow + 128, :], ysb[:], r=[ysb], w=[Yf])
                continue
            r0 = c * 128
            P.dma('act', ob["yf"][:], Yf[lrow:lrow + 128, :], r=[Yf], w=[ob["yf"]])
            P.dma('act', ob["g"][:], S["Gg"][r0:r0 + 128, :], r=[S["Gg"]], w=[ob["g"]])
            P.dma('act', ob["kd0"][:], S["KD0"][r0:r0 + 128, :], r=[S["KD0"]], w=[ob["kd0"]])
            y3 = lambda b_: b_[:].rearrange("p (h n) -> p h n", h=8)
            P.op('dve', lambda e, ybk=ybk: e.tensor_tensor(out=ysb[:], in0=ybk[:], in1=ob["yf"][:], op=ALU.add),
                 r=[ybk, ob["yf"]], w=[ysb])
            P.op('dve', lambda e: e.tensor_reduce(out=sm[:, 0:8], in_=y3(ysb), axis=AX.X, op=ALU.add), r=[ysb], w=[sm])
            P.op('dve', lambda e: e.tensor_scalar(out=sm[:, 0:8], in0=sm[:, 0:8], scalar1=-1.0 / 64, scalar2=None,
                                                  op0=ALU.mult), r=[sm], w=[sm])
            P.op('dve', lambda e: e.tensor_tensor(out=y3(ysb), in0=y3(ysb), in1=AP3(sm[:, 0:8], [[1, 8], [0, 64]]),
                                                  op=ALU.add), r=[ysb, sm], w=[ysb])
            P.op('pool', lambda e: e.tensor_tensor(out=ob["t"][:], in0=ysb[:], in1=ysb[:], op=ALU.mult), r=[ysb], w=[ob["t"]])
            P.op('dve', lambda e: e.tensor_reduce(out=sm[:, 8:16], in_=y3(ob["t"]), axis=AX.X, op=ALU.add),
                 r=[ob["t"]], w=[sm])
            P.op('act', lambda e: e.activation(out=sm[:, 8:16], in_=sm[:, 8:16], func=AF.Sqrt, bias=epsb[:, 2:3],
                                               scale=1.0 / 64), r=[sm, epsb], w=[sm])
            P.op('dve', lambda e: e.reciprocal(out=sm[:, 8:16], in_=sm[:, 8:16]), r=[sm], w=[sm])
            P.op('dve', lambda e: e.tensor_tensor(out=y3(ysb), in0=y3(ysb), in1=AP3(sm[:, 8:16], [[1, 8], [0, 64]]),
                                                  op=ALU.mult), r=[ysb, sm], w=[ysb])
            P.op('pool', lambda e: e.tensor_tensor(out=ysb[:], in0=ysb[:], in1=rows[:, 7, :], op=ALU.mult),
                 r=[ysb, rows], w=[ysb])
            P.op('pool', lambda e: e.tensor_tensor(out=ysb[:], in0=ysb[:], in1=rows[:, 8, :], op=ALU.add),
                 r=[ysb, rows], w=[ysb])
            P.op('dve', lambda e, I=I: e.tensor_tensor(out=ob["t"][:], in0=ob["kd0"][:], in1=I["kd"][:], op=ALU.add),
                 r=[ob["kd0"], I["kd"]], w=[ob["t"]])
            P.op('dve', lambda e, I=I: e.tensor_tensor(out=ob["t"][:], in0=ob["t"][:], in1=I["r"][:], op=ALU.mult),
                 r=[ob["t"], I["r"]], w=[ob["t"]])
            P.op('pool', lambda e: e.tensor_tensor(out=ob["t"][:], in0=ob["t"][:], in1=rows[:, 6, :], op=ALU.mult),
                 r=[ob["t"], rows], w=[ob["t"]])
            P.op('dve', lambda e: e.tensor_reduce(out=sm[:, 16:24], in_=y3(ob["t"]), axis=AX.X, op=ALU.add),
                 r=[ob["t"]], w=[sm])
            P.op('dve', lambda e, I=I: e.tensor_tensor(out=y3(ob["u"]), in0=I["v"][:].rearrange("p (h n) -> p h n", h=8),
                                                       in1=AP3(sm[:, 16:24], [[1, 8], [0, 64]]), op=ALU.mult),
                 r=[I["v"], sm], w=[ob["u"]])
            P.op('pool', lambda e: e.tensor_tensor(out=ysb[:], in0=ysb[:], in1=ob["u"][:], op=ALU.add),
                 r=[ysb, ob["u"]], w=[ysb])
            P.op('pool', lambda e: e.tensor_tensor(out=ysb[:], in0=ysb[:], in1=ob["g"][:], op=ALU.mult),
                 r=[ysb, ob["g"]], w=[ysb])
            P.dma('act', zap(lrow), ysb[:], r=[ysb], w=[zout])
    return P.finish() if own else None


def bc128(v):
    return np.ascontiguousarray(np.broadcast_to(np.asarray(v, np.float32).reshape(1, -1), (128, v.size)))


def rwkv_inputs(x1b, xcb, hg, modlat, modctx, gmix, W):
    D = x1b.shape[1]
    KC = D // 128
    F = slice(hg * 512, (hg + 1) * 512)

    def padT(x):
        xt = np.zeros((D, x.shape[0] + 2), np.float32)
        xt[:, 1:-1] = x.T
        return np.ascontiguousarray(xt.reshape(KC, 128, -1))

    def fm(v):
        return np.ascontiguousarray(np.asarray(v, np.float32).reshape(KC, 128).T)
    modv = np.stack([np.stack([fm(modlat[0]), fm(modlat[1])], axis=1), np.stack([fm(modctx[0]), fm(modctx[1])], axis=1)], axis=1)
    muv = np.stack([fm(W["mu"][m]) for m in range(6)], axis=1)
    rowsl = [W["w0"][0][F], W["w0"][1][F], W["a0"][0][F], W["a0"][1][F], W["k_k"][F], W["k_a"][F],
             W["r_k"].reshape(-1)[F], W["lnx_w"][F], W["lnx_b"][F]]
    idx = np.arange(128)
    SU = (idx[:, None] < idx[None, :]).astype(np.float32)
    masks = np.stack([SU, SU.T, SU + np.eye(128, dtype=np.float32), SU.T + np.eye(128, dtype=np.float32)])
    return {"xcT": padT(xcb), "xlT": padT(x1b), "modv": np.ascontiguousarray(modv), "gmixv": fm(gmix), "muv": np.ascontiguousarray(muv),
            "wr": np.ascontiguousarray(W["wr"][:, F]), "wk": np.ascontiguousarray(W["wk"][:, F]),
            "wv": np.ascontiguousarray(W["wv"][:, F]), "g1": np.ascontiguousarray(W["g1"]),
            "g2": np.ascontiguousarray(W["g2"][:, F]),
            "w1": np.ascontiguousarray(np.concatenate([W["w1"][0], W["w1"][1]], axis=1)),
            "a1": np.ascontiguousarray(np.concatenate([W["a1"][0], W["a1"][1]], axis=1)),
            "w2": np.ascontiguousarray(np.stack([W["w2"][0][:, F], W["w2"][1][:, F]], axis=1)),
            "a2": np.ascontiguousarray(np.stack([W["a2"][0][:, F], W["a2"][1][:, F]], axis=1)),
            "rows": np.stack([bc128(v) for v in rowsl]), "masks": masks, "ident": np.eye(128, dtype=np.float32)}


def _run(nc, in_maps):
    res = run_bass_kernel_spmd(nc, in_maps, core_ids=list(range(len(in_maps))))
    return res.results


def kernel(x, c, ctx, c_ctx, ada_w, ada_b, norm_mix_g, norm_ffn_g, attn_w_in, attn_w_out, a_q_gain, a_k_gain, a_sink,
           b_q_gain, b_k_gain, b_rpb, rw_mu, rw_wr, rw_wk, rw_wv, rw_wo, rw_w0, rw_w1, rw_w2, rw_a0, rw_a1, rw_a2, rw_g1,
           rw_g2, rw_k_k, rw_k_a, rw_r_k, rw_lnx_w, rw_lnx_b, moe_w_grp, moe_w_exp, moe_w1, moe_w3, moe_w2):
    f32 = lambda a: np.ascontiguousarray(np.asarray(a, dtype=np.float32))
    x, ctx = f32(x), f32(ctx)
    B, L, D = x.shape
    NC = 8
    QPB = NC // B
    TPC = L // QPB
    NQT = TPC // 128
    ident = np.eye(128, dtype=np.float32)
    mod = run_ada(f32(c), f32(c_ctx), f32(ada_w), f32(ada_b))

    def moe_launch(layer, x0_list, z_list, wo, modsets_list, tile_set):
        NT = x0_list[0].shape[0]
        nc = build_moe(NT, tile_set)
        wr_cat = np.ascontiguousarray(np.concatenate([f32(moe_w_grp[layer]), f32(moe_w_exp[layer])], axis=1))
        w1_, w3_, w2_ = f32(moe_w1[layer]), f32(moe_w3[layer]), f32(moe_w2[layer])
        gn = bc128(f32(norm_ffn_g[layer]))
        ims = []
        for k in range(NC):
            ms = np.stack([np.stack([bc128(v) for v in st]) for st in modsets_list[k]])
            ims.append({"x0": x0_list[k], "z": z_list[k], "wo": wo, "mods": ms, "gnorm": gn, "wr": wr_cat,
                        "w1": w1_, "w3": w3_, "w2": w2_, "ident": ident})
        return [r["out"] for r in _run(nc, ims)]

    nc = build_att(NQT=NQT)
    ims = []
    for k in range(NC):
        b, q = k // QPB, k % QPB
        ims.append(att_inputs(NQT, L, q * TPC, x[b], ctx[b], (mod[0, b, 1], mod[0, b, 0]), (mod[0, 2, 1], mod[0, 2, 0]),
                              f32(norm_mix_g[0]), f32(attn_w_in[0]),
                              [f32(a_q_gain[0]), f32(a_k_gain[0]), f32(b_q_gain[0]), f32(b_k_gain[0])],
                              f32(a_sink[0]), f32(b_rpb[0])))
    z0 = [r["zout"] for r in _run(nc, ims)]
    del ims
    pad = np.zeros((256, D), np.float32)
    x0_list, z_list, msets = [], [], []
    for k in range(NC):
        b, q = k // QPB, k % QPB
        x0_list.append(np.ascontiguousarray(np.concatenate([x[b, q * TPC:(q + 1) * TPC], ctx[b], pad], axis=0)))
        z_list.append(np.ascontiguousarray(np.concatenate([z0[k], pad], axis=0)))
        msets.append([[mod[0, b, 2], mod[0, b, 4], mod[0, b, 3], mod[0, b, 5]],
                      [mod[0, 2, 2], mod[0, 2, 4], mod[0, 2, 3], mod[0, 2, 5]]])
    o0 = moe_launch(0, x0_list, z_list, f32(attn_w_out[0]), msets, [0] * NQT + [1] * 4)
    del x0_list, z_list, z0
    x1 = np.stack([np.concatenate([o0[b * QPB + q][:TPC] for q in range(QPB)], axis=0) for b in range(B)])
    xc1 = np.stack([o0[b * QPB][TPC:TPC + 256] for b in range(B)])
    del o0
    if os.environ.get('KDUMP'):
        np.save(os.environ['KDUMP'] + '_x1.npy', x1); np.save(os.environ['KDUMP'] + '_xc1.npy', xc1)
    W = {"mu": f32(rw_mu[0]), "wr": f32(rw_wr[0]), "wk": f32(rw_wk[0]), "wv": f32(rw_wv[0]), "w0": f32(rw_w0[0]),
         "w1": f32(rw_w1[0]), "w2": f32(rw_w2[0]), "a0": f32(rw_a0[0]), "a1": f32(rw_a1[0]), "a2": f32(rw_a2[0]),
         "g1": f32(rw_g1[0]), "g2": f32(rw_g2[0]), "k_k": f32(rw_k_k[0]), "k_a": f32(rw_k_a[0]), "r_k": f32(rw_r_k[0]),
         "lnx_w": f32(rw_lnx_w[0]), "lnx_b": f32(rw_lnx_b[0])}
    nc = build_rwkv(NLAT=L)
    ims = []
    for k in range(NC):
        b, hg = k // QPB, k % QPB
        ims.append(rwkv_inputs(x1[b], xc1[b], hg, (mod[1, b, 1], mod[1, b, 0]), (mod[1, 2, 1], mod[1, 2, 0]),
                               f32(norm_mix_g[1]), W))
    zr = [r["zout"] for r in _run(nc, ims)]
    del ims
    z1 = np.stack([np.concatenate([zr[b * QPB + hg] for hg in range(QPB)], axis=1) for b in range(B)])
    del zr
    if os.environ.get('KDUMP'):
        np.save(os.environ['KDUMP'] + '_z1.npy', z1)
    x0_list, z_list, msets = [], [], []
    for k in range(NC):
        b, q = k // QPB, k % QPB
        x0_list.append(np.ascontiguousarray(x1[b, q * TPC:(q + 1) * TPC]))
        z_list.append(np.ascontiguousarray(z1[b, q * TPC:(q + 1) * TPC]))
        msets.append([[mod[1, b, 2], mod[1, b, 4], mod[1, b, 3], mod[1, b, 5]]])
    o1 = moe_launch(1, x0_list, z_list, f32(rw_wo[0]), msets, [0] * NQT)
    out = np.stack([np.concatenate([o1[b * QPB + q] for q in range(QPB)], axis=0) for b in range(B)])
    return np.ascontiguousarray(out.astype(np.float32))
```
